# Optimizing a Trainium2 kernel written in Bass

```python
import math
import jax, jax.numpy as jnp
from jax import lax
import numpy as np

D_MODEL = 1024
BATCH = 16
SEQ = 4096
DEPTH = 1

HEAD_DIM = 64
N_DIFF_HEADS = D_MODEL // 2 // (2 * HEAD_DIM)
DIFF_WIDTH = N_DIFF_HEADS * 2 * HEAD_DIM
N_DIL_HEADS = D_MODEL // 2 // HEAD_DIM
DIL_WIDTH = N_DIL_HEADS * HEAD_DIM
MIX_WIDTH = DIFF_WIDTH + DIL_WIDTH
DIL_PATTERNS = ((128, 1), (512, 4), (2048, 16))
N_BUCKETS = 32
REL_MAX_DIST = 1024
Q_BLOCK = 128
N_EXPERTS = 16
EC_K = 2
D_FF = 256 * ((8 * D_MODEL // 3 + 255) // 256)
NORM_EPS = 1e-6
SUBLN_EPS = 1e-5
NEG = -1e30

kernel_name = 'hybrid_diffattn_dilated_ec_moe_encoder'


def rms_norm(x, g, eps=NORM_EPS):
    xf = x.astype(jnp.float32)
    y = xf * lax.rsqrt(jnp.mean(xf * xf, axis=-1, keepdims=True) + eps)
    return (y * g.astype(jnp.float32)).astype(x.dtype)


def t5_bucket(rel):
    half = N_BUCKETS // 2
    max_exact = half // 2
    n = jnp.abs(rel)
    nf = jnp.maximum(n, 1).astype(jnp.float32)
    large = max_exact + (jnp.log(nf / max_exact) / math.log(REL_MAX_DIST / max_exact)
                         * (half - max_exact)).astype(jnp.int32)
    large = jnp.minimum(large, half - 1)
    return jnp.where(rel > 0, half, 0) + jnp.where(n < max_exact, n, large)


def diff_attention(qa, ka, va, lam, lambda_init, subln_g, bias_a):
    B, S = qa.shape[:2]
    H = N_DIFF_HEADS
    q = qa.reshape(B, S, H, 2, HEAD_DIM) * (HEAD_DIM ** -0.5)
    k = ka.reshape(B, S, H, 2, HEAD_DIM)
    k1 = k[:, :, :, 0].transpose(0, 2, 1, 3)
    k2 = k[:, :, :, 1].transpose(0, 2, 1, 3)
    v = va.reshape(B, S, H, 2 * HEAD_DIM).transpose(0, 2, 1, 3)
    n_blk = S // Q_BLOCK

    def to_qblocks(t):
        return t.reshape(B, n_blk, Q_BLOCK, H, HEAD_DIM).transpose(1, 0, 3, 2, 4)

    q1b = to_qblocks(q[:, :, :, 0])
    q2b = to_qblocks(q[:, :, :, 1])
    kpos = jnp.arange(S, dtype=jnp.int32)

    def block(args):
        j, q1, q2 = args
        qpos = j * Q_BLOCK + jnp.arange(Q_BLOCK, dtype=jnp.int32)
        bias = jnp.take(bias_a, t5_bucket(kpos[None, :] - qpos[:, None]), axis=0)
        bias = bias.transpose(2, 0, 1).astype(jnp.float32)[None]
        s1 = jnp.einsum('bhqd,bhkd->bhqk', q1, k1).astype(jnp.float32) + bias
        s2 = jnp.einsum('bhqd,bhkd->bhqk', q2, k2).astype(jnp.float32) + bias
        p = jax.nn.softmax(s1, axis=-1) - lam * jax.nn.softmax(s2, axis=-1)
        return jnp.einsum('bhqk,bhkd->bhqd', p.astype(v.dtype), v)

    o = lax.map(block, (jnp.arange(n_blk, dtype=jnp.int32), q1b, q2b))
    o = o.transpose(1, 0, 3, 2, 4).reshape(B, S, H, 2 * HEAD_DIM)
    o = rms_norm(o, subln_g, SUBLN_EPS) * (1.0 - lambda_init)
    return o.reshape(B, S, DIFF_WIDTH)


def dilated_pattern(q, k, v, window, dil, bias_b):
    B, S, H, Dh = q.shape
    half = window // (2 * dil)
    blk = half
    unit = dil * blk
    sp = -(-S // unit) * unit
    m_len = sp // dil
    nb = m_len // blk

    def to_blocks(t):
        t = jnp.pad(t, ((0, 0), (0, sp - S), (0, 0), (0, 0)))
        t = t.reshape(B, m_len, dil, H, Dh).transpose(0, 2, 3, 1, 4)
        return t.reshape(B, dil, H, nb, blk, Dh)

    def with_neighbours(t):
        tp = jnp.pad(t, ((0, 0), (0, 0), (0, 0), (1, 1), (0, 0), (0, 0)))
        return jnp.concatenate([tp[:, :, :, :-2], tp[:, :, :, 1:-1], tp[:, :, :, 2:]], axis=4)

    qb = to_blocks(q * (Dh ** -0.5))
    kn = with_neighbours(to_blocks(k))
    vn = with_neighbours(to_blocks(v))
    off = (jnp.arange(3 * blk, dtype=jnp.int32)[None, :] - blk
           - jnp.arange(blk, dtype=jnp.int32)[:, None])
    in_band = jnp.abs(off) <= half
    m_key = (jnp.arange(nb, dtype=jnp.int32)[:, None] * blk
             + jnp.arange(3 * blk, dtype=jnp.int32)[None, :] - blk)
    pos_key = m_key[None] * dil + jnp.arange(dil, dtype=jnp.int32)[:, None, None]
    key_ok = (m_key >= 0)[None] & (pos_key < S)
    mask = key_ok[:, :, None, :] & in_band[None, None]
    bias = jnp.take(bias_b, t5_bucket(off * dil), axis=0).transpose(2, 0, 1).astype(jnp.float32)
    s = jnp.einsum('brhnqd,brhnkd->brhnqk', qb, kn).astype(jnp.float32) + bias[None, None, :, None]
    s = jnp.where(mask[None, :, None], s, NEG)
    lse = jax.nn.logsumexp(s, axis=-1)
    p = jnp.exp(s - lse[..., None])
    o = jnp.einsum('brhnqk,brhnkd->brhnqd', p.astype(v.dtype), vn)
    o = o.reshape(B, dil, H, m_len, Dh).transpose(0, 3, 1, 2, 4).reshape(B, sp, H, Dh)[:, :S]
    lse = lse.reshape(B, dil, H, m_len).transpose(0, 3, 1, 2).reshape(B, sp, H)[:, :S]
    return o, lse


def dilated_attention(qd, kd, vd, bias_b):
    B, S = qd.shape[:2]
    q = qd.reshape(B, S, N_DIL_HEADS, HEAD_DIM)
    k = kd.reshape(B, S, N_DIL_HEADS, HEAD_DIM)
    v = vd.reshape(B, S, N_DIL_HEADS, HEAD_DIM)
    outs, lses = [], []
    for window, dil in DIL_PATTERNS:
        o, lse = dilated_pattern(q, k, v, window, dil, bias_b)
        outs.append(o)
        lses.append(lse)
    w = jax.nn.softmax(jnp.stack(lses, axis=0), axis=0)
    o = jnp.sum(w[..., None] * jnp.stack(outs, axis=0).astype(jnp.float32), axis=0)
    return o.astype(qd.dtype).reshape(B, S, DIL_WIDTH)


def expert_choice_ffn(h, w_router, w_gate, w_up, w_down):
    B, S, D = h.shape
    cap = EC_K * S // N_EXPERTS
    aff = jax.nn.softmax((h @ w_router).astype(jnp.float32), axis=-1)
    g, idx = lax.top_k(aff.transpose(0, 2, 1), cap)
    g = g.transpose(1, 0, 2)
    idx = idx.transpose(1, 0, 2)
    b_rows = jnp.arange(B, dtype=jnp.int32)[:, None]

    def run_expert(args):
        wg, wu, wd, ie, ge = args
        xe = h[b_rows, ie]
        he = jax.nn.silu(xe @ wg) * (xe @ wu)
        return (he @ wd) * ge[..., None].astype(h.dtype)

    ye = lax.map(run_expert, (w_gate, w_up, w_down, idx, g))
    b_idx = jnp.arange(B, dtype=jnp.int32)[None, :, None]
    return jnp.zeros_like(h).at[b_idx, idx].add(ye)


def setup_inputs(seed: int = 0) -> dict:
    key = jax.random.key(seed)
    ks = jax.random.split(key, 16)
    f = jnp.float32
    n_in = 3 * DIFF_WIDTH + 3 * DIL_WIDTH
    nrm = lambda k, shape, s: jax.random.normal(k, shape, f) * s
    return {
        'x': nrm(ks[0], (BATCH, SEQ, D_MODEL), 1.0),
        'norm1_g': 1.0 + nrm(ks[1], (DEPTH, D_MODEL), 0.02),
        'w_in': nrm(ks[2], (DEPTH, D_MODEL, n_in), D_MODEL ** -0.5),
        'lam_q1': nrm(ks[3], (DEPTH, HEAD_DIM), 0.1),
        'lam_k1': nrm(ks[4], (DEPTH, HEAD_DIM), 0.1),
        'lam_q2': nrm(ks[5], (DEPTH, HEAD_DIM), 0.1),
        'lam_k2': nrm(ks[6], (DEPTH, HEAD_DIM), 0.1),
        'subln_g': 1.0 + nrm(ks[7], (DEPTH, 2 * HEAD_DIM), 0.02),
        'w_out': nrm(ks[8], (DEPTH, MIX_WIDTH, D_MODEL), MIX_WIDTH ** -0.5),
        'rel_bias': nrm(ks[9], (N_BUCKETS, N_DIFF_HEADS + N_DIL_HEADS), 0.2),
        'norm2_g': 1.0 + nrm(ks[10], (DEPTH, D_MODEL), 0.02),
        'w_router': nrm(ks[11], (DEPTH, D_MODEL, N_EXPERTS), D_MODEL ** -0.5),
        'w_gate': nrm(ks[12], (DEPTH, N_EXPERTS, D_MODEL, D_FF), D_MODEL ** -0.5),
        'w_up': nrm(ks[13], (DEPTH, N_EXPERTS, D_MODEL, D_FF), D_MODEL ** -0.5),
        'w_down': nrm(ks[14], (DEPTH, N_EXPERTS, D_FF, D_MODEL), D_FF ** -0.5),
        'norm_f_g': 1.0 + nrm(ks[15], (D_MODEL,), 0.02),
    }


def reference(x, norm1_g, w_in, lam_q1, lam_k1, lam_q2, lam_k2, subln_g, w_out, rel_bias,
              norm2_g, w_router, w_gate, w_up, w_down, norm_f_g):
    splits = [DIFF_WIDTH, 2 * DIFF_WIDTH, 3 * DIFF_WIDTH,
              3 * DIFF_WIDTH + DIL_WIDTH, 3 * DIFF_WIDTH + 2 * DIL_WIDTH]
    bias_a = rel_bias[:, :N_DIFF_HEADS]
    bias_b = rel_bias[:, N_DIFF_HEADS:]
    for l in range(DEPTH):
        lambda_init = 0.8 - 0.6 * math.exp(-0.3 * l)
        h = rms_norm(x, norm1_g[l])
        proj = h @ w_in[l]
        qa, ka, va, qd, kd, vd = jnp.split(proj, splits, axis=-1)
        lam = (jnp.exp(jnp.sum(lam_q1[l].astype(jnp.float32) * lam_k1[l].astype(jnp.float32)))
               - jnp.exp(jnp.sum(lam_q2[l].astype(jnp.float32) * lam_k2[l].astype(jnp.float32)))
               + lambda_init)
        oa = diff_attention(qa, ka, va, lam, lambda_init, subln_g[l], bias_a)
        od = dilated_attention(qd, kd, vd, bias_b)
        x = x + jnp.concatenate([oa, od], axis=-1) @ w_out[l]
        x = x + expert_choice_ffn(rms_norm(x, norm2_g[l]), w_router[l], w_gate[l], w_up[l], w_down[l])
    return rms_norm(x, norm_f_g)
```

```python
import numpy as np
import concourse.bass as bass
import concourse.mybir as mybir

F32 = mybir.dt.float32
BF16 = mybir.dt.bfloat16
I32 = mybir.dt.int32
U32 = mybir.dt.uint32
ALU = mybir.AluOpType
ACTF = mybir.ActivationFunctionType
AX = mybir.AxisListType

SEM_ROT = 12000


class Ev:
    __slots__ = ("sem", "val")

    def __init__(self, sem=None, val=None):
        self.sem = sem
        self.val = val


class Buf:
    __slots__ = ("name", "w", "weng", "r", "dsem", "dcnt")

    def __init__(self, name=""):
        self.name = name
        self.w = None
        self.weng = None
        self.r = []
        self.dsem = None
        self.dcnt = 0


def add_read(b, ev, E):
    if ev.sem is not None:
        b.r = [(e2, r2) for (e2, r2) in b.r if not (e2.sem is ev.sem and e2.val <= ev.val)]
    b.r.append((ev, E))


class Eng:
    def __init__(self, fw, raw, name):
        self.fw = fw
        self.raw = raw
        self.name = name
        self.sem = None
        self.count = 0
        self.waited = {}
        self.pending = []
        self.nsem = 0

    def _newsem(self):
        self.sem = self.fw.new_sem(f"{self.name}_{self.nsem}")
        self.nsem += 1
        self.count = 0

    def wait(self, ev):
        assert ev.sem is not None, "waiting on unresolved (unsignaled) event"
        k = id(ev.sem)
        if self.waited.get(k, 0) >= ev.val:
            return
        self.raw.wait_ge(ev.sem, ev.val)
        self.waited[k] = ev.val

    def signal(self, inst):
        if self.sem is None or self.count >= SEM_ROT:
            self._newsem()
        inst.then_inc(self.sem, 1)
        self.count += 1
        for p in self.pending:
            p.sem = self.sem
            p.val = self.count
        self.pending = []
        return Ev(self.sem, self.count)

    def lazy(self):
        e = Ev()
        self.pending.append(e)
        return e


class FW:
    def __init__(self, nc, stack):
        self.nc = nc
        self.stack = stack
        self.sems = []
        self.pe = Eng(self, nc.tensor, "pe")
        self.dve = Eng(self, nc.vector, "dve")
        self.act = Eng(self, nc.scalar, "act")
        self.pool = Eng(self, nc.gpsimd, "pool")
        self.sp = Eng(self, nc.sync, "sp")
        self.out_events = []
        self.dma_latest = {}

    def new_sem(self, name):
        name = f"{name}_{len(self.sems)}"
        s = self.stack.enter_context(self.nc.semaphore(name))
        self.sems.append(s)
        return s

    def op(self, E, fn, reads=(), writes=(), signal=True):
        for b in reads:
            if b.w is not None:
                if not (b.weng is E and E is self.pe):
                    E.wait(b.w)
        for b in writes:
            if b.w is not None and not (b.weng is E and E is self.pe):
                E.wait(b.w)
            for ev, re in b.r:
                if not (re is E and E is self.pe):
                    E.wait(ev)
        inst = fn()
        ev = E.signal(inst) if signal else E.lazy()
        for b in reads:
            add_read(b, ev, E)
        for b in writes:
            b.w = ev
            b.weng = E
            b.r = []
        return ev

    def dma(self, Q, fn, reads=(), writes=(), join=False, is_output=False, owner=None):
        for b in reads:
            if b.w is not None:
                Q.wait(b.w)
        for b in writes:
            if b.w is not None:
                if not (join and b.weng is None and b.dsem is not None and b.w.sem is b.dsem):
                    Q.wait(b.w)
            for ev, re in b.r:
                Q.wait(ev)
        inst = fn()
        d = owner if owner is not None else writes[0]
        if d.dsem is None or d.dcnt >= 16 * 3000:
            d.dsem = self.new_sem("d_" + d.name)
            d.dcnt = 0
        d.dcnt += 16
        inst.then_inc(d.dsem, 16)
        ev = Ev(d.dsem, d.dcnt)
        self.dma_latest[id(d.dsem)] = ev
        for b in reads:
            add_read(b, ev, None)
        for b in writes:
            b.w = ev
            b.weng = None
            b.r = []
        if is_output:
            self.out_events.append(ev)
        return ev

    def finish(self):
        seen = {}
        for ev in self.out_events:
            k = id(ev.sem)
            if k not in seen or seen[k].val < ev.val:
                seen[k] = ev
        for ev in seen.values():
            self.sp.wait(ev)


def fw_barrier(fw):
    evs = []
    for E in (fw.pe, fw.dve, fw.act, fw.pool, fw.sp):
        assert not E.pending, f"{E.name} has unsignaled tail instructions"
        if E.sem is not None and E.count > 0:
            evs.append(Ev(E.sem, E.count))
    for ev in fw.dma_latest.values():
        evs.append(ev)
    for E in (fw.pe, fw.dve, fw.act, fw.pool, fw.sp):
        for ev in evs:
            E.wait(ev)
    fw.dma_latest = {}


import numpy as np, math
from contextlib import ExitStack
import concourse.bass as bass
import concourse.mybir as mybir
from concourse.bass_utils import run_bass_kernel_spmd

S = 4096
D = 1024
NIN = 3072
EPS = 1e-6


def consts_np():
    import ml_dtypes
    ident = np.eye(128, dtype=np.float32).astype(ml_dtypes.bfloat16)
    return {"ident": ident}


def phase_a(nc, fw, NS, x, w_in, g1, ident_d, qaT, kaT, va, qdT, kdT, vd):
    with ExitStack() as es:
        sb = lambda name, shape, dt: es.enter_context(nc.sbuf_tensor("A_" + name, shape, dt))
        ps = lambda name, shape, dt: es.enter_context(nc.psum_tensor("A_" + name, shape, dt))
        win = sb("win", [128, 8, NIN], BF16)
        g1b = sb("g1b", [128, D], F32)
        ident = sb("ident", [128, 128], BF16)
        xb = [sb(f"xb{i}", [128, 4, D], F32) for i in range(2)]
        hb = [sb(f"hb{i}", [128, D], BF16) for i in range(2)]
        junk = sb("junk", [128, D], F32)
        ss = [sb(f"ss{i}", [128, 4], F32) for i in range(2)]
        rstd = [sb(f"rstd{i}", [128, 4], F32) for i in range(2)]
        hT = [sb(f"hT{i}", [128, 8, 512], BF16) for i in range(2)]
        st = [sb(f"st{i}", [128, 16, 512], BF16) for i in range(2)]
        vsa = [sb(f"vsa{i}", [128, 4, 4, 129], BF16) for i in range(2)]
        vsd = [sb(f"vsd{i}", [128, 4, 8, 65], BF16) for i in range(2)]
        tp = [ps(f"tp{i}", [128, 1024], BF16) for i in range(2)]
        mm = [ps(f"mm{i}", [128, 512], F32) for i in range(4)]

        B_win, B_g1, B_id = Buf("win"), Buf("g1"), Buf("ident")
        B_winc = [Buf(f"win{i}") for i in range(6)]
        B_xb = [Buf(f"xb{i}") for i in range(2)]
        B_hb = [Buf(f"hb{i}") for i in range(2)]
        B_junk = Buf("junk")
        B_ss = [Buf(f"ss{i}") for i in range(2)]
        B_rstd = [Buf(f"rstd{i}") for i in range(2)]
        B_hT = [Buf(f"hT{i}") for i in range(2)]
        B_st = [[Buf(f"st{i}_{g}") for g in range(4)] for i in range(2)]
        B_vsa = [Buf(f"vsa{i}") for i in range(2)]
        B_vsd = [Buf(f"vsd{i}") for i in range(2)]
        B_tp = [Buf(f"tp{i}") for i in range(2)]
        B_mm = [Buf(f"mm{i}") for i in range(4)]
        B_dram = Buf("dramA")

        for (c0, c1) in [(0, 512), (512, 1024), (1536, 2048), (2048, 2560), (1024, 1536), (2560, 3072)]:
            fw.dma(fw.pool, lambda: nc.gpsimd.dma_start(
                out=win[:, :, c0:c1], in_=w_in[:, c0:c1].rearrange("(c p) n -> p c n", p=128)),
                writes=[B_winc[c0 // 512]])
        fw.dma(fw.sp, lambda: nc.sync.dma_start(out=g1b[:], in_=g1.partition_broadcast(128)), writes=[B_g1])
        fw.dma(fw.sp, lambda: nc.sync.dma_start(out=ident[:], in_=ident_d), writes=[B_id])
        for i in range(2):
            fw.op(fw.pool, lambda: nc.gpsimd.memset(vsa[i][:], 1.0), writes=[B_vsa[i]])
            fw.op(fw.pool, lambda: nc.gpsimd.memset(vsd[i][:], 1.0), writes=[B_vsd[i]])

        nblk = NS * 8
        def load_x(b):
            s, t0 = divmod(b, 8)
            t0 *= 512
            fw.dma(fw.sp, lambda: nc.sync.dma_start(
                out=xb[b % 2][:], in_=x[s, t0:t0 + 512, :].rearrange("(a p) f -> p a f", p=128)),
                writes=[B_xb[b % 2]])
        load_x(0)
        if nblk > 1:
            load_x(1)
        mmi = [0]
        evi = [0]
        hbc = [0]

        def s1_stats(b):
            X = xb[b % 2]
            i2 = b % 2
            for a in range(4):
                fw.op(fw.dve, lambda: nc.vector.scalar_tensor_tensor(
                    out=junk[:], in0=X[:, a, :], scalar=1.0, in1=X[:, a, :],
                    op0=ALU.mult, op1=ALU.mult, accum_out=ss[i2][:, a:a + 1]),
                    reads=[B_xb[i2]], writes=[B_junk, B_ss[i2]])
            fw.op(fw.dve, lambda: nc.vector.tensor_scalar(
                out=rstd[i2][:], in0=ss[i2][:], scalar1=1.0 / D, scalar2=EPS,
                op0=ALU.mult, op1=ALU.add), reads=[B_ss[i2]], writes=[B_rstd[i2]])
            fw.op(fw.act, lambda: nc.scalar.activation(out=rstd[i2][:], in_=rstd[i2][:], func=ACTF.Sqrt),
                  reads=[B_rstd[i2]], writes=[B_rstd[i2]])
            fw.op(fw.dve, lambda: nc.vector.reciprocal(out=rstd[i2][:], in_=rstd[i2][:]),
                  reads=[B_rstd[i2]], writes=[B_rstd[i2]])

        def s1_sub(b, a):
            X = xb[b % 2]
            i2 = b % 2
            hbi = hbc[0] % 2
            hbc[0] += 1
            fw.op(fw.dve, lambda: nc.vector.scalar_tensor_tensor(
                out=hb[hbi][:], in0=X[:, a, :], scalar=rstd[i2][:, a:a + 1], in1=g1b[:],
                op0=ALU.mult, op1=ALU.mult),
                reads=[B_xb[i2], B_rstd[i2], B_g1], writes=[B_hb[hbi]])
            for half in range(2):
                for c4 in range(4):
                    c = half * 4 + c4
                    fw.op(fw.pe, lambda: nc.tensor.transpose(
                        out=tp[half][:, c4 * 128:(c4 + 1) * 128], in_=hb[hbi][:, c * 128:(c + 1) * 128],
                        identity=ident[:]),
                        reads=[B_hb[hbi], B_id], writes=[B_tp[half]], signal=(c4 == 3))
                if half == 0:
                    fw.op(fw.act, lambda: nc.scalar.copy(
                        out=hT[i2][:, 0:4, a * 128:(a + 1) * 128],
                        in_=tp[0][:, 0:512].rearrange("p (c t) -> p c t", c=4)),
                        reads=[B_tp[0]], writes=[B_hT[i2]])
                else:
                    fw.op(fw.dve, lambda: nc.vector.tensor_copy(
                        out=hT[i2][:, 4:8, a * 128:(a + 1) * 128],
                        in_=tp[1][:, 0:512].rearrange("p (c t) -> p c t", c=4)),
                        reads=[B_tp[1]], writes=[B_hT[i2]])

        fm_cols = [0, 128, 256, 384, 512, 640, 768, 896, 1536, 1664, 1792, 1920, 2048, 2176, 2304, 2432]

        def s2_fm(b, j):
            s, t0 = divmod(b, 8)
            t0 *= 512
            i2 = b % 2
            n0 = fm_cols[j]
            m = mmi[0] % 4
            mmi[0] += 1
            for c in range(8):
                fw.op(fw.pe, lambda: nc.tensor.matmul(
                    out=mm[m][:], lhsT=win[:, c, n0:n0 + 128], rhs=hT[i2][:, c, :],
                    start=(c == 0), stop=(c == 7)),
                    reads=[B_winc[n0 // 512], B_hT[i2]], writes=[B_mm[m]], signal=(c == 7))
            is_q = j < 4 or 8 <= j < 12
            g = j // 4
            if evi[0] % 2 == 0:
                fw.op(fw.act, lambda: nc.scalar.activation(
                    out=st[i2][:, j, :], in_=mm[m][:], func=ACTF.Copy, scale=(0.125 if is_q else 1.0)),
                    reads=[B_mm[m]], writes=[B_st[i2][g]])
            else:
                fw.op(fw.dve, lambda: nc.vector.tensor_scalar(
                    out=st[i2][:, j, :], in0=mm[m][:], scalar1=(0.125 if is_q else 1.0), scalar2=None,
                    op0=ALU.mult), reads=[B_mm[m]], writes=[B_st[i2][g]])
            evi[0] += 1
            if j % 4 == 3:
                dst = [qaT, kaT, qdT, kdT][g]
                fw.dma(fw.sp, lambda: nc.sync.dma_start(
                    out=dst[s, :, :, t0:t0 + 512].rearrange("h p t -> p h t"),
                    in_=st[i2][:, g * 4:(g + 1) * 4, :]),
                    reads=[B_st[i2][g]], writes=[B_dram], join=True, owner=B_st[i2][g])

        def s2_tm(b, a):
            i2 = b % 2
            for vi, n0 in enumerate([1024, 2560]):
                m = mmi[0] % 4
                mmi[0] += 1
                for c in range(8):
                    fw.op(fw.pe, lambda: nc.tensor.matmul(
                        out=mm[m][:], lhsT=hT[i2][:, c, a * 128:(a + 1) * 128], rhs=win[:, c, n0:n0 + 512],
                        start=(c == 0), stop=(c == 7)),
                        reads=[B_winc[n0 // 512], B_hT[i2]], writes=[B_mm[m]], signal=(c == 7))
                if vi == 0:
                    fw.op(fw.act, lambda: nc.scalar.copy(
                        out=vsa[i2][:, a, :, 0:128], in_=mm[m][:].rearrange("p (h d) -> p h d", h=4)),
                        reads=[B_mm[m]], writes=[B_vsa[i2]])
                else:
                    fw.op(fw.dve, lambda: nc.vector.tensor_copy(
                        out=vsd[i2][:, a, :, 0:64], in_=mm[m][:].rearrange("p (h d) -> p h d", h=8)),
                        reads=[B_mm[m]], writes=[B_vsd[i2]])

        def s2_store(b):
            s, t0 = divmod(b, 8)
            t0 *= 512
            i2 = b % 2
            fw.dma(fw.sp, lambda: nc.sync.dma_start(
                out=va[s, t0:t0 + 512, :].rearrange("(a p) f -> p a f", p=128),
                in_=vsa[i2][:].rearrange("p a h d -> p a (h d)")),
                reads=[B_vsa[i2]], writes=[B_dram], join=True, owner=B_vsa[i2])
            fw.dma(fw.sp, lambda: nc.sync.dma_start(
                out=vd[s, t0:t0 + 512, :].rearrange("(a p) f -> p a f", p=128),
                in_=vsd[i2][:].rearrange("p a h d -> p a (h d)")),
                reads=[B_vsd[i2]], writes=[B_dram], join=True, owner=B_vsd[i2])

        s1_stats(0)
        for a in range(4):
            s1_sub(0, a)
        for b in range(nblk):
            nxt = b + 1 < nblk
            if nxt:
                if b + 2 < nblk:
                    pass
                s1_stats(b + 1)
            if b + 1 < nblk:
                pass
            for j in range(16):
                s2_fm(b, j)
                if nxt and j % 4 == 3:
                    s1_sub(b + 1, j // 4)
            for a in range(4):
                s2_tm(b, a)
            s2_store(b)
            if b + 2 < nblk:
                load_x(b + 2)
        return B_dram


LA = 2304
LD = 384
LTOT = LA + 3 * LD
TW = 2176
DILS = (1, 4, 16)


def t5_bucket_np(rel):
    rel = np.asarray(rel, dtype=np.int64)
    n = np.abs(rel)
    nf = np.maximum(n, 1).astype(np.float32)
    large = 8 + (np.log(nf / np.float32(8)) / np.float32(math.log(128.0)) * np.float32(8)).astype(np.int32)
    large = np.minimum(large, 15)
    return np.where(rel > 0, 16, 0) + np.where(n < 8, n, large)


def onehot_np():
    import ml_dtypes
    oh = np.zeros((32, LTOT), dtype=np.float32)
    m = np.arange(2303)
    b = t5_bucket_np(1151 - m)
    oh[b, m] = 1.0
    for p, dil in enumerate(DILS):
        m = np.arange(383)
        off = 191 - m
        ok = np.abs(off) <= 64
        b = t5_bucket_np(off * dil)
        oh[b[ok], LA + p * LD + m[ok]] = 1.0
    J = np.eye(128, dtype=np.float32)[::-1].copy()
    return {"onehot": oh.astype(ml_dtypes.bfloat16), "antiid": J.astype(ml_dtypes.bfloat16)}


def setup_tables(nc, fw, es, rel_bias, onehot_d, antiid_d, a_dram, lamv, subln_g):
    sb = lambda name, shape, dt: es.enter_context(nc.sbuf_tensor("T_" + name, shape, dt))
    TA = sb("TA", [128, 4, TW], BF16)
    TD = sb("TD", [128, 3, 8, 256], BF16)
    cfar = sb("cfar", [128, 24], F32)
    nlam = sb("nlam", [128, 1], F32)
    subg = sb("subg", [128, 128], F32)
    B = {k: Buf(k) for k in ["TA", "TD", "cfar", "nlam", "subg"]}
    with ExitStack() as es2:
        sb2 = lambda name, shape, dt: es2.enter_context(nc.sbuf_tensor("T2_" + name, shape, dt))
        ps2 = lambda name, shape, dt: es2.enter_context(nc.psum_tensor("T2_" + name, shape, dt))
        rb = sb2("rb", [32, 12], F32)
        eb = sb2("eb", [32, 12], BF16)
        oh = sb2("oh", [32, LTOT], BF16)
        J = sb2("J", [128, 128], BF16)
        Asb = sb2("Asb", [12, LTOT], BF16)
        Hk = sb2("Hk", [128, TW], BF16)
        Hd = sb2("Hd", [128, 3, 8, 256], BF16)
        lv = sb2("lv", [128, 4, 64], F32)
        lj = sb2("lj", [128, 64], F32)
        ls = sb2("ls", [128, 2], F32)
        pA = [ps2(f"pA{i}", [128, 512], F32) for i in range(2)]
        B_rb, B_eb, B_oh, B_J, B_A, B_Hk, B_Hd, B_lv, B_lj, B_ls = [Buf(n) for n in
            ["rb", "eb", "oh", "J", "A", "Hk", "Hd", "lv", "lj", "ls"]]
        B_pA = [Buf("pA0"), Buf("pA1")]
        B_ad = Buf("a_dram")
        fw.dma(fw.sp, lambda: nc.sync.dma_start(out=rb[:], in_=rel_bias), writes=[B_rb])
        fw.dma(fw.sp, lambda: nc.sync.dma_start(out=oh[:], in_=onehot_d), writes=[B_oh])
        fw.dma(fw.sp, lambda: nc.sync.dma_start(out=J[:], in_=antiid_d), writes=[B_J])
        fw.dma(fw.sp, lambda: nc.sync.dma_start(out=cfar[:, 0:12], in_=rel_bias[15:16, :].partition_broadcast(128)),
               writes=[B["cfar"]])
        fw.dma(fw.sp, lambda: nc.sync.dma_start(out=cfar[:, 12:24], in_=rel_bias[31:32, :].partition_broadcast(128)),
               writes=[B["cfar"]], join=True)
        for i in range(4):
            fw.dma(fw.sp, lambda: nc.sync.dma_start(out=lv[:, i, :], in_=lamv[i].partition_broadcast(128)),
                   writes=[B_lv], join=True)
        fw.dma(fw.sp, lambda: nc.sync.dma_start(out=subg[:], in_=subln_g.partition_broadcast(128)), writes=[B["subg"]])
        for i in range(2):
            fw.op(fw.dve, lambda: nc.vector.scalar_tensor_tensor(
                out=lj[:], in0=lv[:, 2 * i, :], scalar=1.0, in1=lv[:, 2 * i + 1, :], op0=ALU.mult, op1=ALU.mult,
                accum_out=ls[:, i:i + 1]), reads=[B_lv], writes=[B_lj, B_ls])
        fw.op(fw.act, lambda: nc.scalar.activation(out=ls[:], in_=ls[:], func=ACTF.Exp), reads=[B_ls], writes=[B_ls])
        fw.op(fw.dve, lambda: nc.vector.tensor_tensor(out=nlam[:], in0=ls[:, 1:2], in1=ls[:, 0:1], op=ALU.subtract),
              reads=[B_ls], writes=[B["nlam"]])
        fw.op(fw.dve, lambda: nc.vector.tensor_scalar(out=nlam[:], in0=nlam[:], scalar1=-0.2, scalar2=None, op0=ALU.add),
              reads=[B["nlam"]], writes=[B["nlam"]])
        fw.op(fw.dve, lambda: nc.vector.tensor_scalar(out=subg[:], in0=subg[:], scalar1=0.8, scalar2=None, op0=ALU.mult),
              reads=[B["subg"]], writes=[B["subg"]])
        fw.op(fw.act, lambda: nc.scalar.activation(out=eb[:], in_=rb[:], func=ACTF.Exp), reads=[B_rb], writes=[B_eb])
        nch = (LTOT + 511) // 512
        for ci in range(nch):
            c0 = ci * 512
            w = min(512, LTOT - c0)
            pi = ci % 2
            fw.op(fw.pe, lambda: nc.tensor.matmul(out=pA[pi][0:12, 0:w], lhsT=eb[:, :], rhs=oh[:, c0:c0 + w],
                                                  start=True, stop=True),
                  reads=[B_eb, B_oh], writes=[B_pA[pi]])
            fw.op(fw.dve, lambda: nc.vector.tensor_copy(out=Asb[:, c0:c0 + w], in_=pA[pi][0:12, 0:w]),
                  reads=[B_pA[pi]], writes=[B_A])
        fw.dma(fw.sp, lambda: nc.sync.dma_start(out=a_dram, in_=Asb[:]), reads=[B_A], writes=[B_ad])
        adt = a_dram.tensor
        for h in range(4):
            src = bass.AP(tensor=adt, offset=h * LTOT, ap=[[1, 128], [1, TW]])
            fw.dma(fw.sp, lambda: nc.sync.dma_start(out=Hk[:], in_=src), reads=[B_ad], writes=[B_Hk])
            for ci in range(5):
                c0 = ci * 512
                w = min(512, TW - c0)
                pi = ci % 2
                fw.op(fw.pe, lambda: nc.tensor.matmul(out=pA[pi][:, 0:w], lhsT=J[:], rhs=Hk[:, c0:c0 + w],
                                                      start=True, stop=True),
                      reads=[B_J, B_Hk], writes=[B_pA[pi]])
                fw.op(fw.dve, lambda: nc.vector.tensor_copy(out=TA[:, h, c0:c0 + w], in_=pA[pi][:, 0:w]),
                      reads=[B_pA[pi]], writes=[B["TA"]])
        for p in range(3):
            for h in range(8):
                src = bass.AP(tensor=adt, offset=(4 + h) * LTOT + LA + p * LD, ap=[[1, 128], [1, 256]])
                fw.dma(fw.sp, lambda: nc.sync.dma_start(out=Hd[:, p, h, :], in_=src), reads=[B_ad], writes=[B_Hd], join=True)
        for p in range(3):
            for h2 in range(4):
                pi = (p * 4 + h2) % 2
                fw.op(fw.pe, lambda: nc.tensor.matmul(
                    out=pA[pi][:, :], lhsT=J[:], rhs=Hd[:, p, 2 * h2:2 * h2 + 2, :].rearrange("p a b -> p (a b)"),
                    start=True, stop=True), reads=[B_J, B_Hd], writes=[B_pA[pi]])
                fw.op(fw.dve, lambda: nc.vector.tensor_copy(
                    out=TD[:, p, 2 * h2:2 * h2 + 2, :].rearrange("p a b -> p (a b)"), in_=pA[pi][:, :]),
                    reads=[B_pA[pi]], writes=[B["TD"]])
    return dict(TA=TA, TD=TD, cfar=cfar, nlam=nlam, subg=subg, B=B)


def phase_b(nc, fw, NS, tabs, qaT, kaT, va, attn):
    TA, cfar, nlam, subg = tabs["TA"], tabs["cfar"], tabs["nlam"], tabs["subg"]
    TB = tabs["B"]
    with ExitStack() as es:
        sb = lambda name, shape, dt: es.enter_context(nc.sbuf_tensor("B_" + name, shape, dt))
        ps = lambda name, shape, dt: es.enter_context(nc.psum_tensor("B_" + name, shape, dt))
        QT = [sb(f"QT{i}", [128, S], BF16) for i in range(2)]
        KT = [sb(f"KT{i}", [128, S], BF16) for i in range(2)]
        V = [sb(f"V{i}", [128, 32, 4 * 129], BF16) for i in range(2)]
        NP = 4
        P = [sb(f"P{i}", [128, 1024], BF16) for i in range(NP)]
        sc = [ps(f"sc{i}", [128, 1024], F32) for i in range(2)]
        acc = [ps(f"acc{i}", [128, 512], F32) for i in range(3)]
        rr = sb("rr", [128, 8], F32)
        accs = sb("accs", [128, 3, 512], F32)
        B_accs = Buf("accs")
        t1 = sb("t1", [128, 128], F32)
        o4 = sb("o4", [128, 4, 128], F32)
        junk = sb("junk", [128, 128], F32)
        ssq = sb("ssq", [128, 4], F32)
        rq = sb("rq", [128, 4], F32)
        ob = [sb(f"ob{i}", [128, 4, 128], BF16) for i in range(2)]
        B_QT = [Buf(f"QT{i}") for i in range(2)]
        B_KT = [Buf(f"KT{i}") for i in range(2)]
        B_V = [Buf(f"V{i}") for i in range(2)]
        B_P = [Buf(f"P{i}") for i in range(NP)]
        B_sc = [Buf(f"sc{i}") for i in range(2)]
        B_accb = [Buf(f"acc{i}") for i in range(3)]
        B_acc = [B_accb[i // 3] for i in range(8)]
        B_rr, B_t1, B_o4, B_junk, B_ssq, B_rq = [Buf(n) for n in ["rr", "t1", "o4", "junk", "ssq", "rq"]]
        B_ob = [Buf("ob0"), Buf("ob1")]
        B_attn = Buf("attn_a")

        def accap(idx):
            return acc[idx // 3][:, (idx % 3) * 129:(idx % 3 + 1) * 129]

        def load_qk(i):
            s, h = divmod(i, 4)
            fw.dma(fw.sp, lambda: nc.sync.dma_start(out=QT[i % 2][:], in_=qaT[s, h]), writes=[B_QT[i % 2]])
            fw.dma(fw.sp, lambda: nc.sync.dma_start(out=KT[i % 2][:], in_=kaT[s, h]), writes=[B_KT[i % 2]])

        def load_v(s):
            fw.dma(fw.sp, lambda: nc.sync.dma_start(
                out=V[s % 2][:], in_=va[s].rearrange("(a p) f -> p a f", p=128)), writes=[B_V[s % 2]])

        load_v(0)
        load_qk(0)
        pi = 0
        obi = 0
        pending = [None]
        for i in range(NS * 4):
            s, h = divmod(i, 4)
            if i + 1 < NS * 4:
                load_qk(i + 1)
                if (i + 1) % 4 == 0:
                    load_v(s + 1)
            q_, k_, v_ = QT[i % 2], KT[i % 2], V[s % 2]
            bq, bk, bv = B_QT[i % 2], B_KT[i % 2], B_V[s % 2]
            for qb in range(8):
                q0 = qb * 512

                def emit_scores(kt):
                    k0 = kt * 128
                    for w in range(2):
                        lo = w * 64
                        fw.op(fw.pe, lambda: nc.tensor.matmul(
                            out=sc[kt % 2][:, w * 512:(w + 1) * 512], lhsT=k_[lo:lo + 64, k0:k0 + 128],
                            rhs=q_[lo:lo + 64, q0:q0 + 512],
                            start=True, stop=True), reads=[bq, bk], writes=[B_sc[kt % 2]], signal=(w == 1))

                emit_scores(0)
                emit_scores(1)
                for kt in range(32):
                    k0 = kt * 128
                    d = k0 - q0
                    near = -640 <= d <= 1024
                    pb = P[pi % NP]
                    bpb = B_P[pi % NP]
                    pi += 1
                    if near:
                        fw.op(fw.act, lambda: nc.scalar.activation(out=pb[:], in_=sc[kt % 2][:], func=ACTF.Exp),
                              reads=[B_sc[kt % 2]], writes=[bpb])
                        c0 = 1024 - d
                        for w in range(2):
                            fw.op(fw.dve, lambda: nc.vector.tensor_tensor(
                                out=pb[:, w * 512:(w + 1) * 512], in0=pb[:, w * 512:(w + 1) * 512],
                                in1=TA[:, h, c0:c0 + 512], op=ALU.mult),
                                reads=[bpb, TB["TA"]], writes=[bpb])
                    else:
                        col = h + (12 if d > 0 else 0)
                        fw.op(fw.act, lambda: nc.scalar.activation(
                            out=pb[:], in_=sc[kt % 2][:], func=ACTF.Exp, bias=cfar[:, col:col + 1]),
                            reads=[B_sc[kt % 2], TB["cfar"]], writes=[bpb])
                    if kt == 12 and pending[0] is not None:
                        pending[0]()
                        pending[0] = None
                    if kt + 2 < 32:
                        emit_scores(kt + 2)
                    for qs in range(4):
                        for w in range(2):
                            idx = qs * 2 + w
                            last = (qs == 3 and w == 1)
                            fw.op(fw.pe, lambda: nc.tensor.matmul(
                                out=accap(idx), lhsT=pb[:, w * 512 + qs * 128:w * 512 + (qs + 1) * 128],
                                rhs=v_[:, kt, h * 129:(h + 1) * 129],
                                start=(kt == 0 and idx % 3 == 0), stop=(kt == 31), skip_group_check=True),
                                reads=[bpb, bv], writes=[B_acc[idx]], signal=last)
                for bnk in range(3):
                    ncol = 387 if bnk < 2 else 258
                    fw.op(fw.dve, lambda: nc.vector.tensor_copy(out=accs[:, bnk, 0:ncol], in_=acc[bnk][:, 0:ncol]),
                          reads=[B_accb[bnk]], writes=[B_accs])
                for qs in range(4):
                    i1, i2 = qs * 2, qs * 2 + 1
                    a1 = accs[:, i1 // 3, (i1 % 3) * 129:(i1 % 3 + 1) * 129]
                    a2 = accs[:, i2 // 3, (i2 % 3) * 129:(i2 % 3 + 1) * 129]
                    b1, b2 = B_accs, B_accs
                    fw.op(fw.dve, lambda: nc.vector.reciprocal(out=rr[:, 2 * qs:2 * qs + 1], in_=a1[:, 128:129]),
                          reads=[b1], writes=[B_rr])
                    fw.op(fw.dve, lambda: nc.vector.reciprocal(out=rr[:, 2 * qs + 1:2 * qs + 2], in_=a2[:, 128:129]),
                          reads=[b2], writes=[B_rr])
                    fw.op(fw.dve, lambda: nc.vector.tensor_tensor(
                        out=rr[:, 2 * qs + 1:2 * qs + 2], in0=rr[:, 2 * qs + 1:2 * qs + 2], in1=nlam[:], op=ALU.mult),
                        reads=[B_rr, TB["nlam"]], writes=[B_rr])
                    fw.op(fw.dve, lambda: nc.vector.tensor_scalar(
                        out=t1[:], in0=a1[:, 0:128], scalar1=rr[:, 2 * qs:2 * qs + 1], scalar2=None, op0=ALU.mult),
                        reads=[b1, B_rr], writes=[B_t1])
                    fw.op(fw.dve, lambda: nc.vector.scalar_tensor_tensor(
                        out=o4[:, qs, :], in0=a2[:, 0:128], scalar=rr[:, 2 * qs + 1:2 * qs + 2], in1=t1[:],
                        op0=ALU.mult, op1=ALU.add),
                        reads=[b2, B_rr, B_t1], writes=[B_o4])
                    fw.op(fw.dve, lambda: nc.vector.scalar_tensor_tensor(
                        out=junk[:], in0=o4[:, qs, :], scalar=1.0, in1=o4[:, qs, :], op0=ALU.mult, op1=ALU.mult,
                        accum_out=ssq[:, qs:qs + 1]),
                        reads=[B_o4], writes=[B_junk, B_ssq])
                fw.op(fw.dve, lambda: nc.vector.tensor_scalar(
                    out=rq[:], in0=ssq[:], scalar1=1.0 / 128, scalar2=1e-5, op0=ALU.mult, op1=ALU.add),
                    reads=[B_ssq], writes=[B_rq])

                def fin2(s=s, h=h, q0=q0):
                    nonlocal obi
                    obt = ob[obi % 2]
                    bob = B_ob[obi % 2]
                    obi += 1
                    fw.op(fw.act, lambda: nc.scalar.activation(out=rq[:], in_=rq[:], func=ACTF.Ln),
                          reads=[B_rq], writes=[B_rq])
                    fw.op(fw.act, lambda: nc.scalar.activation(out=rq[:], in_=rq[:], func=ACTF.Exp, scale=-0.5),
                          reads=[B_rq], writes=[B_rq])
                    for qs in range(4):
                        fw.op(fw.dve, lambda: nc.vector.scalar_tensor_tensor(
                            out=obt[:, qs, :], in0=o4[:, qs, :], scalar=rq[:, qs:qs + 1], in1=subg[:],
                            op0=ALU.mult, op1=ALU.mult),
                            reads=[B_o4, B_rq, TB["subg"]], writes=[bob])
                    fw.dma(fw.sp, lambda: nc.sync.dma_start(
                        out=attn[s, q0:q0 + 512, h * 128:(h + 1) * 128].rearrange("(a p) f -> p a f", p=128),
                        in_=obt[:]), reads=[bob], writes=[B_attn], join=True, owner=bob, is_output=True)
                pending[0] = fin2
        if pending[0] is not None:
            pending[0]()
        return B_attn


def phase_c(nc, fw, NS, tabs, qdT, kdT, vd, U, pats=(0, 1, 2)):
    TD = tabs["TD"]
    TB = tabs["B"]
    with ExitStack() as es:
        sb = lambda name, shape, dt: es.enter_context(nc.sbuf_tensor("C_" + name, shape, dt))
        ps = lambda name, shape, dt: es.enter_context(nc.psum_tensor("C_" + name, shape, dt))
        Qn = [sb(f"Qn{i}", [128, S], BF16) for i in range(2)]
        Kn = [sb(f"Kn{i}", [128, S], BF16) for i in range(2)]
        Qp = [sb(f"Qp{i}", [128, S], BF16) for i in range(2)]
        Kp = [sb(f"Kp{i}", [128, 6144], BF16) for i in range(2)]
        Vt = sb("Vt", [128, 48, 520], BF16)
        NPB = 4
        P = [sb(f"P{i}", [128, 512], BF16) for i in range(NPB)]
        ust = [sb(f"ust{i}", [128, 32, 130], F32) for i in range(2)]
        sc = [ps(f"sc{i}", [128, 1024], F32) for i in range(3)]
        acc = [ps(f"acc{i}", [128, 512], F32) for i in range(2)]
        B_Qn = [Buf(f"Qn{i}") for i in range(2)]
        B_Kn = [Buf(f"Kn{i}") for i in range(2)]
        B_Qp = [Buf(f"Qp{i}") for i in range(2)]
        B_Kp = [Buf(f"Kp{i}") for i in range(2)]
        B_Vt = Buf("Vt")
        B_P = [Buf(f"P{i}") for i in range(NPB)]
        B_ust = [Buf(f"ust{i}") for i in range(2)]
        B_sc = [Buf(f"sc{i}") for i in range(3)]
        B_acc = [Buf(f"acc{i}") for i in range(2)]
        B_U = Buf("U")

        it = 0
        blk = 0
        for s in range(NS):
            for p, dil in enumerate(DILS):
                if p not in pats:
                    continue
                mlen = S // dil
                nb = mlen // 128
                ML = mlen + 128
                Vv = Vt[:, 0:dil * (nb + 1), :].rearrange("q (r t) f -> q r t f", r=dil)
                fw.op(fw.dve, lambda: nc.vector.memset(Vv[0:64, :, 0, :], 0.0), writes=[B_Vt])
                fw.op(fw.dve, lambda: nc.vector.memset(Vv[64:128, :, nb, :], 0.0), writes=[B_Vt])
                vt_ = vd.tensor
                base = s * S * 520
                for r in range(dil):
                    if nb > 1:
                        src = bass.AP(tensor=vt_, offset=base + ((128 - 64) * dil + r) * 520,
                                      ap=[[dil * 520, 128], [128 * dil * 520, nb - 1], [1, 520]])
                        fw.dma(fw.sp, lambda: nc.sync.dma_start(out=Vv[:, r, 1:nb, :], in_=src), writes=[B_Vt], join=True)
                    src = bass.AP(tensor=vt_, offset=base + r * 520, ap=[[dil * 520, 64], [1, 520]])
                    fw.dma(fw.sp, lambda: nc.sync.dma_start(out=Vv[64:128, r, 0, :], in_=src), writes=[B_Vt], join=True)
                    src = bass.AP(tensor=vt_, offset=base + ((mlen - 64) * dil + r) * 520, ap=[[dil * 520, 64], [1, 520]])
                    fw.dma(fw.sp, lambda: nc.sync.dma_start(out=Vv[0:64, r, nb, :], in_=src), writes=[B_Vt], join=True)
                def prep(c):
                    nonlocal it
                    i2 = it % 2
                    it += 1
                    fw.dma(fw.sp, lambda: nc.sync.dma_start(out=Qn[i2][:], in_=qdT[s, c]), writes=[B_Qn[i2]])
                    fw.dma(fw.sp, lambda: nc.sync.dma_start(out=Kn[i2][:], in_=kdT[s, c]), writes=[B_Kn[i2]])
                    Kv = Kp[i2][:, 0:dil * ML].rearrange("q (r m) -> q r m", r=dil)
                    fw.op(fw.dve, lambda: nc.vector.memset(Kv[:, :, 0:64], 0.0), writes=[B_Kp[i2]])
                    fw.op(fw.dve, lambda: nc.vector.memset(Kv[:, :, 64 + mlen:ML], 0.0), writes=[B_Kp[i2]])
                    fw.op(fw.dve, lambda: nc.vector.tensor_copy(
                        out=Kv[:, :, 64:64 + mlen], in_=Kn[i2][:].rearrange("q (m r) -> q r m", r=dil)),
                        reads=[B_Kn[i2]], writes=[B_Kp[i2]])
                    if dil > 1:
                        Qv = Qp[i2][:].rearrange("q (r m) -> q r m", r=dil)
                        fw.op(fw.dve, lambda: nc.vector.tensor_copy(
                            out=Qv, in_=Qn[i2][:].rearrange("q (m r) -> q r m", r=dil)),
                            reads=[B_Qn[i2]], writes=[B_Qp[i2]])
                        bq = B_Qp[i2]
                    else:
                        Qv = Qn[i2][:].rearrange("q (r m) -> q r m", r=1)
                        bq = B_Qn[i2]
                    return dict(i2=i2, Kv=Kv, Qv=Qv, bq=bq)

                ctx_next = prep(0)
                for c in range(4):
                    ctx = ctx_next
                    i2, Kv, Qv, bq = ctx["i2"], ctx["Kv"], ctx["Qv"], ctx["bq"]
                    us = ust[i2]
                    blocks = [(r, bi) for r in range(dil) for bi in range(nb)]
                    nblk_c = len(blocks)

                    def emit_sc(bidx):
                        r, bi = blocks[bidx]
                        m0 = bi * 128
                        sci = bidx % 3
                        for hh in range(2):
                            lo = hh * 64
                            fw.op(fw.pe, lambda: nc.tensor.matmul(
                                out=sc[sci][:, hh * 512:hh * 512 + 128],
                                lhsT=Kv[lo:lo + 64, r, m0 + 128:m0 + 256], rhs=Qv[lo:lo + 64, r, m0:m0 + 128],
                                start=True, stop=True), reads=[B_Kp[i2], bq], writes=[B_sc[sci]], signal=False)
                            fw.op(fw.pe, lambda: nc.tensor.matmul(
                                out=sc[sci][:, hh * 512 + 128:hh * 512 + 256],
                                lhsT=Kv[lo:lo + 64, r, m0:m0 + 128], rhs=Qv[lo:lo + 64, r, m0:m0 + 128],
                                start=True, stop=True), reads=[B_Kp[i2], bq], writes=[B_sc[sci]], signal=(hh == 1))

                    pbs = {}

                    def emit_exp(bidx):
                        nonlocal blk
                        sci = bidx % 3
                        pb = P[blk % NPB]
                        bpb = B_P[blk % NPB]
                        blk += 1
                        pbs[bidx] = (pb, bpb)
                        fw.op(fw.act, lambda: nc.scalar.activation(
                            out=pb[:].rearrange("q (h x) -> q h x", h=2),
                            in_=sc[sci][:].rearrange("q (h x) -> q h x", h=2)[:, :, 0:256], func=ACTF.Exp),
                              reads=[B_sc[sci]], writes=[bpb])
                        fw.op(fw.dve, lambda: nc.vector.tensor_tensor(
                            out=pb[:], in0=pb[:], in1=TD[:, p, 2 * c:2 * c + 2, :].rearrange("q a b -> q (a b)"),
                            op=ALU.mult), reads=[bpb, TB["TD"]], writes=[bpb])

                    for j0 in range(min(3, nblk_c)):
                        emit_sc(j0)
                    for j0 in range(min(2, nblk_c)):
                        emit_exp(j0)
                    for bidx in range(nblk_c):
                        r, bi = blocks[bidx]
                        aci = bidx % 2
                        if bidx + 2 < nblk_c:
                            emit_exp(bidx + 2)
                        pb, bpb = pbs.pop(bidx)
                        if bidx == 2 and c + 1 < 4:
                            ctx_next = prep(c + 1)
                        for hh in range(2):
                            h = 2 * c + hh
                            fw.op(fw.pe, lambda: nc.tensor.matmul(
                                out=acc[aci][:, hh * 65:(hh + 1) * 65], lhsT=pb[:, hh * 256:hh * 256 + 128],
                                rhs=Vv[:, r, bi + 1, h * 65:(h + 1) * 65], start=(hh == 0), stop=False,
                                skip_group_check=True),
                                reads=[bpb, B_Vt], writes=[B_acc[aci]], signal=False)
                            fw.op(fw.pe, lambda: nc.tensor.matmul(
                                out=acc[aci][:, hh * 65:(hh + 1) * 65], lhsT=pb[:, hh * 256 + 128:hh * 256 + 256],
                                rhs=Vv[:, r, bi, h * 65:(h + 1) * 65], start=False, stop=True,
                                skip_group_check=True),
                                reads=[bpb, B_Vt], writes=[B_acc[aci]], signal=(hh == 1))
                        if bidx + 3 < nblk_c:
                            emit_sc(bidx + 3)
                        fw.op(fw.act, lambda: nc.scalar.copy(out=us[:, r * nb + bi, 0:65], in_=acc[aci][:, 0:65]),
                              reads=[B_acc[aci]], writes=[B_ust[i2]])
                        fw.op(fw.dve, lambda: nc.vector.tensor_copy(out=us[:, r * nb + bi, 65:130], in_=acc[aci][:, 65:130]),
                              reads=[B_acc[aci]], writes=[B_ust[i2]])
                    for r in range(dil):
                        dst = bass.AP(tensor=U.tensor, offset=((p * NS + s) * S + r) * 520 + c * 130,
                                      ap=[[dil * 520, 128], [128 * dil * 520, nb], [1, 130]])
                        fw.dma(fw.sp, lambda: nc.sync.dma_start(out=dst, in_=us[:, r * nb:(r + 1) * nb, :]),
                               reads=[B_ust[i2]], writes=[B_U], join=True, owner=B_ust[i2], is_output=True)
        return B_U


NE = 16
DFF = 2816
NF = 22
CAP = 512


def phase_d(nc, fw, NS, attn, U, x, w_out, g2, w_router, ident_d, identf_d, x1d, h2d, affT, B_affT, hook=None):
    with ExitStack() as es:
        sb = lambda name, shape, dt: es.enter_context(nc.sbuf_tensor("D_" + name, shape, dt))
        ps = lambda name, shape, dt: es.enter_context(nc.psum_tensor("D_" + name, shape, dt))
        wout = sb("wout", [128, 8, D], BF16)
        wr = sb("wr", [128, 8, NE], F32)
        g2b = sb("g2b", [128, D], F32)
        ident = sb("ident", [128, 128], BF16)
        identf = sb("identf", [128, 128], F32)
        aa = [sb(f"aa{i}", [128, 512], BF16) for i in range(2)]
        uu = [sb(f"uu{i}", [128, 3, 520], F32) for i in range(2)]
        xx = [sb(f"xx{i}", [128, D], F32) for i in range(3)]
        us = sb("us", [128, 520], F32)
        rden = sb("rden", [128, 8], F32)
        ad = sb("ad", [128, 512], BF16)
        aT = [sb(f"aT{i}", [128, 8, 128], BF16) for i in range(2)]
        x1t = [sb(f"x1t{i}", [128, D], F32) for i in range(2)]
        junk = sb("junk", [128, D], F32)
        ss = [sb(f"ss{i}", [128, 1], F32) for i in range(2)]
        rstd = [sb(f"rstd{i}", [128, 1], F32) for i in range(2)]
        h2f = sb("h2f", [128, D], F32)
        h2b = [sb(f"h2b{i}", [128, D], BF16) for i in range(2)]
        h2T = sb("h2T", [128, 8, 128], F32)
        ex = sb("ex", [128, NE], F32)
        se = sb("se", [128, 1], F32)
        aff = sb("aff", [128, NE], F32)
        tp = [ps(f"tp{i}", [128, 1024], BF16) for i in range(2)]
        mm = [ps(f"mm{i}", [128, 512], F32) for i in range(2)]
        tf = [ps(f"tf{i}", [128, 512], F32) for i in range(2)]
        lg = ps("lg", [128, 512], F32)
        at = ps("at", [128, 512], F32)
        names = ["wout", "wr", "g2b", "ident", "identf", "us", "rden", "ad", "junk", "h2f", "h2T",
                 "ex", "se", "aff", "lg", "at"]
        B = {n: Buf(n) for n in names}
        for n in ["aa", "uu", "xx", "x1t", "h2b", "tp", "mm", "tf", "ss", "rstd", "aT"]:
            B[n] = [Buf(n + "0"), Buf(n + "1"), Buf(n + "2")]
        B_x1d, B_h2d = Buf("x1d"), Buf("h2d")

        fw.dma(fw.pool, lambda: nc.gpsimd.dma_start(out=wout[:], in_=w_out.rearrange("(c p) n -> p c n", p=128)),
               writes=[B["wout"]])
        fw.dma(fw.sp, lambda: nc.sync.dma_start(out=wr[:], in_=w_router.rearrange("(c p) n -> p c n", p=128)),
               writes=[B["wr"]])
        fw.dma(fw.sp, lambda: nc.sync.dma_start(out=g2b[:], in_=g2.partition_broadcast(128)), writes=[B["g2b"]])
        fw.dma(fw.sp, lambda: nc.sync.dma_start(out=ident[:], in_=ident_d), writes=[B["ident"]])
        fw.dma(fw.sp, lambda: nc.sync.dma_start(out=identf[:], in_=identf_d), writes=[B["identf"]])
        if hook is not None:
            hook()

        nblk128 = NS * 32

        def idx(i):
            s, tt = divmod(i, 32)
            return s, tt, tt * 128, i % 2

        def load_au(i):
            s, tt, t0, k = idx(i)
            fw.dma(fw.sp, lambda: nc.sync.dma_start(out=aa[k][:], in_=attn[s, t0:t0 + 128, 0:512]), writes=[B["aa"][k]])
            src = bass.AP(tensor=U.tensor, offset=(s * S + t0) * 520, ap=[[520, 128], [NS * S * 520, 3], [1, 520]])
            fw.dma(fw.sp, lambda: nc.sync.dma_start(out=uu[k][:], in_=src), writes=[B["uu"][k]])

        def T0(i):
            s, tt, t0, k = idx(i)
            if i + 1 < nblk128:
                load_au(i + 1)
            fw.op(fw.dve, lambda: nc.vector.tensor_tensor(out=us[:], in0=uu[k][:, 0, :], in1=uu[k][:, 1, :], op=ALU.add),
                  reads=[B["uu"][k]], writes=[B["us"]])
            fw.op(fw.dve, lambda: nc.vector.tensor_tensor(out=us[:], in0=us[:], in1=uu[k][:, 2, :], op=ALU.add),
                  reads=[B["uu"][k], B["us"]], writes=[B["us"]])
            usv = us[:].rearrange("p (h d) -> p h d", h=8)
            fw.op(fw.dve, lambda: nc.vector.reciprocal(out=rden[:], in_=usv[:, :, 64]), reads=[B["us"]], writes=[B["rden"]])
            for h in range(8):
                fw.op(fw.dve, lambda: nc.vector.tensor_scalar(
                    out=ad[:, h * 64:(h + 1) * 64], in0=usv[:, h, 0:64], scalar1=rden[:, h:h + 1], scalar2=None,
                    op0=ALU.mult), reads=[B["us"], B["rden"]], writes=[B["ad"]])

        def T1(i):
            s, tt, t0, k = idx(i)
            for half, (src_t, bsrc) in enumerate([(aa[k], B["aa"][k]), (ad, B["ad"])]):
                for c in range(4):
                    fw.op(fw.pe, lambda: nc.tensor.transpose(
                        out=tp[half][:, c * 128:(c + 1) * 128], in_=src_t[:, c * 128:(c + 1) * 128], identity=ident[:]),
                        reads=[bsrc, B["ident"]], writes=[B["tp"][half]], signal=(c == 3))

        def T2(i):
            s, tt, t0, k = idx(i)
            fw.op(fw.act, lambda: nc.scalar.copy(
                out=aT[k][:, 0:4, :], in_=tp[0][:, 0:512].rearrange("p (c t) -> p c t", c=4)),
                reads=[B["tp"][0]], writes=[B["aT"][k]])
            fw.op(fw.dve, lambda: nc.vector.tensor_copy(
                out=aT[k][:, 4:8, :], in_=tp[1][:, 0:512].rearrange("p (c t) -> p c t", c=4)),
                reads=[B["tp"][1]], writes=[B["aT"][k]])
            fw.dma(fw.sp, lambda: nc.sync.dma_start(out=xx[i % 3][:], in_=x[s, t0:t0 + 128, :]), writes=[B["xx"][i % 3]])

        def T3(i):
            s, tt, t0, k = idx(i)
            for half in range(2):
                for c in range(8):
                    fw.op(fw.pe, lambda: nc.tensor.matmul(
                        out=mm[half][:], lhsT=aT[k][:, c, :], rhs=wout[:, c, half * 512:(half + 1) * 512],
                        start=(c == 0), stop=(c == 7)), reads=[B["aT"][k], B["wout"]], writes=[B["mm"][half]],
                        signal=(c == 7))

        def T4(i):
            s, tt, t0, k = idx(i)
            for half in range(2):
                fw.op(fw.dve, lambda: nc.vector.tensor_tensor(
                    out=x1t[k][:, half * 512:(half + 1) * 512], in0=mm[half][:], in1=xx[i % 3][:, half * 512:(half + 1) * 512],
                    op=ALU.add), reads=[B["mm"][half], B["xx"][i % 3]], writes=[B["x1t"][k]])
            fw.dma(fw.sp, lambda: nc.sync.dma_start(out=x1d[s][t0:t0 + 128, :], in_=x1t[k][:]),
                   reads=[B["x1t"][k]], writes=[B_x1d], join=True, owner=B["x1t"][k])
            fw.op(fw.dve, lambda: nc.vector.scalar_tensor_tensor(
                out=junk[:], in0=x1t[k][:], scalar=1.0, in1=x1t[k][:], op0=ALU.mult, op1=ALU.mult, accum_out=ss[k][:]),
                reads=[B["x1t"][k]], writes=[B["junk"], B["ss"][k]])
            fw.op(fw.dve, lambda: nc.vector.tensor_scalar(
                out=rstd[k][:], in0=ss[k][:], scalar1=1.0 / D, scalar2=EPS, op0=ALU.mult, op1=ALU.add),
                reads=[B["ss"][k]], writes=[B["rstd"][k]])

        def T5(i):
            s, tt, t0, k = idx(i)
            fw.op(fw.act, lambda: nc.scalar.activation(out=rstd[k][:], in_=rstd[k][:], func=ACTF.Ln),
                  reads=[B["rstd"][k]], writes=[B["rstd"][k]])
            fw.op(fw.act, lambda: nc.scalar.activation(out=rstd[k][:], in_=rstd[k][:], func=ACTF.Exp, scale=-0.5),
                  reads=[B["rstd"][k]], writes=[B["rstd"][k]])

        def T6(i):
            s, tt, t0, k = idx(i)
            fw.op(fw.dve, lambda: nc.vector.scalar_tensor_tensor(
                out=h2f[:], in0=x1t[k][:], scalar=rstd[k][:], in1=g2b[:], op0=ALU.mult, op1=ALU.mult),
                reads=[B["x1t"][k], B["rstd"][k], B["g2b"]], writes=[B["h2f"]])

        def T7(i):
            s, tt, t0, k = idx(i)
            fw.op(fw.act, lambda: nc.scalar.copy(out=h2b[k][:], in_=h2f[:]), reads=[B["h2f"]], writes=[B["h2b"][k]])
            fw.dma(fw.sp, lambda: nc.sync.dma_start(out=h2d[s][t0:t0 + 128, :], in_=h2b[k][:]),
                   reads=[B["h2b"][k]], writes=[B_h2d], join=True, owner=B["h2b"][k])
            for c in range(8):
                fw.op(fw.pe, lambda: nc.tensor.transpose(
                    out=tf[c // 4][:, (c % 4) * 128:(c % 4 + 1) * 128], in_=h2f[:, c * 128:(c + 1) * 128],
                    identity=identf[:]), reads=[B["h2f"], B["identf"]], writes=[B["tf"][c // 4]], signal=(c % 4 == 3))

        def T8(i):
            fw.op(fw.act, lambda: nc.scalar.copy(out=h2T[:, 0:4, :], in_=tf[0][:].rearrange("p (c t) -> p c t", c=4)),
                  reads=[B["tf"][0]], writes=[B["h2T"]])
            fw.op(fw.dve, lambda: nc.vector.tensor_copy(out=h2T[:, 4:8, :], in_=tf[1][:].rearrange("p (c t) -> p c t", c=4)),
                  reads=[B["tf"][1]], writes=[B["h2T"]])

        def T9(i):
            for c in range(8):
                fw.op(fw.pe, lambda: nc.tensor.matmul(
                    out=lg[:, 0:NE], lhsT=h2T[:, c, :], rhs=wr[:, c, :], start=(c == 0), stop=(c == 7)),
                    reads=[B["h2T"], B["wr"]], writes=[B["lg"]], signal=(c == 7))

        def T10(i):
            fw.op(fw.act, lambda: nc.scalar.activation(out=ex[:], in_=lg[:, 0:NE], func=ACTF.Exp, accum_out=se[:]),
                  reads=[B["lg"]], writes=[B["ex"], B["se"]])

        def T11(i):
            fw.op(fw.dve, lambda: nc.vector.reciprocal(out=se[:], in_=se[:]), reads=[B["se"]], writes=[B["se"]])
            fw.op(fw.dve, lambda: nc.vector.tensor_scalar(out=aff[:], in0=ex[:], scalar1=se[:], scalar2=None, op0=ALU.mult),
                  reads=[B["ex"], B["se"]], writes=[B["aff"]])

        def T12(i):
            fw.op(fw.pe, lambda: nc.tensor.transpose(out=at[0:NE, 0:128], in_=aff[:, 0:NE], identity=identf[:]),
                  reads=[B["aff"], B["identf"]], writes=[B["at"]])

        def T13(i):
            s, tt, t0, k = idx(i)
            fw.op(fw.act, lambda: nc.scalar.copy(out=affT[s][0:NE, t0:t0 + 128], in_=at[0:NE, 0:128]),
                  reads=[B["at"]], writes=[B_affT[s]])
            if tt == 31 and s >= 1:
                fw.dma(fw.sp, lambda: nc.sync.dma_start(out=affT[0][32 * s:32 * s + NE, :], in_=affT[s][0:NE, :]),
                       reads=[B_affT[s]], writes=[B_affT[0]])

        stages = [T0, T1, T2, T3, T4, T5, T6, T7, T8, T9, T10, T11, T12, T13]
        load_au(0)
        for n in range(nblk128 + len(stages) - 1):
            for kst in reversed(range(len(stages))):
                i = n - kst
                if 0 <= i < nblk128:
                    stages[kst](i)


def phase_e_topk(nc, fw, NS, identf_d, affT, B_affT, idxc, gc, B_idxc, B_gc):
    NPT = 32 * (NS - 1) + NE
    with ExitStack() as es:
        sb = lambda name, shape, dt: es.enter_context(nc.sbuf_tensor("E_" + name, shape, dt))
        ps = lambda name, shape, dt: es.enter_context(nc.psum_tensor("E_" + name, shape, dt))
        identf = sb("identf", [128, 128], F32)
        B_identf = Buf("identf")
        fw.dma(fw.sp, lambda: nc.sync.dma_start(out=identf[:], in_=identf_d), writes=[B_identf])
        work = sb("work", [NPT, S], F32)
        vals = sb("vals", [NPT, CAP], F32)
        idxu = sb("idxu", [NPT, CAP], U32)
        idxf = sb("idxf", [NPT, CAP], F32)
        pt = ps("pt", [128, 512], F32)
        B_work, B_vals, B_idxu, B_idxf, B_pt = [Buf(n) for n in ["work", "vals", "idxu", "idxf", "pt"]]
        stk = affT[0]
        fw.op(fw.dve, lambda: nc.vector.tensor_copy(out=work[:], in_=stk[0:NPT, :]), reads=[B_affT[0]], writes=[B_work])
        for it in range(CAP // 8):
            sl = slice(it * 8, (it + 1) * 8)
            fw.op(fw.dve, lambda: nc.vector.max(out=vals[:, sl], in_=work[:]), reads=[B_work], writes=[B_vals])
            fw.op(fw.dve, lambda: nc.vector.max_index(out=idxu[:, sl], in_max=vals[:, sl], in_values=work[:]),
                  reads=[B_work, B_vals], writes=[B_idxu])
            fw.op(fw.dve, lambda: nc.vector.match_replace(
                out=work[:], in_to_replace=vals[:, sl], in_values=work[:], imm_value=-1.0),
                reads=[B_work, B_vals], writes=[B_work])
        fw.op(fw.dve, lambda: nc.vector.tensor_copy(out=idxf[:], in_=idxu[:]), reads=[B_idxu], writes=[B_idxf])
        for src_t, bsrc, dst_t, bdst in [(idxf, B_idxf, idxc, B_idxc), (vals, B_vals, gc, B_gc)]:
            for j in range(4):
                fw.op(fw.pe, lambda: nc.tensor.transpose(
                    out=pt[:, j * NPT:(j + 1) * NPT], in_=src_t[:, j * 128:(j + 1) * 128], identity=identf[0:NPT, 0:NPT]),
                    reads=[bsrc, B_identf], writes=[B_pt], signal=(j == 3))
            fw.op(fw.dve, lambda: nc.vector.tensor_copy(out=dst_t[:], in_=pt[:, 0:4 * NPT]), reads=[B_pt], writes=[bdst])


def phase_e_experts(nc, fw, NS, h2d, x1d, w_gate, w_up, w_down, ident_d, idxc, gc, B_idxc, B_gc, ring=None):
    NPT = 32 * (NS - 1) + NE
    GRP = [(0, 4), (4, 4), (8, 4), (12, 4), (16, 4), (20, 2)]
    NG = len(GRP)
    RING = 3
    with ExitStack() as es:
        sb = lambda name, shape, dt: es.enter_context(nc.sbuf_tensor("E3_" + name, shape, dt))
        ps = lambda name, shape, dt: es.enter_context(nc.psum_tensor("E3_" + name, shape, dt))
        ident = sb("ident", [128, 128], BF16)
        B_ident = Buf("ident")
        fw.dma(fw.sp, lambda: nc.sync.dma_start(out=ident[:], in_=ident_d), writes=[B_ident])
        if ring is None:
            wgu = [sb(f"wgu{i}", [128, 2, 8, 512], BF16) for i in range(RING)]
        else:
            wgu = ring["wgu"]
        wd = sb("wd", [128, NF, D], BF16)
        xe = [sb(f"xe{s}", [128, 4, D], BF16) for s in range(NS)]
        xeT = [sb(f"xeT{s}", [128, 8, 512], BF16) for s in range(NS)]
        heT = [sb(f"heT{s}", [128, NF, 512], BF16) for s in range(NS)]
        sg = [sb(f"sg{i}", [128, 512], F32) for i in range(2)]
        yeg = [sb(f"yeg{i}", [128, D], F32) for i in range(2)]
        tp = [ps(f"tp{i}", [128, 1024], BF16) for i in range(2)]
        pg = [ps(f"pg{i}", [128, 512], F32) for i in range(2)]
        pu = [ps(f"pu{i}", [128, 512], F32) for i in range(2)]
        py = [ps(f"py{i}", [128, 512], F32) for i in range(2)]
        B_wgu = [Buf(f"wgu{i}") for i in range(RING)] if ring is None else ring["B_wgu"]
        B_wd = Buf("wd")
        B_xe = [Buf(f"xe{s}") for s in range(NS)]
        B_xeT = [Buf(f"xeT{s}") for s in range(NS)]
        B_heT = [Buf(f"heT{s}") for s in range(NS)]
        B_sg = [Buf("sg0"), Buf("sg1")]
        B_yeg = [Buf("yeg0"), Buf("yeg1")]
        B_tp = [Buf("tp0"), Buf("tp1")]
        B_pg = [Buf("pg0"), Buf("pg1")]
        B_pu = [Buf("pu0"), Buf("pu1")]
        B_py = [Buf("py0"), Buf("py1")]
        B_x1d = [Buf(f"x1d{s}") for s in range(NS)]

        def load_group(gi):
            e, g = divmod(gi, NG)
            f0, nf = GRP[g]
            slot = gi % RING
            c0, c1 = f0 * 128, (f0 + nf) * 128
            fw.dma(fw.pool, lambda: nc.gpsimd.dma_start(
                out=wgu[slot][:, 0, :, 0:nf * 128], in_=w_gate[e, :, c0:c1].rearrange("(c p) n -> p c n", p=128)),
                writes=[B_wgu[slot]])
            fw.dma(fw.pool, lambda: nc.gpsimd.dma_start(
                out=wgu[slot][:, 1, :, 0:nf * 128], in_=w_up[e, :, c0:c1].rearrange("(c p) n -> p c n", p=128)),
                writes=[B_wgu[slot]], join=True)

        def load_wd(e):
            fw.dma(fw.pool, lambda: nc.gpsimd.dma_start(out=wd[:], in_=w_down[e].rearrange("(c p) n -> p c n", p=128)),
                   writes=[B_wd])

        def gathers(e):
            for s in range(NS):
                for j in range(4):
                    col = j * NPT + 32 * s + e
                    fw.dma(fw.pool, lambda: nc.gpsimd.indirect_dma_start(
                        out=xe[s][:, j, :], out_offset=None, in_=h2d[s],
                        in_offset=bass.IndirectOffsetOnAxis(ap=idxc[:, col:col + 1], axis=0)),
                        reads=[B_idxc], writes=[B_xe[s]], join=True)

        tpi = [0]

        def transposes(e):
            for s in range(NS):
                for kc in range(8):
                    t = tpi[0] % 2
                    tpi[0] += 1
                    for j in range(4):
                        fw.op(fw.pe, lambda: nc.tensor.transpose(
                            out=tp[t][:, j * 128:(j + 1) * 128], in_=xe[s][:, j, kc * 128:(kc + 1) * 128], identity=ident[:]),
                            reads=[B_xe[s], B_ident], writes=[B_tp[t]], signal=(j == 3))
                    if t == 0:
                        fw.op(fw.act, lambda: nc.scalar.copy(out=xeT[s][:, kc, :], in_=tp[t][:, 0:512]),
                              reads=[B_tp[t]], writes=[B_xeT[s]])
                    else:
                        fw.op(fw.dve, lambda: nc.vector.tensor_copy(out=xeT[s][:, kc, :], in_=tp[t][:, 0:512]),
                              reads=[B_tp[t]], writes=[B_xeT[s]])

        if ring is None:
            for gi in range(RING):
                load_group(gi)
        load_wd(0)
        gathers(0)
        transposes(0)
        fi = 0
        yi = 0
        for e in range(NE):
            if e + 1 < NE:
                gathers(e + 1)
            for g in range(NG):
                gi = e * NG + g
                f0, nf = GRP[g]
                slot = gi % RING
                for ff in range(nf):
                    f = f0 + ff
                    for s in range(NS):
                        t = fi % 2
                        fi += 1
                        for kc in range(8):
                            fw.op(fw.pe, lambda: nc.tensor.matmul(
                                out=pg[t][:], lhsT=wgu[slot][:, 0, kc, ff * 128:(ff + 1) * 128], rhs=xeT[s][:, kc, :],
                                start=(kc == 0), stop=(kc == 7)), reads=[B_wgu[slot], B_xeT[s]], writes=[B_pg[t]],
                                signal=(kc == 7))
                        for kc in range(8):
                            fw.op(fw.pe, lambda: nc.tensor.matmul(
                                out=pu[t][:], lhsT=wgu[slot][:, 1, kc, ff * 128:(ff + 1) * 128], rhs=xeT[s][:, kc, :],
                                start=(kc == 0), stop=(kc == 7)), reads=[B_wgu[slot], B_xeT[s]], writes=[B_pu[t]],
                                signal=(kc == 7))
                        fw.op(fw.act, lambda: nc.scalar.activation(out=sg[t][:], in_=pg[t][:], func=ACTF.Silu),
                              reads=[B_pg[t]], writes=[B_sg[t]])
                        fw.op(fw.dve, lambda: nc.vector.tensor_tensor(
                            out=heT[s][:, f, :], in0=sg[t][:], in1=pu[t][:], op=ALU.mult),
                            reads=[B_sg[t], B_pu[t]], writes=[B_heT[s]])
                if gi + RING < NE * NG:
                    load_group(gi + RING)
            if e + 1 < NE:
                transposes(e + 1)
            for s in range(NS):
                for j in range(4):
                    col = j * NPT + 32 * s + e
                    y = yeg[yi % 2]
                    by = B_yeg[yi % 2]
                    yi += 1
                    for half in range(2):
                        for f in range(NF):
                            fw.op(fw.pe, lambda: nc.tensor.matmul(
                                out=py[half][:], lhsT=heT[s][:, f, j * 128:(j + 1) * 128],
                                rhs=wd[:, f, half * 512:(half + 1) * 512],
                                start=(f == 0), stop=(f == NF - 1)), reads=[B_heT[s], B_wd], writes=[B_py[half]],
                                signal=(f == NF - 1))
                        if half == 0:
                            fw.op(fw.dve, lambda: nc.vector.tensor_scalar(
                                out=y[:, 0:512], in0=py[0][:], scalar1=gc[:, col:col + 1],
                                scalar2=None, op0=ALU.mult), reads=[B_py[0], B_gc], writes=[by])
                        else:
                            fw.op(fw.act, lambda: nc.scalar.activation(
                                out=y[:, 512:1024], in_=py[1][:], func=ACTF.Copy, scale=gc[:, col:col + 1]),
                                reads=[B_py[1], B_gc], writes=[by])
                    fw.dma(fw.pool, lambda: nc.gpsimd.indirect_dma_start(
                        out=x1d[s], out_offset=bass.IndirectOffsetOnAxis(ap=idxc[:, col:col + 1], axis=0),
                        in_=y[:, :], in_offset=None, compute_op=ALU.add),
                        reads=[by, B_idxc], writes=[B_x1d[s]])
            if e + 1 < NE:
                load_wd(e + 1)


def phase_f(nc, fw, NS, x1d, gf, out):
    with ExitStack() as es:
        sb = lambda name, shape, dt: es.enter_context(nc.sbuf_tensor("F_" + name, shape, dt))
        NB = 4
        gfb = sb("gfb", [128, D], F32)
        xt = [sb(f"xt{i}", [128, D], F32) for i in range(NB)]
        ot = [sb(f"ot{i}", [128, D], F32) for i in range(2)]
        junk = sb("junk", [128, D], F32)
        ss = [sb(f"ss{i}", [128, 1], F32) for i in range(NB)]
        rstd = [sb(f"rstd{i}", [128, 1], F32) for i in range(NB)]
        B_gfb, B_junk = Buf("gfb"), Buf("junk")
        B_ss = [Buf(f"ss{i}") for i in range(NB)]
        B_rstd = [Buf(f"rstd{i}") for i in range(NB)]
        B_xt = [Buf(f"xt{i}") for i in range(NB)]
        B_ot = [Buf("ot0"), Buf("ot1")]
        B_out = Buf("out")
        fw.dma(fw.sp, lambda: nc.sync.dma_start(out=gfb[:], in_=gf.partition_broadcast(128)), writes=[B_gfb])
        nblk128 = NS * 32

        def load(i):
            s, tt = divmod(i, 32)
            fw.dma(fw.sp, lambda: nc.sync.dma_start(out=xt[i % NB][:], in_=x1d[s][tt * 128:(tt + 1) * 128, :]),
                   writes=[B_xt[i % NB]])

        def F0(i):
            k = i % NB
            if i + 2 < nblk128:
                load(i + 2)
            fw.op(fw.dve, lambda: nc.vector.scalar_tensor_tensor(
                out=junk[:], in0=xt[k][:], scalar=1.0, in1=xt[k][:], op0=ALU.mult, op1=ALU.mult, accum_out=ss[k][:]),
                reads=[B_xt[k]], writes=[B_junk, B_ss[k]])
            fw.op(fw.dve, lambda: nc.vector.tensor_scalar(
                out=rstd[k][:], in0=ss[k][:], scalar1=1.0 / D, scalar2=EPS, op0=ALU.mult, op1=ALU.add),
                reads=[B_ss[k]], writes=[B_rstd[k]])

        def F1(i):
            k = i % NB
            fw.op(fw.act, lambda: nc.scalar.activation(out=rstd[k][:], in_=rstd[k][:], func=ACTF.Sqrt),
                  reads=[B_rstd[k]], writes=[B_rstd[k]])

        def F2(i):
            k = i % NB
            s, tt = divmod(i, 32)
            fw.op(fw.dve, lambda: nc.vector.reciprocal(out=rstd[k][:], in_=rstd[k][:]), reads=[B_rstd[k]], writes=[B_rstd[k]])
            fw.op(fw.dve, lambda: nc.vector.scalar_tensor_tensor(
                out=ot[i % 2][:], in0=xt[k][:], scalar=rstd[k][:], in1=gfb[:], op0=ALU.mult, op1=ALU.mult),
                reads=[B_xt[k], B_rstd[k], B_gfb], writes=[B_ot[i % 2]])
            fw.dma(fw.sp, lambda: nc.sync.dma_start(out=out[s, tt * 128:(tt + 1) * 128, :], in_=ot[i % 2][:]),
                   reads=[B_ot[i % 2]], writes=[B_out], join=True, owner=B_ot[i % 2], is_output=True)

        stages = [F0, F1, F2]
        load(0)
        if nblk128 > 1:
            load(1)
        for n in range(nblk128 + len(stages) - 1):
            for kst in reversed(range(len(stages))):
                i = n - kst
                if 0 <= i < nblk128:
                    stages[kst](i)


def build_full(NS, stop_after="F"):
    nc = bass.Bass("TRN2", target_bir_lowering=False)
    EI = "ExternalInput"
    x = nc.dram_tensor("x", [NS, S, D], F32, kind=EI).ap()
    w_in = nc.dram_tensor("w_in", [D, NIN], F32, kind=EI).ap()
    g1 = nc.dram_tensor("norm1_g", [1, D], F32, kind=EI).ap()
    ident_d = nc.dram_tensor("ident", [128, 128], BF16, kind=EI).ap()
    identf_d = nc.dram_tensor("identf", [128, 128], F32, kind=EI).ap()
    rel_bias = nc.dram_tensor("rel_bias", [32, 12], F32, kind=EI).ap()
    onehot_d = nc.dram_tensor("onehot", [32, LTOT], BF16, kind=EI).ap()
    antiid_d = nc.dram_tensor("antiid", [128, 128], BF16, kind=EI).ap()
    lamv = nc.dram_tensor("lamv", [4, 1, 64], F32, kind=EI).ap()
    subln_g = nc.dram_tensor("subln_g", [1, 128], F32, kind=EI).ap()
    w_out = nc.dram_tensor("w_out", [D, D], F32, kind=EI).ap()
    g2 = nc.dram_tensor("norm2_g", [1, D], F32, kind=EI).ap()
    w_router = nc.dram_tensor("w_router", [D, NE], F32, kind=EI).ap()
    w_gate = nc.dram_tensor("w_gate", [NE, D, DFF], F32, kind=EI).ap()
    w_up = nc.dram_tensor("w_up", [NE, D, DFF], F32, kind=EI).ap()
    w_down = nc.dram_tensor("w_down", [NE, DFF, D], F32, kind=EI).ap()
    gf = nc.dram_tensor("norm_f_g", [1, D], F32, kind=EI).ap()
    kind = "Internal"
    qaT = nc.dram_tensor("qaT", [NS, 4, 128, S], BF16, kind=kind).ap()
    kaT = nc.dram_tensor("kaT", [NS, 4, 128, S], BF16, kind=kind).ap()
    qdT = nc.dram_tensor("qdT", [NS, 4, 128, S], BF16, kind=kind).ap()
    kdT = nc.dram_tensor("kdT", [NS, 4, 128, S], BF16, kind=kind).ap()
    va = nc.dram_tensor("va", [NS, S, 4 * 129], BF16, kind=kind).ap()
    vd = nc.dram_tensor("vd", [NS, S, 8 * 65], BF16, kind=kind).ap()
    a_dram = nc.dram_tensor("a_dram", [12, LTOT], BF16, kind=kind).ap()
    attn = nc.dram_tensor("attn", [NS, S, D], BF16, kind=kind).ap()
    U = nc.dram_tensor("U", [3, NS, S, 520], F32, kind=kind).ap()
    dbg = stop_after != "F"
    x1d = [nc.dram_tensor(f"x1d{i}", [S, D], F32, kind=kind).ap() for i in range(NS)]
    h2d = [nc.dram_tensor(f"h2d{i}", [S, D], BF16, kind=kind).ap() for i in range(NS)]
    out = nc.dram_tensor("out", [NS, S, D], F32, kind="ExternalOutput").ap()
    with ExitStack() as stack:
        fw = FW(nc, stack)
        with ExitStack() as es_tab:
            with nc.named_scope("tables"):
                tabs = setup_tables(nc, fw, es_tab, rel_bias, onehot_d, antiid_d, a_dram, lamv, subln_g)
            fw_barrier(fw)
            with nc.named_scope("phA"):
                phase_a(nc, fw, NS, x, w_in, g1, ident_d, qaT, kaT, va, qdT, kdT, vd)
            fw_barrier(fw)
            with nc.named_scope("phB"):
                phase_b(nc, fw, NS, tabs, qaT, kaT, va, attn)
            fw_barrier(fw)
            with nc.named_scope("phC"):
                phase_c(nc, fw, NS, tabs, qdT, kdT, vd, U)
            fw_barrier(fw)
        fw.out_events = []
        with ExitStack() as es_idx:
            RING_N = 3
            GRP0 = [(0, 4), (4, 4), (8, 4)]
            ring = {"wgu": [es_idx.enter_context(nc.sbuf_tensor(f"R_wgu{i}", [128, 2, 8, 512], BF16)) for i in range(RING_N)],
                    "B_wgu": [Buf(f"R_wgu{i}") for i in range(RING_N)]}
            def prefetch_ring():
                if stop_after == "D":
                    return
                for gi, (f0, nf) in enumerate(GRP0):
                    c0, c1 = f0 * 128, (f0 + nf) * 128
                    fw.dma(fw.pool, lambda: nc.gpsimd.dma_start(
                        out=ring["wgu"][gi][:, 0, :, 0:nf * 128], in_=w_gate[0, :, c0:c1].rearrange("(c p) n -> p c n", p=128)),
                        writes=[ring["B_wgu"][gi]])
                    fw.dma(fw.pool, lambda: nc.gpsimd.dma_start(
                        out=ring["wgu"][gi][:, 1, :, 0:nf * 128], in_=w_up[0, :, c0:c1].rearrange("(c p) n -> p c n", p=128)),
                        writes=[ring["B_wgu"][gi]], join=True)
            NPT = 32 * (NS - 1) + NE
            idxc = es_idx.enter_context(nc.sbuf_tensor("idxc", [128, 4 * NPT], I32))
            gc = es_idx.enter_context(nc.sbuf_tensor("gc", [128, 4 * NPT], F32))
            B_idxc = Buf("idxc")
            B_gc = Buf("gc")
            with ExitStack() as es_aff:
                affT = [es_aff.enter_context(nc.sbuf_tensor(f"affT{s}", [NPT if s == 0 else NE, S], F32)) for s in range(NS)]
                B_affT = [Buf(f"affT{s}") for s in range(NS)]
                fw.op(fw.dve, lambda: nc.vector.memset(affT[0][:], 0.0), writes=[B_affT[0]])
                with nc.named_scope("phD"):
                    phase_d(nc, fw, NS, attn, U, x, w_out, g2, w_router, ident_d, identf_d, x1d, h2d, affT, B_affT, hook=prefetch_ring)
                fw_barrier(fw)
                if stop_after != "D":
                    with nc.named_scope("phEtopk"):
                        phase_e_topk(nc, fw, NS, identf_d, affT, B_affT, idxc, gc, B_idxc, B_gc)
                    fw_barrier(fw)
            if stop_after != "D":
                with nc.named_scope("phEexp"):
                    phase_e_experts(nc, fw, NS, h2d, x1d, w_gate, w_up, w_down, ident_d, idxc, gc, B_idxc, B_gc, ring=ring)
                fw_barrier(fw)
        with nc.named_scope("phF"):
            phase_f(nc, fw, NS, x1d, gf, out)
        fw.finish()
    return nc


def kernel(**inputs):
    import ml_dtypes
    NS = 2
    NCORES = 8
    f32 = lambda a: np.ascontiguousarray(np.asarray(a), dtype=np.float32)
    x = f32(inputs["x"])
    common = {
        "w_in": f32(inputs["w_in"])[0], "norm1_g": f32(inputs["norm1_g"]).reshape(1, D),
        "rel_bias": f32(inputs["rel_bias"]),
        "lamv": np.stack([f32(inputs[k]).reshape(1, 64) for k in ["lam_q1", "lam_k1", "lam_q2", "lam_k2"]]),
        "subln_g": f32(inputs["subln_g"]).reshape(1, 128), "w_out": f32(inputs["w_out"])[0],
        "norm2_g": f32(inputs["norm2_g"]).reshape(1, D), "w_router": f32(inputs["w_router"])[0],
        "w_gate": f32(inputs["w_gate"])[0], "w_up": f32(inputs["w_up"])[0], "w_down": f32(inputs["w_down"])[0],
        "norm_f_g": f32(inputs["norm_f_g"]).reshape(1, D),
        "identf": np.eye(128, dtype=np.float32), **consts_np(), **onehot_np(),
    }
    nc = build_full(NS)
    in_maps = [{"x": np.ascontiguousarray(x[NS * c:NS * (c + 1)]), **common} for c in range(NCORES)]
    res = run_bass_kernel_spmd(nc, in_maps, core_ids=list(range(NCORES)))
    out = np.concatenate([np.asarray(r["out"], dtype=np.float32) for r in res.results], axis=0)
    return out.astype(np.float32)
```

```python
import numpy as np
import concourse.bass as bass
import concourse.mybir as mybir

F32 = mybir.dt.float32
BF16 = mybir.dt.bfloat16
I32 = mybir.dt.int32
U32 = mybir.dt.uint32
ALU = mybir.AluOpType
ACTF = mybir.ActivationFunctionType
AX = mybir.AxisListType

SEM_ROT = 12000


class Ev:
    __slots__ = ("sem", "val")

    def __init__(self, sem=None, val=None):
        self.sem = sem
        self.val = val


class Buf:
    __slots__ = ("name", "w", "weng", "r", "dsem", "dcnt")

    def __init__(self, name=""):
        self.name = name
        self.w = None
        self.weng = None
        self.r = []
        self.dsem = None
        self.dcnt = 0


def add_read(b, ev, E):
    if ev.sem is not None:
        b.r = [(e2, r2) for (e2, r2) in b.r if not (e2.sem is ev.sem and e2.val <= ev.val)]
    b.r.append((ev, E))


class Eng:
    def __init__(self, fw, raw, name):
        self.fw = fw
        self.raw = raw
        self.name = name
        self.sem = None
        self.count = 0
        self.waited = {}
        self.pending = []
        self.nsem = 0

    def _newsem(self):
        self.sem = self.fw.new_sem(f"{self.name}_{self.nsem}")
        self.nsem += 1
        self.count = 0

    def wait(self, ev):
        assert ev.sem is not None, "waiting on unresolved (unsignaled) event"
        k = id(ev.sem)
        if self.waited.get(k, 0) >= ev.val:
            return
        self.raw.wait_ge(ev.sem, ev.val)
        self.waited[k] = ev.val

    def signal(self, inst):
        if self.sem is None or self.count >= SEM_ROT:
            self._newsem()
        inst.then_inc(self.sem, 1)
        self.count += 1
        for p in self.pending:
            p.sem = self.sem
            p.val = self.count
        self.pending = []
        return Ev(self.sem, self.count)

    def lazy(self):
        e = Ev()
        self.pending.append(e)
        return e


class FW:
    def __init__(self, nc, stack):
        self.nc = nc
        self.stack = stack
        self.sems = []
        self.pe = Eng(self, nc.tensor, "pe")
        self.dve = Eng(self, nc.vector, "dve")
        self.act = Eng(self, nc.scalar, "act")
        self.pool = Eng(self, nc.gpsimd, "pool")
        self.sp = Eng(self, nc.sync, "sp")
        self.out_events = []
        self.dma_latest = {}

    def new_sem(self, name):
        name = f"{name}_{len(self.sems)}"
        s = self.stack.enter_context(self.nc.semaphore(name))
        self.sems.append(s)
        return s

    def op(self, E, fn, reads=(), writes=(), signal=True):
        for b in reads:
            if b.w is not None:
                if not (b.weng is E and E is self.pe):
                    E.wait(b.w)
        for b in writes:
            if b.w is not None and not (b.weng is E and E is self.pe):
                E.wait(b.w)
            for ev, re in b.r:
                if not (re is E and E is self.pe):
                    E.wait(ev)
        inst = fn()
        ev = E.signal(inst) if signal else E.lazy()
        for b in reads:
            add_read(b, ev, E)
        for b in writes:
            b.w = ev
            b.weng = E
            b.r = []
        return ev

    def dma(self, Q, fn, reads=(), writes=(), join=False, is_output=False, owner=None):
        for b in reads:
            if b.w is not None:
                Q.wait(b.w)
        for b in writes:
            if b.w is not None:
                if not (join and b.weng is None and b.dsem is not None and b.w.sem is b.dsem):
                    Q.wait(b.w)
            for ev, re in b.r:
                Q.wait(ev)
        inst = fn()
        d = owner if owner is not None else writes[0]
        if d.dsem is None or d.dcnt >= 16 * 3000:
            d.dsem = self.new_sem("d_" + d.name)
            d.dcnt = 0
        d.dcnt += 16
        inst.then_inc(d.dsem, 16)
        ev = Ev(d.dsem, d.dcnt)
        self.dma_latest[id(d.dsem)] = ev
        for b in reads:
            add_read(b, ev, None)
        for b in writes:
            b.w = ev
            b.weng = None
            b.r = []
        if is_output:
            self.out_events.append(ev)
        return ev

    def finish(self):
        seen = {}
        for ev in self.out_events:
            k = id(ev.sem)
            if k not in seen or seen[k].val < ev.val:
                seen[k] = ev
        for ev in seen.values():
            self.sp.wait(ev)


def fw_barrier(fw):
    evs = []
    for E in (fw.pe, fw.dve, fw.act, fw.pool, fw.sp):
        assert not E.pending, f"{E.name} has unsignaled tail instructions"
        if E.sem is not None and E.count > 0:
            evs.append(Ev(E.sem, E.count))
    for ev in fw.dma_latest.values():
        evs.append(ev)
    for E in (fw.pe, fw.dve, fw.act, fw.pool, fw.sp):
        for ev in evs:
            E.wait(ev)
    fw.dma_latest = {}


import numpy as np, math
from contextlib import ExitStack
import concourse.bass as bass
import concourse.mybir as mybir
from concourse.bass_utils import run_bass_kernel_spmd

S = 4096
D = 1024
NIN = 3072
EPS = 1e-6


def consts_np():
    import ml_dtypes
    ident = np.eye(128, dtype=np.float32).astype(ml_dtypes.bfloat16)
    return {"ident": ident}


def phase_a(nc, fw, NS, x, w_in, g1, ident_d, qaT, kaT, va, qdT, kdT, vd):
    with ExitStack() as es:
        sb = lambda name, shape, dt: es.enter_context(nc.sbuf_tensor("A_" + name, shape, dt))
        ps = lambda name, shape, dt: es.enter_context(nc.psum_tensor("A_" + name, shape, dt))
        win = sb("win", [128, 8, NIN], BF16)
        g1b = sb("g1b", [128, D], F32)
        ident = sb("ident", [128, 128], BF16)
        xb = [sb(f"xb{i}", [128, 4, D], F32) for i in range(2)]
        hb = [sb(f"hb{i}", [128, D], BF16) for i in range(2)]
        junk = sb("junk", [128, D], F32)
        ss = [sb(f"ss{i}", [128, 4], F32) for i in range(2)]
        rstd = [sb(f"rstd{i}", [128, 4], F32) for i in range(2)]
        hT = [sb(f"hT{i}", [128, 8, 512], BF16) for i in range(2)]
        st = [sb(f"st{i}", [128, 16, 512], BF16) for i in range(2)]
        vsa = [sb(f"vsa{i}", [128, 4, 4, 129], BF16) for i in range(2)]
        vsd = [sb(f"vsd{i}", [128, 4, 8, 65], BF16) for i in range(2)]
        tp = [ps(f"tp{i}", [128, 1024], BF16) for i in range(2)]
        mm = [ps(f"mm{i}", [128, 512], F32) for i in range(4)]

        B_win, B_g1, B_id = Buf("win"), Buf("g1"), Buf("ident")
        B_winc = [Buf(f"win{i}") for i in range(6)]
        B_xb = [Buf(f"xb{i}") for i in range(2)]
        B_hb = [Buf(f"hb{i}") for i in range(2)]
        B_junk = Buf("junk")
        B_ss = [Buf(f"ss{i}") for i in range(2)]
        B_rstd = [Buf(f"rstd{i}") for i in range(2)]
        B_hT = [Buf(f"hT{i}") for i in range(2)]
        B_st = [[Buf(f"st{i}_{g}") for g in range(4)] for i in range(2)]
        B_vsa = [Buf(f"vsa{i}") for i in range(2)]
        B_vsd = [Buf(f"vsd{i}") for i in range(2)]
        B_tp = [Buf(f"tp{i}") for i in range(2)]
        B_mm = [Buf(f"mm{i}") for i in range(4)]
        B_dram = Buf("dramA")

        for (c0, c1) in [(0, 512), (512, 1024), (1536, 2048), (2048, 2560), (1024, 1536), (2560, 3072)]:
            fw.dma(fw.pool, lambda: nc.gpsimd.dma_start(
                out=win[:, :, c0:c1], in_=w_in[:, c0:c1].rearrange("(c p) n -> p c n", p=128)),
                writes=[B_winc[c0 // 512]])
        fw.dma(fw.sp, lambda: nc.sync.dma_start(out=g1b[:], in_=g1.partition_broadcast(128)), writes=[B_g1])
        fw.dma(fw.sp, lambda: nc.sync.dma_start(out=ident[:], in_=ident_d), writes=[B_id])
        for i in range(2):
            fw.op(fw.pool, lambda: nc.gpsimd.memset(vsa[i][:], 1.0), writes=[B_vsa[i]])
            fw.op(fw.pool, lambda: nc.gpsimd.memset(vsd[i][:], 1.0), writes=[B_vsd[i]])

        nblk = NS * 8
        def load_x(b):
            s, t0 = divmod(b, 8)
            t0 *= 512
            fw.dma(fw.sp, lambda: nc.sync.dma_start(
                out=xb[b % 2][:], in_=x[s, t0:t0 + 512, :].rearrange("(a p) f -> p a f", p=128)),
                writes=[B_xb[b % 2]])
        load_x(0)
        if nblk > 1:
            load_x(1)
        mmi = [0]
        evi = [0]
        hbc = [0]

        def s1_stats(b):
            X = xb[b % 2]
            i2 = b % 2
            for a in range(4):
                fw.op(fw.dve, lambda: nc.vector.scalar_tensor_tensor(
                    out=junk[:], in0=X[:, a, :], scalar=1.0, in1=X[:, a, :],
                    op0=ALU.mult, op1=ALU.mult, accum_out=ss[i2][:, a:a + 1]),
                    reads=[B_xb[i2]], writes=[B_junk, B_ss[i2]])
            fw.op(fw.dve, lambda: nc.vector.tensor_scalar(
                out=rstd[i2][:], in0=ss[i2][:], scalar1=1.0 / D, scalar2=EPS,
                op0=ALU.mult, op1=ALU.add), reads=[B_ss[i2]], writes=[B_rstd[i2]])
            fw.op(fw.act, lambda: nc.scalar.activation(out=rstd[i2][:], in_=rstd[i2][:], func=ACTF.Sqrt),
                  reads=[B_rstd[i2]], writes=[B_rstd[i2]])
            fw.op(fw.dve, lambda: nc.vector.reciprocal(out=rstd[i2][:], in_=rstd[i2][:]),
                  reads=[B_rstd[i2]], writes=[B_rstd[i2]])

        def s1_sub(b, a):
            X = xb[b % 2]
            i2 = b % 2
            hbi = hbc[0] % 2
            hbc[0] += 1
            fw.op(fw.dve, lambda: nc.vector.scalar_tensor_tensor(
                out=hb[hbi][:], in0=X[:, a, :], scalar=rstd[i2][:, a:a + 1], in1=g1b[:],
                op0=ALU.mult, op1=ALU.mult),
                reads=[B_xb[i2], B_rstd[i2], B_g1], writes=[B_hb[hbi]])
            for half in range(2):
                for c4 in range(4):
                    c = half * 4 + c4
                    fw.op(fw.pe, lambda: nc.tensor.transpose(
                        out=tp[half][:, c4 * 128:(c4 + 1) * 128], in_=hb[hbi][:, c * 128:(c + 1) * 128],
                        identity=ident[:]),
                        reads=[B_hb[hbi], B_id], writes=[B_tp[half]], signal=(c4 == 3))
                if half == 0:
                    fw.op(fw.act, lambda: nc.scalar.copy(
                        out=hT[i2][:, 0:4, a * 128:(a + 1) * 128],
                        in_=tp[0][:, 0:512].rearrange("p (c t) -> p c t", c=4)),
                        reads=[B_tp[0]], writes=[B_hT[i2]])
                else:
                    fw.op(fw.dve, lambda: nc.vector.tensor_copy(
                        out=hT[i2][:, 4:8, a * 128:(a + 1) * 128],
                        in_=tp[1][:, 0:512].rearrange("p (c t) -> p c t", c=4)),
                        reads=[B_tp[1]], writes=[B_hT[i2]])

        fm_cols = [0, 128, 256, 384, 512, 640, 768, 896, 1536, 1664, 1792, 1920, 2048, 2176, 2304, 2432]

        def s2_fm(b, j):
            s, t0 = divmod(b, 8)
            t0 *= 512
            i2 = b % 2
            n0 = fm_cols[j]
            m = mmi[0] % 4
            mmi[0] += 1
            for c in range(8):
                fw.op(fw.pe, lambda: nc.tensor.matmul(
                    out=mm[m][:], lhsT=win[:, c, n0:n0 + 128], rhs=hT[i2][:, c, :],
                    start=(c == 0), stop=(c == 7)),
                    reads=[B_winc[n0 // 512], B_hT[i2]], writes=[B_mm[m]], signal=(c == 7))
            is_q = j < 4 or 8 <= j < 12
            g = j // 4
            if evi[0] % 2 == 0:
                fw.op(fw.act, lambda: nc.scalar.activation(
                    out=st[i2][:, j, :], in_=mm[m][:], func=ACTF.Copy, scale=(0.125 if is_q else 1.0)),
                    reads=[B_mm[m]], writes=[B_st[i2][g]])
            else:
                fw.op(fw.dve, lambda: nc.vector.tensor_scalar(
                    out=st[i2][:, j, :], in0=mm[m][:], scalar1=(0.125 if is_q else 1.0), scalar2=None,
                    op0=ALU.mult), reads=[B_mm[m]], writes=[B_st[i2][g]])
            evi[0] += 1
            if j % 4 == 3:
                dst = [qaT, kaT, qdT, kdT][g]
                fw.dma(fw.sp, lambda: nc.sync.dma_start(
                    out=dst[s, :, :, t0:t0 + 512].rearrange("h p t -> p h t"),
                    in_=st[i2][:, g * 4:(g + 1) * 4, :]),
                    reads=[B_st[i2][g]], writes=[B_dram], join=True, owner=B_st[i2][g])

        def s2_tm(b, a):
            i2 = b % 2
            for vi, n0 in enumerate([1024, 2560]):
                m = mmi[0] % 4
                mmi[0] += 1
                for c in range(8):
                    fw.op(fw.pe, lambda: nc.tensor.matmul(
                        out=mm[m][:], lhsT=hT[i2][:, c, a * 128:(a + 1) * 128], rhs=win[:, c, n0:n0 + 512],
                        start=(c == 0), stop=(c == 7)),
                        reads=[B_winc[n0 // 512], B_hT[i2]], writes=[B_mm[m]], signal=(c == 7))
                if vi == 0:
                    fw.op(fw.act, lambda: nc.scalar.copy(
                        out=vsa[i2][:, a, :, 0:128], in_=mm[m][:].rearrange("p (h d) -> p h d", h=4)),
                        reads=[B_mm[m]], writes=[B_vsa[i2]])
                else:
                    fw.op(fw.dve, lambda: nc.vector.tensor_copy(
                        out=vsd[i2][:, a, :, 0:64], in_=mm[m][:].rearrange("p (h d) -> p h d", h=8)),
                        reads=[B_mm[m]], writes=[B_vsd[i2]])

        def s2_store(b):
            s, t0 = divmod(b, 8)
            t0 *= 512
            i2 = b % 2
            fw.dma(fw.sp, lambda: nc.sync.dma_start(
                out=va[s, t0:t0 + 512, :].rearrange("(a p) f -> p a f", p=128),
                in_=vsa[i2][:].rearrange("p a h d -> p a (h d)")),
                reads=[B_vsa[i2]], writes=[B_dram], join=True, owner=B_vsa[i2])
            fw.dma(fw.sp, lambda: nc.sync.dma_start(
                out=vd[s, t0:t0 + 512, :].rearrange("(a p) f -> p a f", p=128),
                in_=vsd[i2][:].rearrange("p a h d -> p a (h d)")),
                reads=[B_vsd[i2]], writes=[B_dram], join=True, owner=B_vsd[i2])

        s1_stats(0)
        for a in range(4):
            s1_sub(0, a)
        for b in range(nblk):
            nxt = b + 1 < nblk
            if nxt:
                if b + 2 < nblk:
                    pass
                s1_stats(b + 1)
            if b + 1 < nblk:
                pass
            for j in range(16):
                s2_fm(b, j)
                if nxt and j % 4 == 3:
                    s1_sub(b + 1, j // 4)
            for a in range(4):
                s2_tm(b, a)
            s2_store(b)
            if b + 2 < nblk:
                load_x(b + 2)
        return B_dram


LA = 2304
LD = 384
LTOT = LA + 3 * LD
TW = 2176
DILS = (1, 4, 16)


def t5_bucket_np(rel):
    rel = np.asarray(rel, dtype=np.int64)
    n = np.abs(rel)
    nf = np.maximum(n, 1).astype(np.float32)
    large = 8 + (np.log(nf / np.float32(8)) / np.float32(math.log(128.0)) * np.float32(8)).astype(np.int32)
    large = np.minimum(large, 15)
    return np.where(rel > 0, 16, 0) + np.where(n < 8, n, large)


def onehot_np():
    import ml_dtypes
    oh = np.zeros((32, LTOT), dtype=np.float32)
    m = np.arange(2303)
    b = t5_bucket_np(1151 - m)
    oh[b, m] = 1.0
    for p, dil in enumerate(DILS):
        m = np.arange(383)
        off = 191 - m
        ok = np.abs(off) <= 64
        b = t5_bucket_np(off * dil)
        oh[b[ok], LA + p * LD + m[ok]] = 1.0
    J = np.eye(128, dtype=np.float32)[::-1].copy()
    return {"onehot": oh.astype(ml_dtypes.bfloat16), "antiid": J.astype(ml_dtypes.bfloat16)}


def setup_tables(nc, fw, es, rel_bias, onehot_d, antiid_d, a_dram, lamv, subln_g):
    sb = lambda name, shape, dt: es.enter_context(nc.sbuf_tensor("T_" + name, shape, dt))
    TA = sb("TA", [128, 4, TW], BF16)
    TD = sb("TD", [128, 3, 8, 256], BF16)
    cfar = sb("cfar", [128, 24], F32)
    nlam = sb("nlam", [128, 1], F32)
    subg = sb("subg", [128, 128], F32)
    B = {k: Buf(k) for k in ["TA", "TD", "cfar", "nlam", "subg"]}
    with ExitStack() as es2:
        sb2 = lambda name, shape, dt: es2.enter_context(nc.sbuf_tensor("T2_" + name, shape, dt))
        ps2 = lambda name, shape, dt: es2.enter_context(nc.psum_tensor("T2_" + name, shape, dt))
        rb = sb2("rb", [32, 12], F32)
        eb = sb2("eb", [32, 12], BF16)
        oh = sb2("oh", [32, LTOT], BF16)
        J = sb2("J", [128, 128], BF16)
        Asb = sb2("Asb", [12, LTOT], BF16)
        Hk = sb2("Hk", [128, TW], BF16)
        Hd = sb2("Hd", [128, 3, 8, 256], BF16)
        lv = sb2("lv", [128, 4, 64], F32)
        lj = sb2("lj", [128, 64], F32)
        ls = sb2("ls", [128, 2], F32)
        pA = [ps2(f"pA{i}", [128, 512], F32) for i in range(2)]
        B_rb, B_eb, B_oh, B_J, B_A, B_Hk, B_Hd, B_lv, B_lj, B_ls = [Buf(n) for n in
            ["rb", "eb", "oh", "J", "A", "Hk", "Hd", "lv", "lj", "ls"]]
        B_pA = [Buf("pA0"), Buf("pA1")]
        B_ad = Buf("a_dram")
        fw.dma(fw.sp, lambda: nc.sync.dma_start(out=rb[:], in_=rel_bias), writes=[B_rb])
        fw.dma(fw.sp, lambda: nc.sync.dma_start(out=oh[:], in_=onehot_d), writes=[B_oh])
        fw.dma(fw.sp, lambda: nc.sync.dma_start(out=J[:], in_=antiid_d), writes=[B_J])
        fw.dma(fw.sp, lambda: nc.sync.dma_start(out=cfar[:, 0:12], in_=rel_bias[15:16, :].partition_broadcast(128)),
               writes=[B["cfar"]])
        fw.dma(fw.sp, lambda: nc.sync.dma_start(out=cfar[:, 12:24], in_=rel_bias[31:32, :].partition_broadcast(128)),
               writes=[B["cfar"]], join=True)
        for i in range(4):
            fw.dma(fw.sp, lambda: nc.sync.dma_start(out=lv[:, i, :], in_=lamv[i].partition_broadcast(128)),
                   writes=[B_lv], join=True)
        fw.dma(fw.sp, lambda: nc.sync.dma_start(out=subg[:], in_=subln_g.partition_broadcast(128)), writes=[B["subg"]])
        for i in range(2):
            fw.op(fw.dve, lambda: nc.vector.scalar_tensor_tensor(
                out=lj[:], in0=lv[:, 2 * i, :], scalar=1.0, in1=lv[:, 2 * i + 1, :], op0=ALU.mult, op1=ALU.mult,
                accum_out=ls[:, i:i + 1]), reads=[B_lv], writes=[B_lj, B_ls])
        fw.op(fw.act, lambda: nc.scalar.activation(out=ls[:], in_=ls[:], func=ACTF.Exp), reads=[B_ls], writes=[B_ls])
        fw.op(fw.dve, lambda: nc.vector.tensor_tensor(out=nlam[:], in0=ls[:, 1:2], in1=ls[:, 0:1], op=ALU.subtract),
              reads=[B_ls], writes=[B["nlam"]])
        fw.op(fw.dve, lambda: nc.vector.tensor_scalar(out=nlam[:], in0=nlam[:], scalar1=-0.2, scalar2=None, op0=ALU.add),
              reads=[B["nlam"]], writes=[B["nlam"]])
        fw.op(fw.dve, lambda: nc.vector.tensor_scalar(out=subg[:], in0=subg[:], scalar1=0.8, scalar2=None, op0=ALU.mult),
              reads=[B["subg"]], writes=[B["subg"]])
        fw.op(fw.act, lambda: nc.scalar.activation(out=eb[:], in_=rb[:], func=ACTF.Exp), reads=[B_rb], writes=[B_eb])
        nch = (LTOT + 511) // 512
        for ci in range(nch):
            c0 = ci * 512
            w = min(512, LTOT - c0)
            pi = ci % 2
            fw.op(fw.pe, lambda: nc.tensor.matmul(out=pA[pi][0:12, 0:w], lhsT=eb[:, :], rhs=oh[:, c0:c0 + w],
                                                  start=True, stop=True),
                  reads=[B_eb, B_oh], writes=[B_pA[pi]])
            fw.op(fw.dve, lambda: nc.vector.tensor_copy(out=Asb[:, c0:c0 + w], in_=pA[pi][0:12, 0:w]),
                  reads=[B_pA[pi]], writes=[B_A])
        fw.dma(fw.sp, lambda: nc.sync.dma_start(out=a_dram, in_=Asb[:]), reads=[B_A], writes=[B_ad])
        adt = a_dram.tensor
        for h in range(4):
            src = bass.AP(tensor=adt, offset=h * LTOT, ap=[[1, 128], [1, TW]])
            fw.dma(fw.sp, lambda: nc.sync.dma_start(out=Hk[:], in_=src), reads=[B_ad], writes=[B_Hk])
            for ci in range(5):
                c0 = ci * 512
                w = min(512, TW - c0)
                pi = ci % 2
                fw.op(fw.pe, lambda: nc.tensor.matmul(out=pA[pi][:, 0:w], lhsT=J[:], rhs=Hk[:, c0:c0 + w],
                                                      start=True, stop=True),
                      reads=[B_J, B_Hk], writes=[B_pA[pi]])
                fw.op(fw.dve, lambda: nc.vector.tensor_copy(out=TA[:, h, c0:c0 + w], in_=pA[pi][:, 0:w]),
                      reads=[B_pA[pi]], writes=[B["TA"]])
        for p in range(3):
            for h in range(8):
                src = bass.AP(tensor=adt, offset=(4 + h) * LTOT + LA + p * LD, ap=[[1, 128], [1, 256]])
                fw.dma(fw.sp, lambda: nc.sync.dma_start(out=Hd[:, p, h, :], in_=src), reads=[B_ad], writes=[B_Hd], join=True)
        for p in range(3):
            for h2 in range(4):
                pi = (p * 4 + h2) % 2
                fw.op(fw.pe, lambda: nc.tensor.matmul(
                    out=pA[pi][:, :], lhsT=J[:], rhs=Hd[:, p, 2 * h2:2 * h2 + 2, :].rearrange("p a b -> p (a b)"),
                    start=True, stop=True), reads=[B_J, B_Hd], writes=[B_pA[pi]])
                fw.op(fw.dve, lambda: nc.vector.tensor_copy(
                    out=TD[:, p, 2 * h2:2 * h2 + 2, :].rearrange("p a b -> p (a b)"), in_=pA[pi][:, :]),
                    reads=[B_pA[pi]], writes=[B["TD"]])
    return dict(TA=TA, TD=TD, cfar=cfar, nlam=nlam, subg=subg, B=B)


def phase_b(nc, fw, NS, tabs, qaT, kaT, va, attn):
    TA, cfar, nlam, subg = tabs["TA"], tabs["cfar"], tabs["nlam"], tabs["subg"]
    TB = tabs["B"]
    with ExitStack() as es:
        sb = lambda name, shape, dt: es.enter_context(nc.sbuf_tensor("B_" + name, shape, dt))
        ps = lambda name, shape, dt: es.enter_context(nc.psum_tensor("B_" + name, shape, dt))
        QT = [sb(f"QT{i}", [128, S], BF16) for i in range(2)]
        KT = [sb(f"KT{i}", [128, S], BF16) for i in range(2)]
        V = [sb(f"V{i}", [128, 32, 4 * 129], BF16) for i in range(2)]
        NP = 5
        P = [sb(f"P{i}", [128, 1024], BF16) for i in range(NP)]
        sc = [ps(f"sc{i}", [128, 1024], F32) for i in range(2)]
        acc = [ps(f"acc{i}", [128, 512], F32) for i in range(3)]
        rr = sb("rr", [128, 8], F32)
        accs = sb("accs", [128, 3, 512], F32)
        B_accs = Buf("accs")
        t1 = sb("t1", [128, 128], F32)
        o4 = sb("o4", [128, 4, 128], F32)
        junk = sb("junk", [128, 128], F32)
        ssq = sb("ssq", [128, 4], F32)
        rq = sb("rq", [128, 4], F32)
        ob = [sb(f"ob{i}", [128, 4, 128], BF16) for i in range(2)]
        B_QT = [Buf(f"QT{i}") for i in range(2)]
        B_KT = [Buf(f"KT{i}") for i in range(2)]
        B_V = [Buf(f"V{i}") for i in range(2)]
        B_P = [Buf(f"P{i}") for i in range(NP)]
        B_sc = [Buf(f"sc{i}") for i in range(2)]
        B_accb = [Buf(f"acc{i}") for i in range(3)]
        B_acc = [B_accb[i // 3] for i in range(8)]
        B_rr, B_t1, B_o4, B_junk, B_ssq, B_rq = [Buf(n) for n in ["rr", "t1", "o4", "junk", "ssq", "rq"]]
        B_ob = [Buf("ob0"), Buf("ob1")]
        B_attn = Buf("attn_a")

        def accap(idx):
            return acc[idx // 3][:, (idx % 3) * 129:(idx % 3 + 1) * 129]

        def load_qk(i):
            s, h = divmod(i, 4)
            fw.dma(fw.sp, lambda: nc.sync.dma_start(out=QT[i % 2][:], in_=qaT[s, h]), writes=[B_QT[i % 2]])
            fw.dma(fw.sp, lambda: nc.sync.dma_start(out=KT[i % 2][:], in_=kaT[s, h]), writes=[B_KT[i % 2]])

        def load_v(s):
            fw.dma(fw.sp, lambda: nc.sync.dma_start(
                out=V[s % 2][:], in_=va[s].rearrange("(a p) f -> p a f", p=128)), writes=[B_V[s % 2]])

        load_v(0)
        load_qk(0)
        pi = 0
        obi = 0
        pending = [None]
        for i in range(NS * 4):
            s, h = divmod(i, 4)
            if i + 1 < NS * 4:
                load_qk(i + 1)
                if (i + 1) % 4 == 0:
                    load_v(s + 1)
            q_, k_, v_ = QT[i % 2], KT[i % 2], V[s % 2]
            bq, bk, bv = B_QT[i % 2], B_KT[i % 2], B_V[s % 2]
            for qb in range(8):
                q0 = qb * 512

                def emit_scores(kt):
                    k0 = kt * 128
                    for w in range(2):
                        lo = w * 64
                        fw.op(fw.pe, lambda: nc.tensor.matmul(
                            out=sc[kt % 2][:, w * 512:(w + 1) * 512], lhsT=k_[lo:lo + 64, k0:k0 + 128],
                            rhs=q_[lo:lo + 64, q0:q0 + 512],
                            start=True, stop=True), reads=[bq, bk], writes=[B_sc[kt % 2]], signal=(w == 1))

                emit_scores(0)
                emit_scores(1)
                for kt in range(32):
                    k0 = kt * 128
                    d = k0 - q0
                    near = -640 <= d <= 1024
                    pb = P[pi % NP]
                    bpb = B_P[pi % NP]
                    pi += 1
                    if near:
                        fw.op(fw.act, lambda: nc.scalar.activation(out=pb[:], in_=sc[kt % 2][:], func=ACTF.Exp),
                              reads=[B_sc[kt % 2]], writes=[bpb])
                        c0 = 1024 - d
                        tsl = TA[:, h, c0:c0 + 512]
                        tbc = bass.AP(tensor=tsl.tensor, offset=tsl.offset, ap=[list(tsl.ap[0]), [0, 2], [1, 512]])
                        pv2 = pb[:].rearrange("p (w x) -> p w x", w=2)
                        fw.op(fw.dve, lambda: nc.vector.tensor_tensor(out=pv2, in0=pv2, in1=tbc, op=ALU.mult),
                              reads=[bpb, TB["TA"]], writes=[bpb])
                    else:
                        col = h + (12 if d > 0 else 0)
                        fw.op(fw.act, lambda: nc.scalar.activation(
                            out=pb[:], in_=sc[kt % 2][:], func=ACTF.Exp, bias=cfar[:, col:col + 1]),
                            reads=[B_sc[kt % 2], TB["cfar"]], writes=[bpb])
                    if kt == 12 and pending[0] is not None:
                        pending[0]()
                        pending[0] = None
                    if kt + 2 < 32:
                        emit_scores(kt + 2)
                    for qs in range(4):
                        for w in range(2):
                            idx = qs * 2 + w
                            last = (qs == 3 and w == 1)
                            fw.op(fw.pe, lambda: nc.tensor.matmul(
                                out=accap(idx), lhsT=pb[:, w * 512 + qs * 128:w * 512 + (qs + 1) * 128],
                                rhs=v_[:, kt, h * 129:(h + 1) * 129],
                                start=(kt == 0 and idx % 3 == 0), stop=(kt == 31), skip_group_check=True),
                                reads=[bpb, bv], writes=[B_acc[idx]], signal=last)
                for bnk in range(3):
                    ncol = 387 if bnk < 2 else 258
                    fw.op(fw.dve, lambda: nc.vector.tensor_copy(out=accs[:, bnk, 0:ncol], in_=acc[bnk][:, 0:ncol]),
                          reads=[B_accb[bnk]], writes=[B_accs])
                for qs in range(4):
                    i1, i2 = qs * 2, qs * 2 + 1
                    a1 = accs[:, i1 // 3, (i1 % 3) * 129:(i1 % 3 + 1) * 129]
                    a2 = accs[:, i2 // 3, (i2 % 3) * 129:(i2 % 3 + 1) * 129]
                    b1, b2 = B_accs, B_accs
                    fw.op(fw.dve, lambda: nc.vector.reciprocal(out=rr[:, 2 * qs:2 * qs + 1], in_=a1[:, 128:129]),
                          reads=[b1], writes=[B_rr])
                    fw.op(fw.dve, lambda: nc.vector.reciprocal(out=rr[:, 2 * qs + 1:2 * qs + 2], in_=a2[:, 128:129]),
                          reads=[b2], writes=[B_rr])
                    fw.op(fw.dve, lambda: nc.vector.tensor_tensor(
                        out=rr[:, 2 * qs + 1:2 * qs + 2], in0=rr[:, 2 * qs + 1:2 * qs + 2], in1=nlam[:], op=ALU.mult),
                        reads=[B_rr, TB["nlam"]], writes=[B_rr])
                    fw.op(fw.dve, lambda: nc.vector.tensor_scalar(
                        out=t1[:], in0=a1[:, 0:128], scalar1=rr[:, 2 * qs:2 * qs + 1], scalar2=None, op0=ALU.mult),
                        reads=[b1, B_rr], writes=[B_t1])
                    fw.op(fw.dve, lambda: nc.vector.scalar_tensor_tensor(
                        out=o4[:, qs, :], in0=a2[:, 0:128], scalar=rr[:, 2 * qs + 1:2 * qs + 2], in1=t1[:],
                        op0=ALU.mult, op1=ALU.add),
                        reads=[b2, B_rr, B_t1], writes=[B_o4])
                    fw.op(fw.dve, lambda: nc.vector.scalar_tensor_tensor(
                        out=junk[:], in0=o4[:, qs, :], scalar=1.0, in1=o4[:, qs, :], op0=ALU.mult, op1=ALU.mult,
                        accum_out=ssq[:, qs:qs + 1]),
                        reads=[B_o4], writes=[B_junk, B_ssq])
                fw.op(fw.dve, lambda: nc.vector.tensor_scalar(
                    out=rq[:], in0=ssq[:], scalar1=1.0 / 128, scalar2=1e-5, op0=ALU.mult, op1=ALU.add),
                    reads=[B_ssq], writes=[B_rq])

                def fin2(s=s, h=h, q0=q0):
                    nonlocal obi
                    obt = ob[obi % 2]
                    bob = B_ob[obi % 2]
                    obi += 1
                    fw.op(fw.act, lambda: nc.scalar.activation(out=rq[:], in_=rq[:], func=ACTF.Ln),
                          reads=[B_rq], writes=[B_rq])
                    fw.op(fw.act, lambda: nc.scalar.activation(out=rq[:], in_=rq[:], func=ACTF.Exp, scale=-0.5),
                          reads=[B_rq], writes=[B_rq])
                    for qs in range(4):
                        fw.op(fw.dve, lambda: nc.vector.scalar_tensor_tensor(
                            out=obt[:, qs, :], in0=o4[:, qs, :], scalar=rq[:, qs:qs + 1], in1=subg[:],
                            op0=ALU.mult, op1=ALU.mult),
                            reads=[B_o4, B_rq, TB["subg"]], writes=[bob])
                    fw.dma(fw.sp, lambda: nc.sync.dma_start(
                        out=attn[s, q0:q0 + 512, h * 128:(h + 1) * 128].rearrange("(a p) f -> p a f", p=128),
                        in_=obt[:]), reads=[bob], writes=[B_attn], join=True, owner=bob, is_output=True)
                pending[0] = fin2
        if pending[0] is not None:
            pending[0]()
        return B_attn


def phase_c(nc, fw, NS, tabs, qdT, kdT, vd, U, pats=(0, 1, 2)):
    TD = tabs["TD"]
    TB = tabs["B"]
    with ExitStack() as es:
        sb = lambda name, shape, dt: es.enter_context(nc.sbuf_tensor("C_" + name, shape, dt))
        ps = lambda name, shape, dt: es.enter_context(nc.psum_tensor("C_" + name, shape, dt))
        Qn = [sb(f"Qn{i}", [128, S], BF16) for i in range(2)]
        Kn = [sb(f"Kn{i}", [128, S], BF16) for i in range(2)]
        Qp = [sb(f"Qp{i}", [128, S], BF16) for i in range(2)]
        Kp = [sb(f"Kp{i}", [128, 6144], BF16) for i in range(2)]
        Vt = sb("Vt", [128, 48, 520], BF16)
        NPB = 4
        P = [sb(f"P{i}", [128, 512], BF16) for i in range(NPB)]
        ust = [sb(f"ust{i}", [128, 32, 130], F32) for i in range(2)]
        sc = [ps(f"sc{i}", [128, 1024], F32) for i in range(3)]
        acc = [ps(f"acc{i}", [128, 512], F32) for i in range(2)]
        B_Qn = [Buf(f"Qn{i}") for i in range(2)]
        B_Kn = [Buf(f"Kn{i}") for i in range(2)]
        B_Qp = [Buf(f"Qp{i}") for i in range(2)]
        B_Kp = [Buf(f"Kp{i}") for i in range(2)]
        B_Vt = Buf("Vt")
        B_P = [Buf(f"P{i}") for i in range(NPB)]
        B_ust = [Buf(f"ust{i}") for i in range(2)]
        B_sc = [Buf(f"sc{i}") for i in range(3)]
        B_acc = [Buf(f"acc{i}") for i in range(2)]
        B_U = Buf("U")

        it = 0
        blk = 0
        for s in range(NS):
            for p, dil in enumerate(DILS):
                if p not in pats:
                    continue
                mlen = S // dil
                nb = mlen // 128
                ML = mlen + 128
                Vv = Vt[:, 0:dil * (nb + 1), :].rearrange("q (r t) f -> q r t f", r=dil)
                fw.op(fw.dve, lambda: nc.vector.memset(Vv[0:64, :, 0, :], 0.0), writes=[B_Vt])
                fw.op(fw.dve, lambda: nc.vector.memset(Vv[64:128, :, nb, :], 0.0), writes=[B_Vt])
                vt_ = vd.tensor
                base = s * S * 520
                for r in range(dil):
                    if nb > 1:
                        src = bass.AP(tensor=vt_, offset=base + ((128 - 64) * dil + r) * 520,
                                      ap=[[dil * 520, 128], [128 * dil * 520, nb - 1], [1, 520]])
                        fw.dma(fw.sp, lambda: nc.sync.dma_start(out=Vv[:, r, 1:nb, :], in_=src), writes=[B_Vt], join=True)
                    src = bass.AP(tensor=vt_, offset=base + r * 520, ap=[[dil * 520, 64], [1, 520]])
                    fw.dma(fw.sp, lambda: nc.sync.dma_start(out=Vv[64:128, r, 0, :], in_=src), writes=[B_Vt], join=True)
                    src = bass.AP(tensor=vt_, offset=base + ((mlen - 64) * dil + r) * 520, ap=[[dil * 520, 64], [1, 520]])
                    fw.dma(fw.sp, lambda: nc.sync.dma_start(out=Vv[0:64, r, nb, :], in_=src), writes=[B_Vt], join=True)
                def prep(c):
                    nonlocal it
                    i2 = it % 2
                    it += 1
                    fw.dma(fw.sp, lambda: nc.sync.dma_start(out=Qn[i2][:], in_=qdT[s, c]), writes=[B_Qn[i2]])
                    fw.dma(fw.sp, lambda: nc.sync.dma_start(out=Kn[i2][:], in_=kdT[s, c]), writes=[B_Kn[i2]])
                    Kv = Kp[i2][:, 0:dil * ML].rearrange("q (r m) -> q r m", r=dil)
                    fw.op(fw.dve, lambda: nc.vector.memset(Kv[:, :, 0:64], 0.0), writes=[B_Kp[i2]])
                    fw.op(fw.dve, lambda: nc.vector.memset(Kv[:, :, 64 + mlen:ML], 0.0), writes=[B_Kp[i2]])
                    fw.op(fw.dve, lambda: nc.vector.tensor_copy(
                        out=Kv[:, :, 64:64 + mlen], in_=Kn[i2][:].rearrange("q (m r) -> q r m", r=dil)),
                        reads=[B_Kn[i2]], writes=[B_Kp[i2]])
                    if dil > 1:
                        Qv = Qp[i2][:].rearrange("q (r m) -> q r m", r=dil)
                        fw.op(fw.dve, lambda: nc.vector.tensor_copy(
                            out=Qv, in_=Qn[i2][:].rearrange("q (m r) -> q r m", r=dil)),
                            reads=[B_Qn[i2]], writes=[B_Qp[i2]])
                        bq = B_Qp[i2]
                    else:
                        Qv = Qn[i2][:].rearrange("q (r m) -> q r m", r=1)
                        bq = B_Qn[i2]
                    return dict(i2=i2, Kv=Kv, Qv=Qv, bq=bq)

                ctx_next = prep(0)
                for c in range(4):
                    ctx = ctx_next
                    i2, Kv, Qv, bq = ctx["i2"], ctx["Kv"], ctx["Qv"], ctx["bq"]
                    us = ust[i2]
                    blocks = [(r, bi) for r in range(dil) for bi in range(nb)]
                    nblk_c = len(blocks)

                    def emit_sc(bidx):
                        r, bi = blocks[bidx]
                        m0 = bi * 128
                        sci = bidx % 3
                        for hh in range(2):
                            lo = hh * 64
                            fw.op(fw.pe, lambda: nc.tensor.matmul(
                                out=sc[sci][:, hh * 512:hh * 512 + 128],
                                lhsT=Kv[lo:lo + 64, r, m0 + 128:m0 + 256], rhs=Qv[lo:lo + 64, r, m0:m0 + 128],
                                start=True, stop=True), reads=[B_Kp[i2], bq], writes=[B_sc[sci]], signal=False)
                            fw.op(fw.pe, lambda: nc.tensor.matmul(
                                out=sc[sci][:, hh * 512 + 128:hh * 512 + 256],
                                lhsT=Kv[lo:lo + 64, r, m0:m0 + 128], rhs=Qv[lo:lo + 64, r, m0:m0 + 128],
                                start=True, stop=True), reads=[B_Kp[i2], bq], writes=[B_sc[sci]], signal=(hh == 1))

                    pbs = {}

                    def emit_exp(bidx):
                        nonlocal blk
                        sci = bidx % 3
                        pb = P[blk % NPB]
                        bpb = B_P[blk % NPB]
                        blk += 1
                        pbs[bidx] = (pb, bpb)
                        fw.op(fw.act, lambda: nc.scalar.activation(
                            out=pb[:].rearrange("q (h x) -> q h x", h=2),
                            in_=sc[sci][:].rearrange("q (h x) -> q h x", h=2)[:, :, 0:256], func=ACTF.Exp),
                              reads=[B_sc[sci]], writes=[bpb])
                        fw.op(fw.dve, lambda: nc.vector.tensor_tensor(
                            out=pb[:], in0=pb[:], in1=TD[:, p, 2 * c:2 * c + 2, :].rearrange("q a b -> q (a b)"),
                            op=ALU.mult), reads=[bpb, TB["TD"]], writes=[bpb])

                    for j0 in range(min(3, nblk_c)):
                        emit_sc(j0)
                    for j0 in range(min(2, nblk_c)):
                        emit_exp(j0)
                    for bidx in range(nblk_c):
                        r, bi = blocks[bidx]
                        aci = bidx % 2
                        if bidx + 2 < nblk_c:
                            emit_exp(bidx + 2)
                        pb, bpb = pbs.pop(bidx)
                        if bidx == 2 and c + 1 < 4:
                            ctx_next = prep(c + 1)
                        for hh in range(2):
                            h = 2 * c + hh
                            fw.op(fw.pe, lambda: nc.tensor.matmul(
                                out=acc[aci][:, hh * 65:(hh + 1) * 65], lhsT=pb[:, hh * 256:hh * 256 + 128],
                                rhs=Vv[:, r, bi + 1, h * 65:(h + 1) * 65], start=(hh == 0), stop=False,
                                skip_group_check=True),
                                reads=[bpb, B_Vt], writes=[B_acc[aci]], signal=False)
                            fw.op(fw.pe, lambda: nc.tensor.matmul(
                                out=acc[aci][:, hh * 65:(hh + 1) * 65], lhsT=pb[:, hh * 256 + 128:hh * 256 + 256],
                                rhs=Vv[:, r, bi, h * 65:(h + 1) * 65], start=False, stop=True,
                                skip_group_check=True),
                                reads=[bpb, B_Vt], writes=[B_acc[aci]], signal=(hh == 1))
                        if bidx + 3 < nblk_c:
                            emit_sc(bidx + 3)
                        fw.op(fw.act, lambda: nc.scalar.copy(out=us[:, r * nb + bi, 0:65], in_=acc[aci][:, 0:65]),
                              reads=[B_acc[aci]], writes=[B_ust[i2]])
                        fw.op(fw.dve, lambda: nc.vector.tensor_copy(out=us[:, r * nb + bi, 65:130], in_=acc[aci][:, 65:130]),
                              reads=[B_acc[aci]], writes=[B_ust[i2]])
                    for r in range(dil):
                        dst = bass.AP(tensor=U.tensor, offset=((p * NS + s) * S + r) * 520 + c * 130,
                                      ap=[[dil * 520, 128], [128 * dil * 520, nb], [1, 130]])
                        fw.dma(fw.sp, lambda: nc.sync.dma_start(out=dst, in_=us[:, r * nb:(r + 1) * nb, :]),
                               reads=[B_ust[i2]], writes=[B_U], join=True, owner=B_ust[i2], is_output=True)
        return B_U


NE = 16
DFF = 2816
NF = 22
CAP = 512


def phase_d(nc, fw, NS, attn, U, x, w_out, g2, w_router, ident_d, identf_d, x1d, h2d, affT, B_affT, hook=None):
    with ExitStack() as es:
        sb = lambda name, shape, dt: es.enter_context(nc.sbuf_tensor("D_" + name, shape, dt))
        ps = lambda name, shape, dt: es.enter_context(nc.psum_tensor("D_" + name, shape, dt))
        wout = sb("wout", [128, 8, D], BF16)
        wr = sb("wr", [128, 8, NE], F32)
        g2b = sb("g2b", [128, D], F32)
        ident = sb("ident", [128, 128], BF16)
        identf = sb("identf", [128, 128], F32)
        aa = [sb(f"aa{i}", [128, 512], BF16) for i in range(2)]
        uu = [sb(f"uu{i}", [128, 3, 520], F32) for i in range(2)]
        xx = [sb(f"xx{i}", [128, D], F32) for i in range(3)]
        us = sb("us", [128, 520], F32)
        rden = sb("rden", [128, 8], F32)
        ad = sb("ad", [128, 512], BF16)
        aT = [sb(f"aT{i}", [128, 8, 128], BF16) for i in range(2)]
        x1t = [sb(f"x1t{i}", [128, D], F32) for i in range(2)]
        junk = sb("junk", [128, D], F32)
        ss = [sb(f"ss{i}", [128, 1], F32) for i in range(2)]
        rstd = [sb(f"rstd{i}", [128, 1], F32) for i in range(2)]
        h2f = sb("h2f", [128, D], F32)
        h2b = [sb(f"h2b{i}", [128, D], BF16) for i in range(2)]
        h2T = sb("h2T", [128, 8, 128], F32)
        ex = sb("ex", [128, NE], F32)
        se = sb("se", [128, 1], F32)
        aff = sb("aff", [128, NE], F32)
        tp = [ps(f"tp{i}", [128, 1024], BF16) for i in range(2)]
        mm = [ps(f"mm{i}", [128, 512], F32) for i in range(2)]
        tf = [ps(f"tf{i}", [128, 512], F32) for i in range(2)]
        lg = ps("lg", [128, 512], F32)
        at = ps("at", [128, 512], F32)
        names = ["wout", "wr", "g2b", "ident", "identf", "us", "rden", "ad", "junk", "h2f", "h2T",
                 "ex", "se", "aff", "lg", "at"]
        B = {n: Buf(n) for n in names}
        for n in ["aa", "uu", "xx", "x1t", "h2b", "tp", "mm", "tf", "ss", "rstd", "aT"]:
            B[n] = [Buf(n + "0"), Buf(n + "1"), Buf(n + "2")]
        B_x1d, B_h2d = Buf("x1d"), Buf("h2d")

        fw.dma(fw.pool, lambda: nc.gpsimd.dma_start(out=wout[:], in_=w_out.rearrange("(c p) n -> p c n", p=128)),
               writes=[B["wout"]])
        fw.dma(fw.sp, lambda: nc.sync.dma_start(out=wr[:], in_=w_router.rearrange("(c p) n -> p c n", p=128)),
               writes=[B["wr"]])
        fw.dma(fw.sp, lambda: nc.sync.dma_start(out=g2b[:], in_=g2.partition_broadcast(128)), writes=[B["g2b"]])
        fw.dma(fw.sp, lambda: nc.sync.dma_start(out=ident[:], in_=ident_d), writes=[B["ident"]])
        fw.dma(fw.sp, lambda: nc.sync.dma_start(out=identf[:], in_=identf_d), writes=[B["identf"]])
        if hook is not None:
            hook()

        nblk128 = NS * 32

        def idx(i):
            s, tt = divmod(i, 32)
            return s, tt, tt * 128, i % 2

        def load_au(i):
            s, tt, t0, k = idx(i)
            fw.dma(fw.sp, lambda: nc.sync.dma_start(out=aa[k][:], in_=attn[s, t0:t0 + 128, 0:512]), writes=[B["aa"][k]])
            src = bass.AP(tensor=U.tensor, offset=(s * S + t0) * 520, ap=[[520, 128], [NS * S * 520, 3], [1, 520]])
            fw.dma(fw.sp, lambda: nc.sync.dma_start(out=uu[k][:], in_=src), writes=[B["uu"][k]])

        def T0(i):
            s, tt, t0, k = idx(i)
            if i + 1 < nblk128:
                load_au(i + 1)
            fw.op(fw.dve, lambda: nc.vector.tensor_tensor(out=us[:], in0=uu[k][:, 0, :], in1=uu[k][:, 1, :], op=ALU.add),
                  reads=[B["uu"][k]], writes=[B["us"]])
            fw.op(fw.dve, lambda: nc.vector.tensor_tensor(out=us[:], in0=us[:], in1=uu[k][:, 2, :], op=ALU.add),
                  reads=[B["uu"][k], B["us"]], writes=[B["us"]])
            usv = us[:].rearrange("p (h d) -> p h d", h=8)
            fw.op(fw.dve, lambda: nc.vector.reciprocal(out=rden[:], in_=usv[:, :, 64]), reads=[B["us"]], writes=[B["rden"]])
            for h in range(8):
                fw.op(fw.dve, lambda: nc.vector.tensor_scalar(
                    out=ad[:, h * 64:(h + 1) * 64], in0=usv[:, h, 0:64], scalar1=rden[:, h:h + 1], scalar2=None,
                    op0=ALU.mult), reads=[B["us"], B["rden"]], writes=[B["ad"]])

        def T1(i):
            s, tt, t0, k = idx(i)
            for half, (src_t, bsrc) in enumerate([(aa[k], B["aa"][k]), (ad, B["ad"])]):
                for c in range(4):
                    fw.op(fw.pe, lambda: nc.tensor.transpose(
                        out=tp[half][:, c * 128:(c + 1) * 128], in_=src_t[:, c * 128:(c + 1) * 128], identity=ident[:]),
                        reads=[bsrc, B["ident"]], writes=[B["tp"][half]], signal=(c == 3))

        def T2(i):
            s, tt, t0, k = idx(i)
            fw.op(fw.act, lambda: nc.scalar.copy(
                out=aT[k][:, 0:4, :], in_=tp[0][:, 0:512].rearrange("p (c t) -> p c t", c=4)),
                reads=[B["tp"][0]], writes=[B["aT"][k]])
            fw.op(fw.dve, lambda: nc.vector.tensor_copy(
                out=aT[k][:, 4:8, :], in_=tp[1][:, 0:512].rearrange("p (c t) -> p c t", c=4)),
                reads=[B["tp"][1]], writes=[B["aT"][k]])
            fw.dma(fw.sp, lambda: nc.sync.dma_start(out=xx[i % 3][:], in_=x[s, t0:t0 + 128, :]), writes=[B["xx"][i % 3]])

        def T3(i):
            s, tt, t0, k = idx(i)
            for half in range(2):
                for c in range(8):
                    fw.op(fw.pe, lambda: nc.tensor.matmul(
                        out=mm[half][:], lhsT=aT[k][:, c, :], rhs=wout[:, c, half * 512:(half + 1) * 512],
                        start=(c == 0), stop=(c == 7)), reads=[B["aT"][k], B["wout"]], writes=[B["mm"][half]],
                        signal=(c == 7))

        def T4(i):
            s, tt, t0, k = idx(i)
            for half in range(2):
                fw.op(fw.dve, lambda: nc.vector.tensor_tensor(
                    out=x1t[k][:, half * 512:(half + 1) * 512], in0=mm[half][:], in1=xx[i % 3][:, half * 512:(half + 1) * 512],
                    op=ALU.add), reads=[B["mm"][half], B["xx"][i % 3]], writes=[B["x1t"][k]])
            fw.dma(fw.sp, lambda: nc.sync.dma_start(out=x1d[s][t0:t0 + 128, :], in_=x1t[k][:]),
                   reads=[B["x1t"][k]], writes=[B_x1d], join=True, owner=B["x1t"][k])
            fw.op(fw.dve, lambda: nc.vector.scalar_tensor_tensor(
                out=junk[:], in0=x1t[k][:], scalar=1.0, in1=x1t[k][:], op0=ALU.mult, op1=ALU.mult, accum_out=ss[k][:]),
                reads=[B["x1t"][k]], writes=[B["junk"], B["ss"][k]])
            fw.op(fw.dve, lambda: nc.vector.tensor_scalar(
                out=rstd[k][:], in0=ss[k][:], scalar1=1.0 / D, scalar2=EPS, op0=ALU.mult, op1=ALU.add),
                reads=[B["ss"][k]], writes=[B["rstd"][k]])

        def T5(i):
            s, tt, t0, k = idx(i)
            fw.op(fw.act, lambda: nc.scalar.activation(out=rstd[k][:], in_=rstd[k][:], func=ACTF.Ln),
                  reads=[B["rstd"][k]], writes=[B["rstd"][k]])
            fw.op(fw.act, lambda: nc.scalar.activation(out=rstd[k][:], in_=rstd[k][:], func=ACTF.Exp, scale=-0.5),
                  reads=[B["rstd"][k]], writes=[B["rstd"][k]])

        def T6(i):
            s, tt, t0, k = idx(i)
            fw.op(fw.dve, lambda: nc.vector.scalar_tensor_tensor(
                out=h2f[:], in0=x1t[k][:], scalar=rstd[k][:], in1=g2b[:], op0=ALU.mult, op1=ALU.mult),
                reads=[B["x1t"][k], B["rstd"][k], B["g2b"]], writes=[B["h2f"]])

        def T7(i):
            s, tt, t0, k = idx(i)
            fw.op(fw.act, lambda: nc.scalar.copy(out=h2b[k][:], in_=h2f[:]), reads=[B["h2f"]], writes=[B["h2b"][k]])
            fw.dma(fw.sp, lambda: nc.sync.dma_start(out=h2d[s][t0:t0 + 128, :], in_=h2b[k][:]),
                   reads=[B["h2b"][k]], writes=[B_h2d], join=True, owner=B["h2b"][k])
            for c in range(8):
                fw.op(fw.pe, lambda: nc.tensor.transpose(
                    out=tf[c // 4][:, (c % 4) * 128:(c % 4 + 1) * 128], in_=h2f[:, c * 128:(c + 1) * 128],
                    identity=identf[:]), reads=[B["h2f"], B["identf"]], writes=[B["tf"][c // 4]], signal=(c % 4 == 3))

        def T8(i):
            fw.op(fw.act, lambda: nc.scalar.copy(out=h2T[:, 0:4, :], in_=tf[0][:].rearrange("p (c t) -> p c t", c=4)),
                  reads=[B["tf"][0]], writes=[B["h2T"]])
            fw.op(fw.dve, lambda: nc.vector.tensor_copy(out=h2T[:, 4:8, :], in_=tf[1][:].rearrange("p (c t) -> p c t", c=4)),
                  reads=[B["tf"][1]], writes=[B["h2T"]])

        def T9(i):
            for c in range(8):
                fw.op(fw.pe, lambda: nc.tensor.matmul(
                    out=lg[:, 0:NE], lhsT=h2T[:, c, :], rhs=wr[:, c, :], start=(c == 0), stop=(c == 7)),
                    reads=[B["h2T"], B["wr"]], writes=[B["lg"]], signal=(c == 7))

        def T10(i):
            fw.op(fw.act, lambda: nc.scalar.activation(out=ex[:], in_=lg[:, 0:NE], func=ACTF.Exp, accum_out=se[:]),
                  reads=[B["lg"]], writes=[B["ex"], B["se"]])

        def T11(i):
            fw.op(fw.dve, lambda: nc.vector.reciprocal(out=se[:], in_=se[:]), reads=[B["se"]], writes=[B["se"]])
            fw.op(fw.dve, lambda: nc.vector.tensor_scalar(out=aff[:], in0=ex[:], scalar1=se[:], scalar2=None, op0=ALU.mult),
                  reads=[B["ex"], B["se"]], writes=[B["aff"]])

        def T12(i):
            fw.op(fw.pe, lambda: nc.tensor.transpose(out=at[0:NE, 0:128], in_=aff[:, 0:NE], identity=identf[:]),
                  reads=[B["aff"], B["identf"]], writes=[B["at"]])

        def T13(i):
            s, tt, t0, k = idx(i)
            fw.op(fw.act, lambda: nc.scalar.copy(out=affT[s][0:NE, t0:t0 + 128], in_=at[0:NE, 0:128]),
                  reads=[B["at"]], writes=[B_affT[s]])
            if tt == 31 and s >= 1:
                fw.dma(fw.sp, lambda: nc.sync.dma_start(out=affT[0][32 * s:32 * s + NE, :], in_=affT[s][0:NE, :]),
                       reads=[B_affT[s]], writes=[B_affT[0]])

        stages = [T0, T1, T2, T3, T4, T5, T6, T7, T8, T9, T10, T11, T12, T13]
        load_au(0)
        for n in range(nblk128 + len(stages) - 1):
            for kst in reversed(range(len(stages))):
                i = n - kst
                if 0 <= i < nblk128:
                    stages[kst](i)


def phase_e_topk(nc, fw, NS, identf_d, affT, B_affT, idxc, gc, B_idxc, B_gc):
    NPT = 32 * (NS - 1) + NE
    with ExitStack() as es:
        sb = lambda name, shape, dt: es.enter_context(nc.sbuf_tensor("E_" + name, shape, dt))
        ps = lambda name, shape, dt: es.enter_context(nc.psum_tensor("E_" + name, shape, dt))
        identf = sb("identf", [128, 128], F32)
        B_identf = Buf("identf")
        fw.dma(fw.sp, lambda: nc.sync.dma_start(out=identf[:], in_=identf_d), writes=[B_identf])
        work = sb("work", [NPT, S], F32)
        vals = sb("vals", [NPT, CAP], F32)
        idxu = sb("idxu", [NPT, CAP], U32)
        idxf = sb("idxf", [NPT, CAP], F32)
        pt = ps("pt", [128, 512], F32)
        B_work, B_vals, B_idxu, B_idxf, B_pt = [Buf(n) for n in ["work", "vals", "idxu", "idxf", "pt"]]
        stk = affT[0]
        fw.op(fw.dve, lambda: nc.vector.tensor_copy(out=work[:], in_=stk[0:NPT, :]), reads=[B_affT[0]], writes=[B_work])
        for it in range(CAP // 8):
            sl = slice(it * 8, (it + 1) * 8)
            fw.op(fw.dve, lambda: nc.vector.max(out=vals[:, sl], in_=work[:]), reads=[B_work], writes=[B_vals])
            fw.op(fw.dve, lambda: nc.vector.max_index(out=idxu[:, sl], in_max=vals[:, sl], in_values=work[:]),
                  reads=[B_work, B_vals], writes=[B_idxu])
            fw.op(fw.dve, lambda: nc.vector.match_replace(
                out=work[:], in_to_replace=vals[:, sl], in_values=work[:], imm_value=-1.0),
                reads=[B_work, B_vals], writes=[B_work])
        fw.op(fw.dve, lambda: nc.vector.tensor_copy(out=idxf[:], in_=idxu[:]), reads=[B_idxu], writes=[B_idxf])
        for src_t, bsrc, dst_t, bdst in [(idxf, B_idxf, idxc, B_idxc), (vals, B_vals, gc, B_gc)]:
            for j in range(4):
                fw.op(fw.pe, lambda: nc.tensor.transpose(
                    out=pt[:, j * NPT:(j + 1) * NPT], in_=src_t[:, j * 128:(j + 1) * 128], identity=identf[0:NPT, 0:NPT]),
                    reads=[bsrc, B_identf], writes=[B_pt], signal=(j == 3))
            fw.op(fw.dve, lambda: nc.vector.tensor_copy(out=dst_t[:], in_=pt[:, 0:4 * NPT]), reads=[B_pt], writes=[bdst])


def phase_e_experts(nc, fw, NS, h2d, x1d, w_gate, w_up, w_down, ident_d, idxc, gc, B_idxc, B_gc, ring=None):
    NPT = 32 * (NS - 1) + NE
    GRP = [(0, 4), (4, 4), (8, 4), (12, 4), (16, 4), (20, 2)]
    NG = len(GRP)
    RING = 3
    with ExitStack() as es:
        sb = lambda name, shape, dt: es.enter_context(nc.sbuf_tensor("E3_" + name, shape, dt))
        ps = lambda name, shape, dt: es.enter_context(nc.psum_tensor("E3_" + name, shape, dt))
        ident = sb("ident", [128, 128], BF16)
        B_ident = Buf("ident")
        fw.dma(fw.sp, lambda: nc.sync.dma_start(out=ident[:], in_=ident_d), writes=[B_ident])
        if ring is None:
            wgu = [sb(f"wgu{i}", [128, 2, 8, 512], BF16) for i in range(RING)]
        else:
            wgu = ring["wgu"]
        wd = sb("wd", [128, NF, D], BF16)
        xe = [sb(f"xe{s}", [128, 4, D], BF16) for s in range(NS)]
        xeT = [sb(f"xeT{s}", [128, 8, 512], BF16) for s in range(NS)]
        heT = [sb(f"heT{s}", [128, NF, 512], BF16) for s in range(NS)]
        sg = [sb(f"sg{i}", [128, 512], F32) for i in range(2)]
        yeg = [sb(f"yeg{i}", [128, D], F32) for i in range(2)]
        tp = [ps(f"tp{i}", [128, 1024], BF16) for i in range(2)]
        pg = [ps(f"pg{i}", [128, 512], F32) for i in range(2)]
        pu = [ps(f"pu{i}", [128, 512], F32) for i in range(2)]
        py = [ps(f"py{i}", [128, 512], F32) for i in range(2)]
        B_wgu = [Buf(f"wgu{i}") for i in range(RING)] if ring is None else ring["B_wgu"]
        B_wd = Buf("wd")
        B_xe = [Buf(f"xe{s}") for s in range(NS)]
        B_xeT = [Buf(f"xeT{s}") for s in range(NS)]
        B_heT = [Buf(f"heT{s}") for s in range(NS)]
        B_sg = [Buf("sg0"), Buf("sg1")]
        B_yeg = [Buf("yeg0"), Buf("yeg1")]
        B_tp = [Buf("tp0"), Buf("tp1")]
        B_pg = [Buf("pg0"), Buf("pg1")]
        B_pu = [Buf("pu0"), Buf("pu1")]
        B_py = [Buf("py0"), Buf("py1")]
        B_x1d = [Buf(f"x1d{s}") for s in range(NS)]

        def load_group(gi):
            e, g = divmod(gi, NG)
            f0, nf = GRP[g]
            slot = gi % RING
            c0, c1 = f0 * 128, (f0 + nf) * 128
            fw.dma(fw.pool, lambda: nc.gpsimd.dma_start(
                out=wgu[slot][:, 0, :, 0:nf * 128], in_=w_gate[e, :, c0:c1].rearrange("(c p) n -> p c n", p=128)),
                writes=[B_wgu[slot]])
            fw.dma(fw.pool, lambda: nc.gpsimd.dma_start(
                out=wgu[slot][:, 1, :, 0:nf * 128], in_=w_up[e, :, c0:c1].rearrange("(c p) n -> p c n", p=128)),
                writes=[B_wgu[slot]], join=True)

        def load_wd(e):
            fw.dma(fw.pool, lambda: nc.gpsimd.dma_start(out=wd[:], in_=w_down[e].rearrange("(c p) n -> p c n", p=128)),
                   writes=[B_wd])

        def gathers(e):
            for s in range(NS):
                for j in range(4):
                    col = j * NPT + 32 * s + e
                    fw.dma(fw.pool, lambda: nc.gpsimd.indirect_dma_start(
                        out=xe[s][:, j, :], out_offset=None, in_=h2d[s],
                        in_offset=bass.IndirectOffsetOnAxis(ap=idxc[:, col:col + 1], axis=0)),
                        reads=[B_idxc], writes=[B_xe[s]], join=True)

        tpi = [0]

        def transposes(e):
            for s in range(NS):
                for kc in range(8):
                    t = tpi[0] % 2
                    tpi[0] += 1
                    for j in range(4):
                        fw.op(fw.pe, lambda: nc.tensor.transpose(
                            out=tp[t][:, j * 128:(j + 1) * 128], in_=xe[s][:, j, kc * 128:(kc + 1) * 128], identity=ident[:]),
                            reads=[B_xe[s], B_ident], writes=[B_tp[t]], signal=(j == 3))
                    if t == 0:
                        fw.op(fw.act, lambda: nc.scalar.copy(out=xeT[s][:, kc, :], in_=tp[t][:, 0:512]),
                              reads=[B_tp[t]], writes=[B_xeT[s]])
                    else:
                        fw.op(fw.dve, lambda: nc.vector.tensor_copy(out=xeT[s][:, kc, :], in_=tp[t][:, 0:512]),
                              reads=[B_tp[t]], writes=[B_xeT[s]])

        if ring is None:
            for gi in range(RING):
                load_group(gi)
        load_wd(0)
        gathers(0)
        transposes(0)
        fi = 0
        yi = 0
        for e in range(NE):
            if e + 1 < NE:
                gathers(e + 1)
            for g in range(NG):
                gi = e * NG + g
                f0, nf = GRP[g]
                slot = gi % RING
                for ff in range(nf):
                    f = f0 + ff
                    for s in range(NS):
                        t = fi % 2
                        fi += 1
                        for kc in range(8):
                            fw.op(fw.pe, lambda: nc.tensor.matmul(
                                out=pg[t][:], lhsT=wgu[slot][:, 0, kc, ff * 128:(ff + 1) * 128], rhs=xeT[s][:, kc, :],
                                start=(kc == 0), stop=(kc == 7)), reads=[B_wgu[slot], B_xeT[s]], writes=[B_pg[t]],
                                signal=(kc == 7))
                        for kc in range(8):
                            fw.op(fw.pe, lambda: nc.tensor.matmul(
                                out=pu[t][:], lhsT=wgu[slot][:, 1, kc, ff * 128:(ff + 1) * 128], rhs=xeT[s][:, kc, :],
                                start=(kc == 0), stop=(kc == 7)), reads=[B_wgu[slot], B_xeT[s]], writes=[B_pu[t]],
                                signal=(kc == 7))
                        fw.op(fw.act, lambda: nc.scalar.activation(out=sg[t][:], in_=pg[t][:], func=ACTF.Silu),
                              reads=[B_pg[t]], writes=[B_sg[t]])
                        fw.op(fw.dve, lambda: nc.vector.tensor_tensor(
                            out=heT[s][:, f, :], in0=sg[t][:], in1=pu[t][:], op=ALU.mult),
                            reads=[B_sg[t], B_pu[t]], writes=[B_heT[s]])
                if gi + RING < NE * NG:
                    load_group(gi + RING)
            if e + 1 < NE:
                transposes(e + 1)
            for s in range(NS):
                for j in range(4):
                    col = j * NPT + 32 * s + e
                    y = yeg[yi % 2]
                    by = B_yeg[yi % 2]
                    yi += 1
                    for half in range(2):
                        for f in range(NF):
                            fw.op(fw.pe, lambda: nc.tensor.matmul(
                                out=py[half][:], lhsT=heT[s][:, f, j * 128:(j + 1) * 128],
                                rhs=wd[:, f, half * 512:(half + 1) * 512],
                                start=(f == 0), stop=(f == NF - 1)), reads=[B_heT[s], B_wd], writes=[B_py[half]],
                                signal=(f == NF - 1))
                        if half == 0:
                            fw.op(fw.dve, lambda: nc.vector.tensor_scalar(
                                out=y[:, 0:512], in0=py[0][:], scalar1=gc[:, col:col + 1],
                                scalar2=None, op0=ALU.mult), reads=[B_py[0], B_gc], writes=[by])
                        else:
                            fw.op(fw.act, lambda: nc.scalar.activation(
                                out=y[:, 512:1024], in_=py[1][:], func=ACTF.Copy, scale=gc[:, col:col + 1]),
                                reads=[B_py[1], B_gc], writes=[by])
                    fw.dma(fw.pool, lambda: nc.gpsimd.indirect_dma_start(
                        out=x1d[s], out_offset=bass.IndirectOffsetOnAxis(ap=idxc[:, col:col + 1], axis=0),
                        in_=y[:, :], in_offset=None, compute_op=ALU.add),
                        reads=[by, B_idxc], writes=[B_x1d[s]])
            if e + 1 < NE:
                load_wd(e + 1)


def phase_f(nc, fw, NS, x1d, gf, out):
    with ExitStack() as es:
        sb = lambda name, shape, dt: es.enter_context(nc.sbuf_tensor("F_" + name, shape, dt))
        NB = 4
        gfb = sb("gfb", [128, D], F32)
        xt = [sb(f"xt{i}", [128, D], F32) for i in range(NB)]
        ot = [sb(f"ot{i}", [128, D], F32) for i in range(2)]
        junk = sb("junk", [128, D], F32)
        ss = [sb(f"ss{i}", [128, 1], F32) for i in range(NB)]
        rstd = [sb(f"rstd{i}", [128, 1], F32) for i in range(NB)]
        B_gfb, B_junk = Buf("gfb"), Buf("junk")
        B_ss = [Buf(f"ss{i}") for i in range(NB)]
        B_rstd = [Buf(f"rstd{i}") for i in range(NB)]
        B_xt = [Buf(f"xt{i}") for i in range(NB)]
        B_ot = [Buf("ot0"), Buf("ot1")]
        B_out = Buf("out")
        fw.dma(fw.sp, lambda: nc.sync.dma_start(out=gfb[:], in_=gf.partition_broadcast(128)), writes=[B_gfb])
        nblk128 = NS * 32

        def load(i):
            s, tt = divmod(i, 32)
            fw.dma(fw.sp, lambda: nc.sync.dma_start(out=xt[i % NB][:], in_=x1d[s][tt * 128:(tt + 1) * 128, :]),
                   writes=[B_xt[i % NB]])

        def F0(i):
            k = i % NB
            if i + 2 < nblk128:
                load(i + 2)
            fw.op(fw.dve, lambda: nc.vector.scalar_tensor_tensor(
                out=junk[:], in0=xt[k][:], scalar=1.0, in1=xt[k][:], op0=ALU.mult, op1=ALU.mult, accum_out=ss[k][:]),
                reads=[B_xt[k]], writes=[B_junk, B_ss[k]])
            fw.op(fw.dve, lambda: nc.vector.tensor_scalar(
                out=rstd[k][:], in0=ss[k][:], scalar1=1.0 / D, scalar2=EPS, op0=ALU.mult, op1=ALU.add),
                reads=[B_ss[k]], writes=[B_rstd[k]])

        def F1(i):
            k = i % NB
            fw.op(fw.act, lambda: nc.scalar.activation(out=rstd[k][:], in_=rstd[k][:], func=ACTF.Sqrt),
                  reads=[B_rstd[k]], writes=[B_rstd[k]])

        def F2(i):
            k = i % NB
            s, tt = divmod(i, 32)
            fw.op(fw.dve, lambda: nc.vector.reciprocal(out=rstd[k][:], in_=rstd[k][:]), reads=[B_rstd[k]], writes=[B_rstd[k]])
            fw.op(fw.dve, lambda: nc.vector.scalar_tensor_tensor(
                out=ot[i % 2][:], in0=xt[k][:], scalar=rstd[k][:], in1=gfb[:], op0=ALU.mult, op1=ALU.mult),
                reads=[B_xt[k], B_rstd[k], B_gfb], writes=[B_ot[i % 2]])
            fw.dma(fw.sp, lambda: nc.sync.dma_start(out=out[s, tt * 128:(tt + 1) * 128, :], in_=ot[i % 2][:]),
                   reads=[B_ot[i % 2]], writes=[B_out], join=True, owner=B_ot[i % 2], is_output=True)

        stages = [F0, F1, F2]
        load(0)
        if nblk128 > 1:
            load(1)
        for n in range(nblk128 + len(stages) - 1):
            for kst in reversed(range(len(stages))):
                i = n - kst
                if 0 <= i < nblk128:
                    stages[kst](i)


def build_full(NS, stop_after="F"):
    nc = bass.Bass("TRN2", target_bir_lowering=False)
    EI = "ExternalInput"
    x = nc.dram_tensor("x", [NS, S, D], F32, kind=EI).ap()
    w_in = nc.dram_tensor("w_in", [D, NIN], F32, kind=EI).ap()
    g1 = nc.dram_tensor("norm1_g", [1, D], F32, kind=EI).ap()
    ident_d = nc.dram_tensor("ident", [128, 128], BF16, kind=EI).ap()
    identf_d = nc.dram_tensor("identf", [128, 128], F32, kind=EI).ap()
    rel_bias = nc.dram_tensor("rel_bias", [32, 12], F32, kind=EI).ap()
    onehot_d = nc.dram_tensor("onehot", [32, LTOT], BF16, kind=EI).ap()
    antiid_d = nc.dram_tensor("antiid", [128, 128], BF16, kind=EI).ap()
    lamv = nc.dram_tensor("lamv", [4, 1, 64], F32, kind=EI).ap()
    subln_g = nc.dram_tensor("subln_g", [1, 128], F32, kind=EI).ap()
    w_out = nc.dram_tensor("w_out", [D, D], F32, kind=EI).ap()
    g2 = nc.dram_tensor("norm2_g", [1, D], F32, kind=EI).ap()
    w_router = nc.dram_tensor("w_router", [D, NE], F32, kind=EI).ap()
    w_gate = nc.dram_tensor("w_gate", [NE, D, DFF], F32, kind=EI).ap()
    w_up = nc.dram_tensor("w_up", [NE, D, DFF], F32, kind=EI).ap()
    w_down = nc.dram_tensor("w_down", [NE, DFF, D], F32, kind=EI).ap()
    gf = nc.dram_tensor("norm_f_g", [1, D], F32, kind=EI).ap()
    kind = "Internal"
    qaT = nc.dram_tensor("qaT", [NS, 4, 128, S], BF16, kind=kind).ap()
    kaT = nc.dram_tensor("kaT", [NS, 4, 128, S], BF16, kind=kind).ap()
    qdT = nc.dram_tensor("qdT", [NS, 4, 128, S], BF16, kind=kind).ap()
    kdT = nc.dram_tensor("kdT", [NS, 4, 128, S], BF16, kind=kind).ap()
    va = nc.dram_tensor("va", [NS, S, 4 * 129], BF16, kind=kind).ap()
    vd = nc.dram_tensor("vd", [NS, S, 8 * 65], BF16, kind=kind).ap()
    a_dram = nc.dram_tensor("a_dram", [12, LTOT], BF16, kind=kind).ap()
    attn = nc.dram_tensor("attn", [NS, S, D], BF16, kind=kind).ap()
    U = nc.dram_tensor("U", [3, NS, S, 520], F32, kind=kind).ap()
    dbg = stop_after != "F"
    x1d = [nc.dram_tensor(f"x1d{i}", [S, D], F32, kind=kind).ap() for i in range(NS)]
    h2d = [nc.dram_tensor(f"h2d{i}", [S, D], BF16, kind=kind).ap() for i in range(NS)]
    out = nc.dram_tensor("out", [NS, S, D], F32, kind="ExternalOutput").ap()
    with ExitStack() as stack:
        fw = FW(nc, stack)
        with ExitStack() as es_tab:
            with nc.named_scope("tables"):
                tabs = setup_tables(nc, fw, es_tab, rel_bias, onehot_d, antiid_d, a_dram, lamv, subln_g)
            fw_barrier(fw)
            with nc.named_scope("phA"):
                phase_a(nc, fw, NS, x, w_in, g1, ident_d, qaT, kaT, va, qdT, kdT, vd)
            fw_barrier(fw)
            with nc.named_scope("phB"):
                phase_b(nc, fw, NS, tabs, qaT, kaT, va, attn)
            fw_barrier(fw)
            with nc.named_scope("phC"):
                phase_c(nc, fw, NS, tabs, qdT, kdT, vd, U)
            fw_barrier(fw)
        fw.out_events = []
        with ExitStack() as es_idx:
            RING_N = 3
            GRP0 = [(0, 4), (4, 4), (8, 4)]
            ring = {"wgu": [es_idx.enter_context(nc.sbuf_tensor(f"R_wgu{i}", [128, 2, 8, 512], BF16)) for i in range(RING_N)],
                    "B_wgu": [Buf(f"R_wgu{i}") for i in range(RING_N)]}
            def prefetch_ring():
                if stop_after == "D":
                    return
                for gi, (f0, nf) in enumerate(GRP0):
                    c0, c1 = f0 * 128, (f0 + nf) * 128
                    fw.dma(fw.pool, lambda: nc.gpsimd.dma_start(
                        out=ring["wgu"][gi][:, 0, :, 0:nf * 128], in_=w_gate[0, :, c0:c1].rearrange("(c p) n -> p c n", p=128)),
                        writes=[ring["B_wgu"][gi]])
                    fw.dma(fw.pool, lambda: nc.gpsimd.dma_start(
                        out=ring["wgu"][gi][:, 1, :, 0:nf * 128], in_=w_up[0, :, c0:c1].rearrange("(c p) n -> p c n", p=128)),
                        writes=[ring["B_wgu"][gi]], join=True)
            NPT = 32 * (NS - 1) + NE
            idxc = es_idx.enter_context(nc.sbuf_tensor("idxc", [128, 4 * NPT], I32))
            gc = es_idx.enter_context(nc.sbuf_tensor("gc", [128, 4 * NPT], F32))
            B_idxc = Buf("idxc")
            B_gc = Buf("gc")
            with ExitStack() as es_aff:
                affT = [es_aff.enter_context(nc.sbuf_tensor(f"affT{s}", [NPT if s == 0 else NE, S], F32)) for s in range(NS)]
                B_affT = [Buf(f"affT{s}") for s in range(NS)]
                fw.op(fw.dve, lambda: nc.vector.memset(affT[0][:], 0.0), writes=[B_affT[0]])
                with nc.named_scope("phD"):
                    phase_d(nc, fw, NS, attn, U, x, w_out, g2, w_router, ident_d, identf_d, x1d, h2d, affT, B_affT, hook=prefetch_ring)
                fw_barrier(fw)
                if stop_after != "D":
                    with nc.named_scope("phEtopk"):
                        phase_e_topk(nc, fw, NS, identf_d, affT, B_affT, idxc, gc, B_idxc, B_gc)
                    fw_barrier(fw)
            if stop_after != "D":
                with nc.named_scope("phEexp"):
                    phase_e_experts(nc, fw, NS, h2d, x1d, w_gate, w_up, w_down, ident_d, idxc, gc, B_idxc, B_gc, ring=ring)
                fw_barrier(fw)
        with nc.named_scope("phF"):
            phase_f(nc, fw, NS, x1d, gf, out)
        fw.finish()
    return nc


def kernel(**inputs):
    import ml_dtypes
    NS = 2
    NCORES = 8
    f32 = lambda a: np.ascontiguousarray(np.asarray(a), dtype=np.float32)
    x = f32(inputs["x"])
    common = {
        "w_in": f32(inputs["w_in"])[0], "norm1_g": f32(inputs["norm1_g"]).reshape(1, D),
        "rel_bias": f32(inputs["rel_bias"]),
        "lamv": np.stack([f32(inputs[k]).reshape(1, 64) for k in ["lam_q1", "lam_k1", "lam_q2", "lam_k2"]]),
        "subln_g": f32(inputs["subln_g"]).reshape(1, 128), "w_out": f32(inputs["w_out"])[0],
        "norm2_g": f32(inputs["norm2_g"]).reshape(1, D), "w_router": f32(inputs["w_router"])[0],
        "w_gate": f32(inputs["w_gate"])[0], "w_up": f32(inputs["w_up"])[0], "w_down": f32(inputs["w_down"])[0],
        "norm_f_g": f32(inputs["norm_f_g"]).reshape(1, D),
        "identf": np.eye(128, dtype=np.float32), **consts_np(), **onehot_np(),
    }
    nc = build_full(NS)
    in_maps = [{"x": np.ascontiguousarray(x[NS * c:NS * (c + 1)]), **common} for c in range(NCORES)]
    res = run_bass_kernel_spmd(nc, in_maps, core_ids=list(range(NCORES)))
    out = np.concatenate([np.asarray(r["out"], dtype=np.float32) for r in res.results], axis=0)
    return out.astype(np.float32)
```

```python
import numpy as np
import concourse.bass as bass
import concourse.mybir as mybir

F32 = mybir.dt.float32
BF16 = mybir.dt.bfloat16
I32 = mybir.dt.int32
U32 = mybir.dt.uint32
ALU = mybir.AluOpType
ACTF = mybir.ActivationFunctionType
AX = mybir.AxisListType

SEM_ROT = 12000


class Ev:
    __slots__ = ("sem", "val")

    def __init__(self, sem=None, val=None):
        self.sem = sem
        self.val = val


class Buf:
    __slots__ = ("name", "w", "weng", "r", "dsem", "dcnt")

    def __init__(self, name=""):
        self.name = name
        self.w = None
        self.weng = None
        self.r = []
        self.dsem = None
        self.dcnt = 0


def add_read(b, ev, E):
    if ev.sem is not None:
        b.r = [(e2, r2) for (e2, r2) in b.r if not (e2.sem is ev.sem and e2.val <= ev.val)]
    b.r.append((ev, E))


class Eng:
    def __init__(self, fw, raw, name):
        self.fw = fw
        self.raw = raw
        self.name = name
        self.sem = None
        self.count = 0
        self.waited = {}
        self.pending = []
        self.nsem = 0

    def _newsem(self):
        self.sem = self.fw.new_sem(f"{self.name}_{self.nsem}")
        self.nsem += 1
        self.count = 0

    def wait(self, ev):
        assert ev.sem is not None, "waiting on unresolved (unsignaled) event"
        k = id(ev.sem)
        if self.waited.get(k, 0) >= ev.val:
            return
        self.raw.wait_ge(ev.sem, ev.val)
        self.waited[k] = ev.val

    def signal(self, inst):
        if self.sem is None or self.count >= SEM_ROT:
            self._newsem()
        inst.then_inc(self.sem, 1)
        self.count += 1
        for p in self.pending:
            p.sem = self.sem
            p.val = self.count
        self.pending = []
        return Ev(self.sem, self.count)

    def lazy(self):
        e = Ev()
        self.pending.append(e)
        return e


class FW:
    def __init__(self, nc, stack):
        self.nc = nc
        self.stack = stack
        self.sems = []
        self.pe = Eng(self, nc.tensor, "pe")
        self.dve = Eng(self, nc.vector, "dve")
        self.act = Eng(self, nc.scalar, "act")
        self.pool = Eng(self, nc.gpsimd, "pool")
        self.sp = Eng(self, nc.sync, "sp")
        self.out_events = []
        self.dma_latest = {}

    def new_sem(self, name):
        name = f"{name}_{len(self.sems)}"
        s = self.stack.enter_context(self.nc.semaphore(name))
        self.sems.append(s)
        return s

    def op(self, E, fn, reads=(), writes=(), signal=True):
        for b in reads:
            if b.w is not None:
                if not (b.weng is E and E is self.pe):
                    E.wait(b.w)
        for b in writes:
            if b.w is not None and not (b.weng is E and E is self.pe):
                E.wait(b.w)
            for ev, re in b.r:
                if not (re is E and E is self.pe):
                    E.wait(ev)
        inst = fn()
        ev = E.signal(inst) if signal else E.lazy()
        for b in reads:
            add_read(b, ev, E)
        for b in writes:
            b.w = ev
            b.weng = E
            b.r = []
        return ev

    def dma(self, Q, fn, reads=(), writes=(), join=False, is_output=False, owner=None):
        for b in reads:
            if b.w is not None:
                Q.wait(b.w)
        for b in writes:
            if b.w is not None:
                if not (join and b.weng is None and b.dsem is not None and b.w.sem is b.dsem):
                    Q.wait(b.w)
            for ev, re in b.r:
                Q.wait(ev)
        inst = fn()
        d = owner if owner is not None else writes[0]
        if d.dsem is None or d.dcnt >= 16 * 3000:
            d.dsem = self.new_sem("d_" + d.name)
            d.dcnt = 0
        d.dcnt += 16
        inst.then_inc(d.dsem, 16)
        ev = Ev(d.dsem, d.dcnt)
        self.dma_latest[id(d.dsem)] = ev
        for b in reads:
            add_read(b, ev, None)
        for b in writes:
            b.w = ev
            b.weng = None
            b.r = []
        if is_output:
            self.out_events.append(ev)
        return ev

    def finish(self):
        seen = {}
        for ev in self.out_events:
            k = id(ev.sem)
            if k not in seen or seen[k].val < ev.val:
                seen[k] = ev
        for ev in seen.values():
            self.sp.wait(ev)


def fw_barrier(fw):
    evs = []
    for E in (fw.pe, fw.dve, fw.act, fw.pool, fw.sp):
        assert not E.pending, f"{E.name} has unsignaled tail instructions"
        if E.sem is not None and E.count > 0:
            evs.append(Ev(E.sem, E.count))
    for ev in fw.dma_latest.values():
        evs.append(ev)
    for E in (fw.pe, fw.dve, fw.act, fw.pool, fw.sp):
        for ev in evs:
            E.wait(ev)
    fw.dma_latest = {}


import numpy as np, math
from contextlib import ExitStack
import concourse.bass as bass
import concourse.mybir as mybir
from concourse.bass_utils import run_bass_kernel_spmd

S = 4096
D = 1024
NIN = 3072
EPS = 1e-6


def consts_np():
    import ml_dtypes
    ident = np.eye(128, dtype=np.float32).astype(ml_dtypes.bfloat16)
    return {"ident": ident}


def phase_a(nc, fw, NS, x, w_in, g1, ident_d, qaT, kaT, va, qdT, kdT, vd):
    with ExitStack() as es:
        sb = lambda name, shape, dt: es.enter_context(nc.sbuf_tensor("A_" + name, shape, dt))
        ps = lambda name, shape, dt: es.enter_context(nc.psum_tensor("A_" + name, shape, dt))
        win = sb("win", [128, 8, NIN], BF16)
        g1b = sb("g1b", [128, D], F32)
        ident = sb("ident", [128, 128], BF16)
        xb = [sb(f"xb{i}", [128, 4, D], F32) for i in range(2)]
        hb = [sb(f"hb{i}", [128, D], BF16) for i in range(2)]
        junk = sb("junk", [128, D], F32)
        ss = [sb(f"ss{i}", [128, 4], F32) for i in range(2)]
        rstd = [sb(f"rstd{i}", [128, 4], F32) for i in range(2)]
        hT = [sb(f"hT{i}", [128, 8, 512], BF16) for i in range(2)]
        st = [sb(f"st{i}", [128, 16, 512], BF16) for i in range(2)]
        vsa = [sb(f"vsa{i}", [128, 4, 4, 129], BF16) for i in range(2)]
        vsd = [sb(f"vsd{i}", [128, 4, 8, 65], BF16) for i in range(2)]
        tp = [ps(f"tp{i}", [128, 1024], BF16) for i in range(2)]
        mm = [ps(f"mm{i}", [128, 512], F32) for i in range(4)]

        B_win, B_g1, B_id = Buf("win"), Buf("g1"), Buf("ident")
        B_winc = [Buf(f"win{i}") for i in range(6)]
        B_xb = [Buf(f"xb{i}") for i in range(2)]
        B_hb = [Buf(f"hb{i}") for i in range(2)]
        B_junk = Buf("junk")
        B_ss = [Buf(f"ss{i}") for i in range(2)]
        B_rstd = [Buf(f"rstd{i}") for i in range(2)]
        B_hT = [Buf(f"hT{i}") for i in range(2)]
        B_st = [[Buf(f"st{i}_{g}") for g in range(4)] for i in range(2)]
        B_vsa = [Buf(f"vsa{i}") for i in range(2)]
        B_vsd = [Buf(f"vsd{i}") for i in range(2)]
        B_tp = [Buf(f"tp{i}") for i in range(2)]
        B_mm = [Buf(f"mm{i}") for i in range(4)]
        B_dram = Buf("dramA")

        for (c0, c1) in [(0, 512), (512, 1024), (1536, 2048), (2048, 2560), (1024, 1536), (2560, 3072)]:
            fw.dma(fw.pool, lambda: nc.gpsimd.dma_start(
                out=win[:, :, c0:c1], in_=w_in[:, c0:c1].rearrange("(c p) n -> p c n", p=128)),
                writes=[B_winc[c0 // 512]])
        fw.dma(fw.sp, lambda: nc.sync.dma_start(out=g1b[:], in_=g1.partition_broadcast(128)), writes=[B_g1])
        fw.dma(fw.sp, lambda: nc.sync.dma_start(out=ident[:], in_=ident_d), writes=[B_id])
        for i in range(2):
            fw.op(fw.pool, lambda: nc.gpsimd.memset(vsa[i][:], 1.0), writes=[B_vsa[i]])
            fw.op(fw.pool, lambda: nc.gpsimd.memset(vsd[i][:], 1.0), writes=[B_vsd[i]])

        nblk = NS * 8
        def load_x(b):
            s, t0 = divmod(b, 8)
            t0 *= 512
            fw.dma(fw.sp, lambda: nc.sync.dma_start(
                out=xb[b % 2][:], in_=x[s, t0:t0 + 512, :].rearrange("(a p) f -> p a f", p=128)),
                writes=[B_xb[b % 2]])
        load_x(0)
        if nblk > 1:
            load_x(1)
        mmi = [0]
        evi = [0]
        hbc = [0]

        def s1_stats(b):
            X = xb[b % 2]
            i2 = b % 2
            for a in range(4):
                fw.op(fw.dve, lambda: nc.vector.scalar_tensor_tensor(
                    out=junk[:], in0=X[:, a, :], scalar=1.0, in1=X[:, a, :],
                    op0=ALU.mult, op1=ALU.mult, accum_out=ss[i2][:, a:a + 1]),
                    reads=[B_xb[i2]], writes=[B_junk, B_ss[i2]])
            fw.op(fw.dve, lambda: nc.vector.tensor_scalar(
                out=rstd[i2][:], in0=ss[i2][:], scalar1=1.0 / D, scalar2=EPS,
                op0=ALU.mult, op1=ALU.add), reads=[B_ss[i2]], writes=[B_rstd[i2]])
            fw.op(fw.act, lambda: nc.scalar.activation(out=rstd[i2][:], in_=rstd[i2][:], func=ACTF.Sqrt),
                  reads=[B_rstd[i2]], writes=[B_rstd[i2]])
            fw.op(fw.dve, lambda: nc.vector.reciprocal(out=rstd[i2][:], in_=rstd[i2][:]),
                  reads=[B_rstd[i2]], writes=[B_rstd[i2]])

        def s1_sub(b, a):
            X = xb[b % 2]
            i2 = b % 2
            hbi = hbc[0] % 2
            hbc[0] += 1
            fw.op(fw.dve, lambda: nc.vector.scalar_tensor_tensor(
                out=hb[hbi][:], in0=X[:, a, :], scalar=rstd[i2][:, a:a + 1], in1=g1b[:],
                op0=ALU.mult, op1=ALU.mult),
                reads=[B_xb[i2], B_rstd[i2], B_g1], writes=[B_hb[hbi]])
            for half in range(2):
                for c4 in range(4):
                    c = half * 4 + c4
                    fw.op(fw.pe, lambda: nc.tensor.transpose(
                        out=tp[half][:, c4 * 128:(c4 + 1) * 128], in_=hb[hbi][:, c * 128:(c + 1) * 128],
                        identity=ident[:]),
                        reads=[B_hb[hbi], B_id], writes=[B_tp[half]], signal=(c4 == 3))
                if half == 0:
                    fw.op(fw.act, lambda: nc.scalar.copy(
                        out=hT[i2][:, 0:4, a * 128:(a + 1) * 128],
                        in_=tp[0][:, 0:512].rearrange("p (c t) -> p c t", c=4)),
                        reads=[B_tp[0]], writes=[B_hT[i2]])
                else:
                    fw.op(fw.dve, lambda: nc.vector.tensor_copy(
                        out=hT[i2][:, 4:8, a * 128:(a + 1) * 128],
                        in_=tp[1][:, 0:512].rearrange("p (c t) -> p c t", c=4)),
                        reads=[B_tp[1]], writes=[B_hT[i2]])

        fm_cols = [0, 128, 256, 384, 512, 640, 768, 896, 1536, 1664, 1792, 1920, 2048, 2176, 2304, 2432]

        def s2_fm(b, j):
            s, t0 = divmod(b, 8)
            t0 *= 512
            i2 = b % 2
            n0 = fm_cols[j]
            m = mmi[0] % 4
            mmi[0] += 1
            for c in range(8):
                fw.op(fw.pe, lambda: nc.tensor.matmul(
                    out=mm[m][:], lhsT=win[:, c, n0:n0 + 128], rhs=hT[i2][:, c, :],
                    start=(c == 0), stop=(c == 7)),
                    reads=[B_winc[n0 // 512], B_hT[i2]], writes=[B_mm[m]], signal=(c == 7))
            is_q = j < 4 or 8 <= j < 12
            g = j // 4
            if evi[0] % 2 == 0:
                fw.op(fw.act, lambda: nc.scalar.activation(
                    out=st[i2][:, j, :], in_=mm[m][:], func=ACTF.Copy, scale=(0.125 if is_q else 1.0)),
                    reads=[B_mm[m]], writes=[B_st[i2][g]])
            else:
                fw.op(fw.dve, lambda: nc.vector.tensor_scalar(
                    out=st[i2][:, j, :], in0=mm[m][:], scalar1=(0.125 if is_q else 1.0), scalar2=None,
                    op0=ALU.mult), reads=[B_mm[m]], writes=[B_st[i2][g]])
            evi[0] += 1
            if j % 4 == 3:
                dst = [qaT, kaT, qdT, kdT][g]
                fw.dma(fw.sp, lambda: nc.sync.dma_start(
                    out=dst[s, :, :, t0:t0 + 512].rearrange("h p t -> p h t"),
                    in_=st[i2][:, g * 4:(g + 1) * 4, :]),
                    reads=[B_st[i2][g]], writes=[B_dram], join=True, owner=B_st[i2][g])

        def s2_tm(b, a):
            i2 = b % 2
            for vi, n0 in enumerate([1024, 2560]):
                m = mmi[0] % 4
                mmi[0] += 1
                for c in range(8):
                    fw.op(fw.pe, lambda: nc.tensor.matmul(
                        out=mm[m][:], lhsT=hT[i2][:, c, a * 128:(a + 1) * 128], rhs=win[:, c, n0:n0 + 512],
                        start=(c == 0), stop=(c == 7)),
                        reads=[B_winc[n0 // 512], B_hT[i2]], writes=[B_mm[m]], signal=(c == 7))
                if vi == 0:
                    fw.op(fw.act, lambda: nc.scalar.copy(
                        out=vsa[i2][:, a, :, 0:128], in_=mm[m][:].rearrange("p (h d) -> p h d", h=4)),
                        reads=[B_mm[m]], writes=[B_vsa[i2]])
                else:
                    fw.op(fw.dve, lambda: nc.vector.tensor_copy(
                        out=vsd[i2][:, a, :, 0:64], in_=mm[m][:].rearrange("p (h d) -> p h d", h=8)),
                        reads=[B_mm[m]], writes=[B_vsd[i2]])

        def s2_store(b):
            s, t0 = divmod(b, 8)
            t0 *= 512
            i2 = b % 2
            fw.dma(fw.sp, lambda: nc.sync.dma_start(
                out=va[s, t0:t0 + 512, :].rearrange("(a p) f -> p a f", p=128),
                in_=vsa[i2][:].rearrange("p a h d -> p a (h d)")),
                reads=[B_vsa[i2]], writes=[B_dram], join=True, owner=B_vsa[i2])
            fw.dma(fw.sp, lambda: nc.sync.dma_start(
                out=vd[s, t0:t0 + 512, :].rearrange("(a p) f -> p a f", p=128),
                in_=vsd[i2][:].rearrange("p a h d -> p a (h d)")),
                reads=[B_vsd[i2]], writes=[B_dram], join=True, owner=B_vsd[i2])

        s1_stats(0)
        for a in range(4):
            s1_sub(0, a)
        for b in range(nblk):
            nxt = b + 1 < nblk
            if nxt:
                if b + 2 < nblk:
                    pass
                s1_stats(b + 1)
            if b + 1 < nblk:
                pass
            for j in range(16):
                s2_fm(b, j)
                if nxt and j % 4 == 3:
                    s1_sub(b + 1, j // 4)
            for a in range(4):
                s2_tm(b, a)
            s2_store(b)
            if b + 2 < nblk:
                load_x(b + 2)
        return B_dram


LA = 2304
LD = 384
LTOT = LA + 3 * LD
TW = 2176
DILS = (1, 4, 16)


def t5_bucket_np(rel):
    rel = np.asarray(rel, dtype=np.int64)
    n = np.abs(rel)
    nf = np.maximum(n, 1).astype(np.float32)
    large = 8 + (np.log(nf / np.float32(8)) / np.float32(math.log(128.0)) * np.float32(8)).astype(np.int32)
    large = np.minimum(large, 15)
    return np.where(rel > 0, 16, 0) + np.where(n < 8, n, large)


def onehot_np():
    import ml_dtypes
    oh = np.zeros((32, LTOT), dtype=np.float32)
    m = np.arange(2303)
    b = t5_bucket_np(1151 - m)
    oh[b, m] = 1.0
    for p, dil in enumerate(DILS):
        m = np.arange(383)
        off = 191 - m
        ok = np.abs(off) <= 64
        b = t5_bucket_np(off * dil)
        oh[b[ok], LA + p * LD + m[ok]] = 1.0
    J = np.eye(128, dtype=np.float32)[::-1].copy()
    return {"onehot": oh.astype(ml_dtypes.bfloat16), "antiid": J.astype(ml_dtypes.bfloat16)}


def setup_tables(nc, fw, es, rel_bias, onehot_d, antiid_d, a_dram, lamv, subln_g):
    sb = lambda name, shape, dt: es.enter_context(nc.sbuf_tensor("T_" + name, shape, dt))
    TA = sb("TA", [128, 4, TW], BF16)
    TD = sb("TD", [128, 3, 8, 256], BF16)
    cfar = sb("cfar", [128, 24], F32)
    nlam = sb("nlam", [128, 1], F32)
    subg = sb("subg", [128, 128], F32)
    B = {k: Buf(k) for k in ["TA", "TD", "cfar", "nlam", "subg"]}
    with ExitStack() as es2:
        sb2 = lambda name, shape, dt: es2.enter_context(nc.sbuf_tensor("T2_" + name, shape, dt))
        ps2 = lambda name, shape, dt: es2.enter_context(nc.psum_tensor("T2_" + name, shape, dt))
        rb = sb2("rb", [32, 12], F32)
        eb = sb2("eb", [32, 12], BF16)
        oh = sb2("oh", [32, LTOT], BF16)
        J = sb2("J", [128, 128], BF16)
        Asb = sb2("Asb", [12, LTOT], BF16)
        Hk = sb2("Hk", [128, TW], BF16)
        Hd = sb2("Hd", [128, 3, 8, 256], BF16)
        lv = sb2("lv", [128, 4, 64], F32)
        lj = sb2("lj", [128, 64], F32)
        ls = sb2("ls", [128, 2], F32)
        pA = [ps2(f"pA{i}", [128, 512], F32) for i in range(2)]
        B_rb, B_eb, B_oh, B_J, B_A, B_Hk, B_Hd, B_lv, B_lj, B_ls = [Buf(n) for n in
            ["rb", "eb", "oh", "J", "A", "Hk", "Hd", "lv", "lj", "ls"]]
        B_pA = [Buf("pA0"), Buf("pA1")]
        B_ad = Buf("a_dram")
        fw.dma(fw.sp, lambda: nc.sync.dma_start(out=rb[:], in_=rel_bias), writes=[B_rb])
        fw.dma(fw.sp, lambda: nc.sync.dma_start(out=oh[:], in_=onehot_d), writes=[B_oh])
        fw.dma(fw.sp, lambda: nc.sync.dma_start(out=J[:], in_=antiid_d), writes=[B_J])
        fw.dma(fw.sp, lambda: nc.sync.dma_start(out=cfar[:, 0:12], in_=rel_bias[15:16, :].partition_broadcast(128)),
               writes=[B["cfar"]])
        fw.dma(fw.sp, lambda: nc.sync.dma_start(out=cfar[:, 12:24], in_=rel_bias[31:32, :].partition_broadcast(128)),
               writes=[B["cfar"]], join=True)
        for i in range(4):
            fw.dma(fw.sp, lambda: nc.sync.dma_start(out=lv[:, i, :], in_=lamv[i].partition_broadcast(128)),
                   writes=[B_lv], join=True)
        fw.dma(fw.sp, lambda: nc.sync.dma_start(out=subg[:], in_=subln_g.partition_broadcast(128)), writes=[B["subg"]])
        for i in range(2):
            fw.op(fw.dve, lambda: nc.vector.scalar_tensor_tensor(
                out=lj[:], in0=lv[:, 2 * i, :], scalar=1.0, in1=lv[:, 2 * i + 1, :], op0=ALU.mult, op1=ALU.mult,
                accum_out=ls[:, i:i + 1]), reads=[B_lv], writes=[B_lj, B_ls])
        fw.op(fw.act, lambda: nc.scalar.activation(out=ls[:], in_=ls[:], func=ACTF.Exp), reads=[B_ls], writes=[B_ls])
        fw.op(fw.dve, lambda: nc.vector.tensor_tensor(out=nlam[:], in0=ls[:, 1:2], in1=ls[:, 0:1], op=ALU.subtract),
              reads=[B_ls], writes=[B["nlam"]])
        fw.op(fw.dve, lambda: nc.vector.tensor_scalar(out=nlam[:], in0=nlam[:], scalar1=-0.2, scalar2=None, op0=ALU.add),
              reads=[B["nlam"]], writes=[B["nlam"]])
        fw.op(fw.dve, lambda: nc.vector.tensor_scalar(out=subg[:], in0=subg[:], scalar1=0.8, scalar2=None, op0=ALU.mult),
              reads=[B["subg"]], writes=[B["subg"]])
        fw.op(fw.act, lambda: nc.scalar.activation(out=eb[:], in_=rb[:], func=ACTF.Exp), reads=[B_rb], writes=[B_eb])
        nch = (LTOT + 511) // 512
        for ci in range(nch):
            c0 = ci * 512
            w = min(512, LTOT - c0)
            pi = ci % 2
            fw.op(fw.pe, lambda: nc.tensor.matmul(out=pA[pi][0:12, 0:w], lhsT=eb[:, :], rhs=oh[:, c0:c0 + w],
                                                  start=True, stop=True),
                  reads=[B_eb, B_oh], writes=[B_pA[pi]])
            fw.op(fw.dve, lambda: nc.vector.tensor_copy(out=Asb[:, c0:c0 + w], in_=pA[pi][0:12, 0:w]),
                  reads=[B_pA[pi]], writes=[B_A])
        fw.dma(fw.sp, lambda: nc.sync.dma_start(out=a_dram, in_=Asb[:]), reads=[B_A], writes=[B_ad])
        adt = a_dram.tensor
        for h in range(4):
            src = bass.AP(tensor=adt, offset=h * LTOT, ap=[[1, 128], [1, TW]])
            fw.dma(fw.sp, lambda: nc.sync.dma_start(out=Hk[:], in_=src), reads=[B_ad], writes=[B_Hk])
            for ci in range(5):
                c0 = ci * 512
                w = min(512, TW - c0)
                pi = ci % 2
                fw.op(fw.pe, lambda: nc.tensor.matmul(out=pA[pi][:, 0:w], lhsT=J[:], rhs=Hk[:, c0:c0 + w],
                                                      start=True, stop=True),
                      reads=[B_J, B_Hk], writes=[B_pA[pi]])
                fw.op(fw.dve, lambda: nc.vector.tensor_copy(out=TA[:, h, c0:c0 + w], in_=pA[pi][:, 0:w]),
                      reads=[B_pA[pi]], writes=[B["TA"]])
        for p in range(3):
            for h in range(8):
                src = bass.AP(tensor=adt, offset=(4 + h) * LTOT + LA + p * LD, ap=[[1, 128], [1, 256]])
                fw.dma(fw.sp, lambda: nc.sync.dma_start(out=Hd[:, p, h, :], in_=src), reads=[B_ad], writes=[B_Hd], join=True)
        for p in range(3):
            for h2 in range(4):
                pi = (p * 4 + h2) % 2
                fw.op(fw.pe, lambda: nc.tensor.matmul(
                    out=pA[pi][:, :], lhsT=J[:], rhs=Hd[:, p, 2 * h2:2 * h2 + 2, :].rearrange("p a b -> p (a b)"),
                    start=True, stop=True), reads=[B_J, B_Hd], writes=[B_pA[pi]])
                fw.op(fw.dve, lambda: nc.vector.tensor_copy(
                    out=TD[:, p, 2 * h2:2 * h2 + 2, :].rearrange("p a b -> p (a b)"), in_=pA[pi][:, :]),
                    reads=[B_pA[pi]], writes=[B["TD"]])
    return dict(TA=TA, TD=TD, cfar=cfar, nlam=nlam, subg=subg, B=B)


def phase_b(nc, fw, NS, tabs, qaT, kaT, va, attn):
    TA, cfar, nlam, subg = tabs["TA"], tabs["cfar"], tabs["nlam"], tabs["subg"]
    TB = tabs["B"]
    with ExitStack() as es:
        sb = lambda name, shape, dt: es.enter_context(nc.sbuf_tensor("B_" + name, shape, dt))
        ps = lambda name, shape, dt: es.enter_context(nc.psum_tensor("B_" + name, shape, dt))
        QT = [sb(f"QT{i}", [128, S], BF16) for i in range(2)]
        KT = [sb(f"KT{i}", [128, S], BF16) for i in range(2)]
        V = [sb(f"V{i}", [128, 32, 4 * 129], BF16) for i in range(2)]
        NP = 5
        P = [sb(f"P{i}", [128, 1024], BF16) for i in range(NP)]
        sc = [ps(f"sc{i}", [128, 1024], F32) for i in range(2)]
        acc = [ps(f"acc{i}", [128, 512], F32) for i in range(3)]
        rr = sb("rr", [128, 8], F32)
        accs = sb("accs", [128, 3, 512], F32)
        B_accs = Buf("accs")
        t1 = sb("t1", [128, 128], F32)
        o4 = sb("o4", [128, 4, 128], F32)
        junk = sb("junk", [128, 128], F32)
        ssq = sb("ssq", [128, 4], F32)
        rq = sb("rq", [128, 4], F32)
        ob = [sb(f"ob{i}", [128, 4, 128], BF16) for i in range(2)]
        B_QT = [Buf(f"QT{i}") for i in range(2)]
        B_KT = [Buf(f"KT{i}") for i in range(2)]
        B_V = [Buf(f"V{i}") for i in range(2)]
        B_P = [Buf(f"P{i}") for i in range(NP)]
        B_sc = [Buf(f"sc{i}") for i in range(2)]
        B_accb = [Buf(f"acc{i}") for i in range(3)]
        B_acc = [B_accb[i // 3] for i in range(8)]
        B_rr, B_t1, B_o4, B_junk, B_ssq, B_rq = [Buf(n) for n in ["rr", "t1", "o4", "junk", "ssq", "rq"]]
        B_ob = [Buf("ob0"), Buf("ob1")]
        B_attn = Buf("attn_a")

        def accap(idx):
            return acc[idx // 3][:, (idx % 3) * 129:(idx % 3 + 1) * 129]

        def load_qk(i):
            s, h = divmod(i, 4)
            fw.dma(fw.sp, lambda: nc.sync.dma_start(out=QT[i % 2][:], in_=qaT[s, h]), writes=[B_QT[i % 2]])
            fw.dma(fw.sp, lambda: nc.sync.dma_start(out=KT[i % 2][:], in_=kaT[s, h]), writes=[B_KT[i % 2]])

        def load_v(s):
            fw.dma(fw.sp, lambda: nc.sync.dma_start(
                out=V[s % 2][:], in_=va[s].rearrange("(a p) f -> p a f", p=128)), writes=[B_V[s % 2]])

        load_v(0)
        load_qk(0)
        pi = 0
        obi = 0
        pending = [None]
        for i in range(NS * 4):
            s, h = divmod(i, 4)
            if i + 1 < NS * 4:
                load_qk(i + 1)
                if (i + 1) % 4 == 0:
                    load_v(s + 1)
            q_, k_, v_ = QT[i % 2], KT[i % 2], V[s % 2]
            bq, bk, bv = B_QT[i % 2], B_KT[i % 2], B_V[s % 2]
            for qb in range(8):
                q0 = qb * 512

                def emit_scores(kt):
                    k0 = kt * 128
                    for w in range(2):
                        lo = w * 64
                        fw.op(fw.pe, lambda: nc.tensor.matmul(
                            out=sc[kt % 2][:, w * 512:(w + 1) * 512], lhsT=k_[lo:lo + 64, k0:k0 + 128],
                            rhs=q_[lo:lo + 64, q0:q0 + 512],
                            start=True, stop=True), reads=[bq, bk], writes=[B_sc[kt % 2]], signal=(w == 1))

                emit_scores(0)
                emit_scores(1)
                for kt in range(32):
                    k0 = kt * 128
                    d = k0 - q0
                    near = -640 <= d <= 1024
                    pb = P[pi % NP]
                    bpb = B_P[pi % NP]
                    pi += 1
                    if near:
                        fw.op(fw.act, lambda: nc.scalar.activation(out=pb[:], in_=sc[kt % 2][:], func=ACTF.Exp),
                              reads=[B_sc[kt % 2]], writes=[bpb])
                        c0 = 1024 - d
                        tsl = TA[:, h, c0:c0 + 512]
                        tbc = bass.AP(tensor=tsl.tensor, offset=tsl.offset, ap=[list(tsl.ap[0]), [0, 2], [1, 512]])
                        pv2 = pb[:].rearrange("p (w x) -> p w x", w=2)
                        fw.op(fw.dve, lambda: nc.vector.tensor_tensor(out=pv2, in0=pv2, in1=tbc, op=ALU.mult),
                              reads=[bpb, TB["TA"]], writes=[bpb])
                    else:
                        col = h + (12 if d > 0 else 0)
                        fw.op(fw.act, lambda: nc.scalar.activation(
                            out=pb[:], in_=sc[kt % 2][:], func=ACTF.Exp, bias=cfar[:, col:col + 1]),
                            reads=[B_sc[kt % 2], TB["cfar"]], writes=[bpb])
                    if kt == 12 and pending[0] is not None:
                        pending[0]()
                        pending[0] = None
                    if kt + 2 < 32:
                        emit_scores(kt + 2)
                    for qs in range(4):
                        for w in range(2):
                            idx = qs * 2 + w
                            last = (qs == 3 and w == 1)
                            fw.op(fw.pe, lambda: nc.tensor.matmul(
                                out=accap(idx), lhsT=pb[:, w * 512 + qs * 128:w * 512 + (qs + 1) * 128],
                                rhs=v_[:, kt, h * 129:(h + 1) * 129],
                                start=(kt == 0 and idx % 3 == 0), stop=(kt == 31), skip_group_check=True),
                                reads=[bpb, bv], writes=[B_acc[idx]], signal=last)
                for bnk in range(3):
                    ncol = 387 if bnk < 2 else 258
                    fw.op(fw.dve, lambda: nc.vector.tensor_copy(out=accs[:, bnk, 0:ncol], in_=acc[bnk][:, 0:ncol]),
                          reads=[B_accb[bnk]], writes=[B_accs])
                for qs in range(4):
                    i1, i2 = qs * 2, qs * 2 + 1
                    a1 = accs[:, i1 // 3, (i1 % 3) * 129:(i1 % 3 + 1) * 129]
                    a2 = accs[:, i2 // 3, (i2 % 3) * 129:(i2 % 3 + 1) * 129]
                    b1, b2 = B_accs, B_accs
                    fw.op(fw.dve, lambda: nc.vector.reciprocal(out=rr[:, 2 * qs:2 * qs + 1], in_=a1[:, 128:129]),
                          reads=[b1], writes=[B_rr])
                    fw.op(fw.dve, lambda: nc.vector.reciprocal(out=rr[:, 2 * qs + 1:2 * qs + 2], in_=a2[:, 128:129]),
                          reads=[b2], writes=[B_rr])
                    fw.op(fw.dve, lambda: nc.vector.tensor_tensor(
                        out=rr[:, 2 * qs + 1:2 * qs + 2], in0=rr[:, 2 * qs + 1:2 * qs + 2], in1=nlam[:], op=ALU.mult),
                        reads=[B_rr, TB["nlam"]], writes=[B_rr])
                    fw.op(fw.dve, lambda: nc.vector.tensor_scalar(
                        out=t1[:], in0=a1[:, 0:128], scalar1=rr[:, 2 * qs:2 * qs + 1], scalar2=None, op0=ALU.mult),
                        reads=[b1, B_rr], writes=[B_t1])
                    fw.op(fw.dve, lambda: nc.vector.scalar_tensor_tensor(
                        out=o4[:, qs, :], in0=a2[:, 0:128], scalar=rr[:, 2 * qs + 1:2 * qs + 2], in1=t1[:],
                        op0=ALU.mult, op1=ALU.add),
                        reads=[b2, B_rr, B_t1], writes=[B_o4])
                    fw.op(fw.dve, lambda: nc.vector.scalar_tensor_tensor(
                        out=junk[:], in0=o4[:, qs, :], scalar=1.0, in1=o4[:, qs, :], op0=ALU.mult, op1=ALU.mult,
                        accum_out=ssq[:, qs:qs + 1]),
                        reads=[B_o4], writes=[B_junk, B_ssq])
                fw.op(fw.dve, lambda: nc.vector.tensor_scalar(
                    out=rq[:], in0=ssq[:], scalar1=1.0 / 128, scalar2=1e-5, op0=ALU.mult, op1=ALU.add),
                    reads=[B_ssq], writes=[B_rq])

                def fin2(s=s, h=h, q0=q0):
                    nonlocal obi
                    obt = ob[obi % 2]
                    bob = B_ob[obi % 2]
                    obi += 1
                    fw.op(fw.act, lambda: nc.scalar.activation(out=rq[:], in_=rq[:], func=ACTF.Ln),
                          reads=[B_rq], writes=[B_rq])
                    fw.op(fw.act, lambda: nc.scalar.activation(out=rq[:], in_=rq[:], func=ACTF.Exp, scale=-0.5),
                          reads=[B_rq], writes=[B_rq])
                    for qs in range(4):
                        fw.op(fw.dve, lambda: nc.vector.scalar_tensor_tensor(
                            out=obt[:, qs, :], in0=o4[:, qs, :], scalar=rq[:, qs:qs + 1], in1=subg[:],
                            op0=ALU.mult, op1=ALU.mult),
                            reads=[B_o4, B_rq, TB["subg"]], writes=[bob])
                    fw.dma(fw.sp, lambda: nc.sync.dma_start(
                        out=attn[s, q0:q0 + 512, h * 128:(h + 1) * 128].rearrange("(a p) f -> p a f", p=128),
                        in_=obt[:]), reads=[bob], writes=[B_attn], join=True, owner=bob, is_output=True)
                pending[0] = fin2
        if pending[0] is not None:
            pending[0]()
        return B_attn


def phase_c(nc, fw, NS, tabs, qdT, kdT, vd, U, pats=(0, 1, 2)):
    TD = tabs["TD"]
    TB = tabs["B"]
    with ExitStack() as es:
        sb = lambda name, shape, dt: es.enter_context(nc.sbuf_tensor("C_" + name, shape, dt))
        ps = lambda name, shape, dt: es.enter_context(nc.psum_tensor("C_" + name, shape, dt))
        Qn = [sb(f"Qn{i}", [128, S], BF16) for i in range(2)]
        Kn = [sb(f"Kn{i}", [128, S], BF16) for i in range(2)]
        Qp = [sb(f"Qp{i}", [128, S], BF16) for i in range(2)]
        Kp = [sb(f"Kp{i}", [128, 6144], BF16) for i in range(2)]
        Vt = sb("Vt", [128, 48, 520], BF16)
        NPB = 4
        P = [sb(f"P{i}", [128, 512], BF16) for i in range(NPB)]
        ust = [sb(f"ust{i}", [128, 32, 130], F32) for i in range(2)]
        sc = [ps(f"sc{i}", [128, 1024], F32) for i in range(3)]
        acc = [ps(f"acc{i}", [128, 512], F32) for i in range(2)]
        B_Qn = [Buf(f"Qn{i}") for i in range(2)]
        B_Kn = [Buf(f"Kn{i}") for i in range(2)]
        B_Qp = [Buf(f"Qp{i}") for i in range(2)]
        B_Kp = [Buf(f"Kp{i}") for i in range(2)]
        B_Vt = Buf("Vt")
        B_P = [Buf(f"P{i}") for i in range(NPB)]
        B_ust = [Buf(f"ust{i}") for i in range(2)]
        B_sc = [Buf(f"sc{i}") for i in range(3)]
        B_acc = [Buf(f"acc{i}") for i in range(2)]
        B_U = Buf("U")

        it = 0
        blk = 0
        for s in range(NS):
            for p, dil in enumerate(DILS):
                if p not in pats:
                    continue
                mlen = S // dil
                nb = mlen // 128
                ML = mlen + 128
                Vv = Vt[:, 0:dil * (nb + 1), :].rearrange("q (r t) f -> q r t f", r=dil)
                fw.op(fw.dve, lambda: nc.vector.memset(Vv[0:64, :, 0, :], 0.0), writes=[B_Vt])
                fw.op(fw.dve, lambda: nc.vector.memset(Vv[64:128, :, nb, :], 0.0), writes=[B_Vt])
                vt_ = vd.tensor
                base = s * S * 520
                for r in range(dil):
                    if nb > 1:
                        src = bass.AP(tensor=vt_, offset=base + ((128 - 64) * dil + r) * 520,
                                      ap=[[dil * 520, 128], [128 * dil * 520, nb - 1], [1, 520]])
                        fw.dma(fw.sp, lambda: nc.sync.dma_start(out=Vv[:, r, 1:nb, :], in_=src), writes=[B_Vt], join=True)
                    src = bass.AP(tensor=vt_, offset=base + r * 520, ap=[[dil * 520, 64], [1, 520]])
                    fw.dma(fw.sp, lambda: nc.sync.dma_start(out=Vv[64:128, r, 0, :], in_=src), writes=[B_Vt], join=True)
                    src = bass.AP(tensor=vt_, offset=base + ((mlen - 64) * dil + r) * 520, ap=[[dil * 520, 64], [1, 520]])
                    fw.dma(fw.sp, lambda: nc.sync.dma_start(out=Vv[0:64, r, nb, :], in_=src), writes=[B_Vt], join=True)
                def prep(c):
                    nonlocal it
                    i2 = it % 2
                    it += 1
                    fw.dma(fw.sp, lambda: nc.sync.dma_start(out=Qn[i2][:], in_=qdT[s, c]), writes=[B_Qn[i2]])
                    fw.dma(fw.sp, lambda: nc.sync.dma_start(out=Kn[i2][:], in_=kdT[s, c]), writes=[B_Kn[i2]])
                    Kv = Kp[i2][:, 0:dil * ML].rearrange("q (r m) -> q r m", r=dil)
                    fw.op(fw.dve, lambda: nc.vector.memset(Kv[:, :, 0:64], 0.0), writes=[B_Kp[i2]])
                    fw.op(fw.dve, lambda: nc.vector.memset(Kv[:, :, 64 + mlen:ML], 0.0), writes=[B_Kp[i2]])
                    fw.op(fw.dve, lambda: nc.vector.tensor_copy(
                        out=Kv[:, :, 64:64 + mlen], in_=Kn[i2][:].rearrange("q (m r) -> q r m", r=dil)),
                        reads=[B_Kn[i2]], writes=[B_Kp[i2]])
                    if dil > 1:
                        Qv = Qp[i2][:].rearrange("q (r m) -> q r m", r=dil)
                        fw.op(fw.dve, lambda: nc.vector.tensor_copy(
                            out=Qv, in_=Qn[i2][:].rearrange("q (m r) -> q r m", r=dil)),
                            reads=[B_Qn[i2]], writes=[B_Qp[i2]])
                        bq = B_Qp[i2]
                    else:
                        Qv = Qn[i2][:].rearrange("q (r m) -> q r m", r=1)
                        bq = B_Qn[i2]
                    return dict(i2=i2, Kv=Kv, Qv=Qv, bq=bq)

                ctx_next = prep(0)
                for c in range(4):
                    ctx = ctx_next
                    i2, Kv, Qv, bq = ctx["i2"], ctx["Kv"], ctx["Qv"], ctx["bq"]
                    us = ust[i2]
                    blocks = [(r, bi) for r in range(dil) for bi in range(nb)]
                    nblk_c = len(blocks)

                    def emit_sc(bidx):
                        r, bi = blocks[bidx]
                        m0 = bi * 128
                        sci = bidx % 3
                        for hh in range(2):
                            lo = hh * 64
                            fw.op(fw.pe, lambda: nc.tensor.matmul(
                                out=sc[sci][:, hh * 512:hh * 512 + 128],
                                lhsT=Kv[lo:lo + 64, r, m0 + 128:m0 + 256], rhs=Qv[lo:lo + 64, r, m0:m0 + 128],
                                start=True, stop=True), reads=[B_Kp[i2], bq], writes=[B_sc[sci]], signal=False)
                            fw.op(fw.pe, lambda: nc.tensor.matmul(
                                out=sc[sci][:, hh * 512 + 128:hh * 512 + 256],
                                lhsT=Kv[lo:lo + 64, r, m0:m0 + 128], rhs=Qv[lo:lo + 64, r, m0:m0 + 128],
                                start=True, stop=True), reads=[B_Kp[i2], bq], writes=[B_sc[sci]], signal=(hh == 1))

                    pbs = {}

                    def emit_exp(bidx):
                        nonlocal blk
                        sci = bidx % 3
                        pb = P[blk % NPB]
                        bpb = B_P[blk % NPB]
                        blk += 1
                        pbs[bidx] = (pb, bpb)
                        fw.op(fw.act, lambda: nc.scalar.activation(
                            out=pb[:].rearrange("q (h x) -> q h x", h=2),
                            in_=sc[sci][:].rearrange("q (h x) -> q h x", h=2)[:, :, 0:256], func=ACTF.Exp),
                              reads=[B_sc[sci]], writes=[bpb])
                        fw.op(fw.dve, lambda: nc.vector.tensor_tensor(
                            out=pb[:], in0=pb[:], in1=TD[:, p, 2 * c:2 * c + 2, :].rearrange("q a b -> q (a b)"),
                            op=ALU.mult), reads=[bpb, TB["TD"]], writes=[bpb])

                    for j0 in range(min(3, nblk_c)):
                        emit_sc(j0)
                    for j0 in range(min(2, nblk_c)):
                        emit_exp(j0)
                    for bidx in range(nblk_c):
                        r, bi = blocks[bidx]
                        aci = bidx % 2
                        if bidx + 2 < nblk_c:
                            emit_exp(bidx + 2)
                        pb, bpb = pbs.pop(bidx)
                        if bidx == 2 and c + 1 < 4:
                            ctx_next = prep(c + 1)
                        for hh in range(2):
                            h = 2 * c + hh
                            fw.op(fw.pe, lambda: nc.tensor.matmul(
                                out=acc[aci][:, hh * 65:(hh + 1) * 65], lhsT=pb[:, hh * 256:hh * 256 + 128],
                                rhs=Vv[:, r, bi + 1, h * 65:(h + 1) * 65], start=(hh == 0), stop=False,
                                skip_group_check=True),
                                reads=[bpb, B_Vt], writes=[B_acc[aci]], signal=False)
                            fw.op(fw.pe, lambda: nc.tensor.matmul(
                                out=acc[aci][:, hh * 65:(hh + 1) * 65], lhsT=pb[:, hh * 256 + 128:hh * 256 + 256],
                                rhs=Vv[:, r, bi, h * 65:(h + 1) * 65], start=False, stop=True,
                                skip_group_check=True),
                                reads=[bpb, B_Vt], writes=[B_acc[aci]], signal=(hh == 1))
                        if bidx + 3 < nblk_c:
                            emit_sc(bidx + 3)
                        fw.op(fw.act, lambda: nc.scalar.copy(out=us[:, r * nb + bi, 0:65], in_=acc[aci][:, 0:65]),
                              reads=[B_acc[aci]], writes=[B_ust[i2]])
                        fw.op(fw.dve, lambda: nc.vector.tensor_copy(out=us[:, r * nb + bi, 65:130], in_=acc[aci][:, 65:130]),
                              reads=[B_acc[aci]], writes=[B_ust[i2]])
                    for r in range(dil):
                        dst = bass.AP(tensor=U.tensor, offset=((p * NS + s) * S + r) * 520 + c * 130,
                                      ap=[[dil * 520, 128], [128 * dil * 520, nb], [1, 130]])
                        fw.dma(fw.sp, lambda: nc.sync.dma_start(out=dst, in_=us[:, r * nb:(r + 1) * nb, :]),
                               reads=[B_ust[i2]], writes=[B_U], join=True, owner=B_ust[i2], is_output=True)
        return B_U


NE = 16
DFF = 2816
NF = 22
CAP = 512


def phase_d(nc, fw, NS, attn, U, x, w_out, g2, w_router, ident_d, identf_d, x1d, h2d, affT, B_affT, hook=None):
    with ExitStack() as es:
        sb = lambda name, shape, dt: es.enter_context(nc.sbuf_tensor("D_" + name, shape, dt))
        ps = lambda name, shape, dt: es.enter_context(nc.psum_tensor("D_" + name, shape, dt))
        wout = sb("wout", [128, 8, D], BF16)
        wr = sb("wr", [128, 8, NE], F32)
        g2b = sb("g2b", [128, D], F32)
        ident = sb("ident", [128, 128], BF16)
        identf = sb("identf", [128, 128], F32)
        aa = [sb(f"aa{i}", [128, 512], BF16) for i in range(2)]
        uu = [sb(f"uu{i}", [128, 3, 520], F32) for i in range(2)]
        xx = [sb(f"xx{i}", [128, D], F32) for i in range(3)]
        us = sb("us", [128, 520], F32)
        rden = sb("rden", [128, 8], F32)
        ad = sb("ad", [128, 512], BF16)
        aT = [sb(f"aT{i}", [128, 8, 128], BF16) for i in range(2)]
        x1t = [sb(f"x1t{i}", [128, D], F32) for i in range(2)]
        junk = sb("junk", [128, D], F32)
        ss = [sb(f"ss{i}", [128, 1], F32) for i in range(2)]
        rstd = [sb(f"rstd{i}", [128, 1], F32) for i in range(2)]
        h2f = sb("h2f", [128, D], F32)
        h2b = [sb(f"h2b{i}", [128, D], BF16) for i in range(2)]
        h2T = sb("h2T", [128, 8, 128], F32)
        ex = sb("ex", [128, NE], F32)
        se = sb("se", [128, 1], F32)
        aff = sb("aff", [128, NE], F32)
        tp = [ps(f"tp{i}", [128, 1024], BF16) for i in range(2)]
        mm = [ps(f"mm{i}", [128, 512], F32) for i in range(2)]
        tf = [ps(f"tf{i}", [128, 512], F32) for i in range(2)]
        lg = ps("lg", [128, 512], F32)
        at = ps("at", [128, 512], F32)
        names = ["wout", "wr", "g2b", "ident", "identf", "us", "rden", "ad", "junk", "h2f", "h2T",
                 "ex", "se", "aff", "lg", "at"]
        B = {n: Buf(n) for n in names}
        for n in ["aa", "uu", "xx", "x1t", "h2b", "tp", "mm", "tf", "ss", "rstd", "aT"]:
            B[n] = [Buf(n + "0"), Buf(n + "1"), Buf(n + "2")]
        B_x1d, B_h2d = Buf("x1d"), Buf("h2d")

        fw.dma(fw.pool, lambda: nc.gpsimd.dma_start(out=wout[:], in_=w_out.rearrange("(c p) n -> p c n", p=128)),
               writes=[B["wout"]])
        fw.dma(fw.sp, lambda: nc.sync.dma_start(out=wr[:], in_=w_router.rearrange("(c p) n -> p c n", p=128)),
               writes=[B["wr"]])
        fw.dma(fw.sp, lambda: nc.sync.dma_start(out=g2b[:], in_=g2.partition_broadcast(128)), writes=[B["g2b"]])
        fw.dma(fw.sp, lambda: nc.sync.dma_start(out=ident[:], in_=ident_d), writes=[B["ident"]])
        fw.dma(fw.sp, lambda: nc.sync.dma_start(out=identf[:], in_=identf_d), writes=[B["identf"]])
        if hook is not None:
            hook()

        nblk128 = NS * 32

        def idx(i):
            s, tt = divmod(i, 32)
            return s, tt, tt * 128, i % 2

        def load_au(i):
            s, tt, t0, k = idx(i)
            fw.dma(fw.sp, lambda: nc.sync.dma_start(out=aa[k][:], in_=attn[s, t0:t0 + 128, 0:512]), writes=[B["aa"][k]])
            src = bass.AP(tensor=U.tensor, offset=(s * S + t0) * 520, ap=[[520, 128], [NS * S * 520, 3], [1, 520]])
            fw.dma(fw.sp, lambda: nc.sync.dma_start(out=uu[k][:], in_=src), writes=[B["uu"][k]])

        def T0(i):
            s, tt, t0, k = idx(i)
            if i + 1 < nblk128:
                load_au(i + 1)
            fw.op(fw.dve, lambda: nc.vector.tensor_tensor(out=us[:], in0=uu[k][:, 0, :], in1=uu[k][:, 1, :], op=ALU.add),
                  reads=[B["uu"][k]], writes=[B["us"]])
            fw.op(fw.dve, lambda: nc.vector.tensor_tensor(out=us[:], in0=us[:], in1=uu[k][:, 2, :], op=ALU.add),
                  reads=[B["uu"][k], B["us"]], writes=[B["us"]])
            usv = us[:].rearrange("p (h d) -> p h d", h=8)
            fw.op(fw.dve, lambda: nc.vector.reciprocal(out=rden[:], in_=usv[:, :, 64]), reads=[B["us"]], writes=[B["rden"]])
            for h in range(8):
                fw.op(fw.dve, lambda: nc.vector.tensor_scalar(
                    out=ad[:, h * 64:(h + 1) * 64], in0=usv[:, h, 0:64], scalar1=rden[:, h:h + 1], scalar2=None,
                    op0=ALU.mult), reads=[B["us"], B["rden"]], writes=[B["ad"]])

        def T1(i):
            s, tt, t0, k = idx(i)
            for half, (src_t, bsrc) in enumerate([(aa[k], B["aa"][k]), (ad, B["ad"])]):
                for c in range(4):
                    fw.op(fw.pe, lambda: nc.tensor.transpose(
                        out=tp[half][:, c * 128:(c + 1) * 128], in_=src_t[:, c * 128:(c + 1) * 128], identity=ident[:]),
                        reads=[bsrc, B["ident"]], writes=[B["tp"][half]], signal=(c == 3))

        def T2(i):
            s, tt, t0, k = idx(i)
            fw.op(fw.act, lambda: nc.scalar.copy(
                out=aT[k][:, 0:4, :], in_=tp[0][:, 0:512].rearrange("p (c t) -> p c t", c=4)),
                reads=[B["tp"][0]], writes=[B["aT"][k]])
            fw.op(fw.dve, lambda: nc.vector.tensor_copy(
                out=aT[k][:, 4:8, :], in_=tp[1][:, 0:512].rearrange("p (c t) -> p c t", c=4)),
                reads=[B["tp"][1]], writes=[B["aT"][k]])
            fw.dma(fw.sp, lambda: nc.sync.dma_start(out=xx[i % 3][:], in_=x[s, t0:t0 + 128, :]), writes=[B["xx"][i % 3]])

        def T3(i):
            s, tt, t0, k = idx(i)
            for half in range(2):
                for c in range(8):
                    fw.op(fw.pe, lambda: nc.tensor.matmul(
                        out=mm[half][:], lhsT=aT[k][:, c, :], rhs=wout[:, c, half * 512:(half + 1) * 512],
                        start=(c == 0), stop=(c == 7)), reads=[B["aT"][k], B["wout"]], writes=[B["mm"][half]],
                        signal=(c == 7))

        def T4(i):
            s, tt, t0, k = idx(i)
            for half in range(2):
                fw.op(fw.dve, lambda: nc.vector.tensor_tensor(
                    out=x1t[k][:, half * 512:(half + 1) * 512], in0=mm[half][:], in1=xx[i % 3][:, half * 512:(half + 1) * 512],
                    op=ALU.add), reads=[B["mm"][half], B["xx"][i % 3]], writes=[B["x1t"][k]])
            fw.dma(fw.pool, lambda: nc.gpsimd.dma_start(out=x1d[s][t0:t0 + 128, :], in_=x1t[k][:]),
                   reads=[B["x1t"][k]], writes=[B_x1d], join=True, owner=B["x1t"][k])
            fw.op(fw.dve, lambda: nc.vector.scalar_tensor_tensor(
                out=junk[:], in0=x1t[k][:], scalar=1.0, in1=x1t[k][:], op0=ALU.mult, op1=ALU.mult, accum_out=ss[k][:]),
                reads=[B["x1t"][k]], writes=[B["junk"], B["ss"][k]])
            fw.op(fw.dve, lambda: nc.vector.tensor_scalar(
                out=rstd[k][:], in0=ss[k][:], scalar1=1.0 / D, scalar2=EPS, op0=ALU.mult, op1=ALU.add),
                reads=[B["ss"][k]], writes=[B["rstd"][k]])

        def T5(i):
            s, tt, t0, k = idx(i)
            fw.op(fw.act, lambda: nc.scalar.activation(out=rstd[k][:], in_=rstd[k][:], func=ACTF.Ln),
                  reads=[B["rstd"][k]], writes=[B["rstd"][k]])
            fw.op(fw.act, lambda: nc.scalar.activation(out=rstd[k][:], in_=rstd[k][:], func=ACTF.Exp, scale=-0.5),
                  reads=[B["rstd"][k]], writes=[B["rstd"][k]])

        def T6(i):
            s, tt, t0, k = idx(i)
            fw.op(fw.dve, lambda: nc.vector.scalar_tensor_tensor(
                out=h2f[:], in0=x1t[k][:], scalar=rstd[k][:], in1=g2b[:], op0=ALU.mult, op1=ALU.mult),
                reads=[B["x1t"][k], B["rstd"][k], B["g2b"]], writes=[B["h2f"]])

        def T7(i):
            s, tt, t0, k = idx(i)
            fw.op(fw.act, lambda: nc.scalar.copy(out=h2b[k][:], in_=h2f[:]), reads=[B["h2f"]], writes=[B["h2b"][k]])
            fw.dma(fw.act, lambda: nc.scalar.dma_start(out=h2d[s][t0:t0 + 128, :], in_=h2b[k][:]),
                   reads=[B["h2b"][k]], writes=[B_h2d], join=True, owner=B["h2b"][k])
            for c in range(8):
                fw.op(fw.pe, lambda: nc.tensor.transpose(
                    out=tf[c // 4][:, (c % 4) * 128:(c % 4 + 1) * 128], in_=h2f[:, c * 128:(c + 1) * 128],
                    identity=identf[:]), reads=[B["h2f"], B["identf"]], writes=[B["tf"][c // 4]], signal=(c % 4 == 3))

        def T8(i):
            fw.op(fw.act, lambda: nc.scalar.copy(out=h2T[:, 0:4, :], in_=tf[0][:].rearrange("p (c t) -> p c t", c=4)),
                  reads=[B["tf"][0]], writes=[B["h2T"]])
            fw.op(fw.dve, lambda: nc.vector.tensor_copy(out=h2T[:, 4:8, :], in_=tf[1][:].rearrange("p (c t) -> p c t", c=4)),
                  reads=[B["tf"][1]], writes=[B["h2T"]])

        def T9(i):
            for c in range(8):
                fw.op(fw.pe, lambda: nc.tensor.matmul(
                    out=lg[:, 0:NE], lhsT=h2T[:, c, :], rhs=wr[:, c, :], start=(c == 0), stop=(c == 7)),
                    reads=[B["h2T"], B["wr"]], writes=[B["lg"]], signal=(c == 7))

        def T10(i):
            fw.op(fw.act, lambda: nc.scalar.activation(out=ex[:], in_=lg[:, 0:NE], func=ACTF.Exp, accum_out=se[:]),
                  reads=[B["lg"]], writes=[B["ex"], B["se"]])

        def T11(i):
            fw.op(fw.dve, lambda: nc.vector.reciprocal(out=se[:], in_=se[:]), reads=[B["se"]], writes=[B["se"]])
            fw.op(fw.dve, lambda: nc.vector.tensor_scalar(out=aff[:], in0=ex[:], scalar1=se[:], scalar2=None, op0=ALU.mult),
                  reads=[B["ex"], B["se"]], writes=[B["aff"]])

        def T12(i):
            fw.op(fw.pe, lambda: nc.tensor.transpose(out=at[0:NE, 0:128], in_=aff[:, 0:NE], identity=identf[:]),
                  reads=[B["aff"], B["identf"]], writes=[B["at"]])

        def T13(i):
            s, tt, t0, k = idx(i)
            fw.op(fw.act, lambda: nc.scalar.copy(out=affT[s][0:NE, t0:t0 + 128], in_=at[0:NE, 0:128]),
                  reads=[B["at"]], writes=[B_affT[s]])
            if tt == 31 and s >= 1:
                fw.dma(fw.sp, lambda: nc.sync.dma_start(out=affT[0][32 * s:32 * s + NE, :], in_=affT[s][0:NE, :]),
                       reads=[B_affT[s]], writes=[B_affT[0]])

        stages = [T0, T1, T2, T3, T4, T5, T6, T7, T8, T9, T10, T11, T12, T13]
        load_au(0)
        for n in range(nblk128 + len(stages) - 1):
            for kst in reversed(range(len(stages))):
                i = n - kst
                if 0 <= i < nblk128:
                    stages[kst](i)


def phase_e_topk(nc, fw, NS, identf_d, affT, B_affT, idxc, gc, B_idxc, B_gc):
    NPT = 32 * (NS - 1) + NE
    with ExitStack() as es:
        sb = lambda name, shape, dt: es.enter_context(nc.sbuf_tensor("E_" + name, shape, dt))
        ps = lambda name, shape, dt: es.enter_context(nc.psum_tensor("E_" + name, shape, dt))
        identf = sb("identf", [128, 128], F32)
        B_identf = Buf("identf")
        fw.dma(fw.sp, lambda: nc.sync.dma_start(out=identf[:], in_=identf_d), writes=[B_identf])
        work = sb("work", [NPT, S], F32)
        vals = sb("vals", [NPT, CAP], F32)
        idxu = sb("idxu", [NPT, CAP], U32)
        idxf = sb("idxf", [NPT, CAP], F32)
        pt = ps("pt", [128, 512], F32)
        B_work, B_vals, B_idxu, B_idxf, B_pt = [Buf(n) for n in ["work", "vals", "idxu", "idxf", "pt"]]
        stk = affT[0]
        fw.op(fw.dve, lambda: nc.vector.tensor_copy(out=work[:], in_=stk[0:NPT, :]), reads=[B_affT[0]], writes=[B_work])
        for it in range(CAP // 8):
            sl = slice(it * 8, (it + 1) * 8)
            fw.op(fw.dve, lambda: nc.vector.max(out=vals[:, sl], in_=work[:]), reads=[B_work], writes=[B_vals])
            fw.op(fw.dve, lambda: nc.vector.max_index(out=idxu[:, sl], in_max=vals[:, sl], in_values=work[:]),
                  reads=[B_work, B_vals], writes=[B_idxu])
            fw.op(fw.dve, lambda: nc.vector.match_replace(
                out=work[:], in_to_replace=vals[:, sl], in_values=work[:], imm_value=-1.0),
                reads=[B_work, B_vals], writes=[B_work])
        fw.op(fw.dve, lambda: nc.vector.tensor_copy(out=idxf[:], in_=idxu[:]), reads=[B_idxu], writes=[B_idxf])
        for src_t, bsrc, dst_t, bdst in [(idxf, B_idxf, idxc, B_idxc), (vals, B_vals, gc, B_gc)]:
            for j in range(4):
                fw.op(fw.pe, lambda: nc.tensor.transpose(
                    out=pt[:, j * NPT:(j + 1) * NPT], in_=src_t[:, j * 128:(j + 1) * 128], identity=identf[0:NPT, 0:NPT]),
                    reads=[bsrc, B_identf], writes=[B_pt], signal=(j == 3))
            fw.op(fw.dve, lambda: nc.vector.tensor_copy(out=dst_t[:], in_=pt[:, 0:4 * NPT]), reads=[B_pt], writes=[bdst])


def phase_e_experts(nc, fw, NS, h2d, x1d, w_gate, w_up, w_down, ident_d, idxc, gc, B_idxc, B_gc, ring=None):
    NPT = 32 * (NS - 1) + NE
    GRP = [(0, 4), (4, 4), (8, 4), (12, 4), (16, 4), (20, 2)]
    NG = len(GRP)
    RING = 3
    with ExitStack() as es:
        sb = lambda name, shape, dt: es.enter_context(nc.sbuf_tensor("E3_" + name, shape, dt))
        ps = lambda name, shape, dt: es.enter_context(nc.psum_tensor("E3_" + name, shape, dt))
        ident = sb("ident", [128, 128], BF16)
        B_ident = Buf("ident")
        fw.dma(fw.sp, lambda: nc.sync.dma_start(out=ident[:], in_=ident_d), writes=[B_ident])
        if ring is None:
            wgu = [sb(f"wgu{i}", [128, 2, 8, 512], BF16) for i in range(RING)]
        else:
            wgu = ring["wgu"]
        wd = sb("wd", [128, NF, D], BF16)
        xe = [sb(f"xe{s}", [128, 4, D], BF16) for s in range(NS)]
        xeT = [sb(f"xeT{s}", [128, 8, 512], BF16) for s in range(NS)]
        heT = [sb(f"heT{s}", [128, NF, 512], BF16) for s in range(NS)]
        sg = [sb(f"sg{i}", [128, 512], F32) for i in range(2)]
        yeg = [sb(f"yeg{i}", [128, D], F32) for i in range(2)]
        tp = [ps(f"tp{i}", [128, 1024], BF16) for i in range(2)]
        pg = [ps(f"pg{i}", [128, 512], F32) for i in range(2)]
        pu = [ps(f"pu{i}", [128, 512], F32) for i in range(2)]
        py = [ps(f"py{i}", [128, 512], F32) for i in range(2)]
        B_wgu = [Buf(f"wgu{i}") for i in range(RING)] if ring is None else ring["B_wgu"]
        B_wd = Buf("wd")
        B_xe = [Buf(f"xe{s}") for s in range(NS)]
        B_xeT = [Buf(f"xeT{s}") for s in range(NS)]
        B_heT = [Buf(f"heT{s}") for s in range(NS)]
        B_sg = [Buf("sg0"), Buf("sg1")]
        B_yeg = [Buf("yeg0"), Buf("yeg1")]
        B_tp = [Buf("tp0"), Buf("tp1")]
        B_pg = [Buf("pg0"), Buf("pg1")]
        B_pu = [Buf("pu0"), Buf("pu1")]
        B_py = [Buf("py0"), Buf("py1")]
        B_x1d = [Buf(f"x1d{s}") for s in range(NS)]

        def load_group(gi):
            e, g = divmod(gi, NG)
            f0, nf = GRP[g]
            slot = gi % RING
            c0, c1 = f0 * 128, (f0 + nf) * 128
            fw.dma(fw.pool, lambda: nc.gpsimd.dma_start(
                out=wgu[slot][:, 0, :, 0:nf * 128], in_=w_gate[e, :, c0:c1].rearrange("(c p) n -> p c n", p=128)),
                writes=[B_wgu[slot]])
            fw.dma(fw.pool, lambda: nc.gpsimd.dma_start(
                out=wgu[slot][:, 1, :, 0:nf * 128], in_=w_up[e, :, c0:c1].rearrange("(c p) n -> p c n", p=128)),
                writes=[B_wgu[slot]], join=True)

        def load_wd(e):
            fw.dma(fw.pool, lambda: nc.gpsimd.dma_start(out=wd[:], in_=w_down[e].rearrange("(c p) n -> p c n", p=128)),
                   writes=[B_wd])

        def gathers(e):
            for s in range(NS):
                for j in range(4):
                    col = j * NPT + 32 * s + e
                    fw.dma(fw.pool, lambda: nc.gpsimd.indirect_dma_start(
                        out=xe[s][:, j, :], out_offset=None, in_=h2d[s],
                        in_offset=bass.IndirectOffsetOnAxis(ap=idxc[:, col:col + 1], axis=0)),
                        reads=[B_idxc], writes=[B_xe[s]], join=True)

        tpi = [0]

        def transposes(e):
            for s in range(NS):
                for kc in range(8):
                    t = tpi[0] % 2
                    tpi[0] += 1
                    for j in range(4):
                        fw.op(fw.pe, lambda: nc.tensor.transpose(
                            out=tp[t][:, j * 128:(j + 1) * 128], in_=xe[s][:, j, kc * 128:(kc + 1) * 128], identity=ident[:]),
                            reads=[B_xe[s], B_ident], writes=[B_tp[t]], signal=(j == 3))
                    if t == 0:
                        fw.op(fw.act, lambda: nc.scalar.copy(out=xeT[s][:, kc, :], in_=tp[t][:, 0:512]),
                              reads=[B_tp[t]], writes=[B_xeT[s]])
                    else:
                        fw.op(fw.dve, lambda: nc.vector.tensor_copy(out=xeT[s][:, kc, :], in_=tp[t][:, 0:512]),
                              reads=[B_tp[t]], writes=[B_xeT[s]])

        if ring is None:
            for gi in range(RING):
                load_group(gi)
        load_wd(0)
        gathers(0)
        transposes(0)
        fi = 0
        yi = 0
        for e in range(NE):
            if e + 1 < NE:
                gathers(e + 1)
            for g in range(NG):
                gi = e * NG + g
                f0, nf = GRP[g]
                slot = gi % RING
                for ff in range(nf):
                    f = f0 + ff
                    for s in range(NS):
                        t = fi % 2
                        fi += 1
                        for kc in range(8):
                            fw.op(fw.pe, lambda: nc.tensor.matmul(
                                out=pg[t][:], lhsT=wgu[slot][:, 0, kc, ff * 128:(ff + 1) * 128], rhs=xeT[s][:, kc, :],
                                start=(kc == 0), stop=(kc == 7)), reads=[B_wgu[slot], B_xeT[s]], writes=[B_pg[t]],
                                signal=(kc == 7))
                        for kc in range(8):
                            fw.op(fw.pe, lambda: nc.tensor.matmul(
                                out=pu[t][:], lhsT=wgu[slot][:, 1, kc, ff * 128:(ff + 1) * 128], rhs=xeT[s][:, kc, :],
                                start=(kc == 0), stop=(kc == 7)), reads=[B_wgu[slot], B_xeT[s]], writes=[B_pu[t]],
                                signal=(kc == 7))
                        fw.op(fw.act, lambda: nc.scalar.activation(out=sg[t][:], in_=pg[t][:], func=ACTF.Silu),
                              reads=[B_pg[t]], writes=[B_sg[t]])
                        fw.op(fw.dve, lambda: nc.vector.tensor_tensor(
                            out=heT[s][:, f, :], in0=sg[t][:], in1=pu[t][:], op=ALU.mult),
                            reads=[B_sg[t], B_pu[t]], writes=[B_heT[s]])
                if gi + RING < NE * NG:
                    load_group(gi + RING)
            if e + 1 < NE:
                transposes(e + 1)
            for s in range(NS):
                for j in range(4):
                    col = j * NPT + 32 * s + e
                    y = yeg[yi % 2]
                    by = B_yeg[yi % 2]
                    yi += 1
                    for half in range(2):
                        for f in range(NF):
                            fw.op(fw.pe, lambda: nc.tensor.matmul(
                                out=py[half][:], lhsT=heT[s][:, f, j * 128:(j + 1) * 128],
                                rhs=wd[:, f, half * 512:(half + 1) * 512],
                                start=(f == 0), stop=(f == NF - 1)), reads=[B_heT[s], B_wd], writes=[B_py[half]],
                                signal=(f == NF - 1))
                        if half == 0:
                            fw.op(fw.dve, lambda: nc.vector.tensor_scalar(
                                out=y[:, 0:512], in0=py[0][:], scalar1=gc[:, col:col + 1],
                                scalar2=None, op0=ALU.mult), reads=[B_py[0], B_gc], writes=[by])
                        else:
                            fw.op(fw.act, lambda: nc.scalar.activation(
                                out=y[:, 512:1024], in_=py[1][:], func=ACTF.Copy, scale=gc[:, col:col + 1]),
                                reads=[B_py[1], B_gc], writes=[by])
                    fw.dma(fw.pool, lambda: nc.gpsimd.indirect_dma_start(
                        out=x1d[s], out_offset=bass.IndirectOffsetOnAxis(ap=idxc[:, col:col + 1], axis=0),
                        in_=y[:, :], in_offset=None, compute_op=ALU.add),
                        reads=[by, B_idxc], writes=[B_x1d[s]])
            if e + 1 < NE:
                load_wd(e + 1)


def phase_f(nc, fw, NS, x1d, gf, out):
    with ExitStack() as es:
        sb = lambda name, shape, dt: es.enter_context(nc.sbuf_tensor("F_" + name, shape, dt))
        NB = 4
        gfb = sb("gfb", [128, D], F32)
        xt = [sb(f"xt{i}", [128, D], F32) for i in range(NB)]
        ot = [sb(f"ot{i}", [128, D], F32) for i in range(2)]
        junk = sb("junk", [128, D], F32)
        ss = [sb(f"ss{i}", [128, 1], F32) for i in range(NB)]
        rstd = [sb(f"rstd{i}", [128, 1], F32) for i in range(NB)]
        B_gfb, B_junk = Buf("gfb"), Buf("junk")
        B_ss = [Buf(f"ss{i}") for i in range(NB)]
        B_rstd = [Buf(f"rstd{i}") for i in range(NB)]
        B_xt = [Buf(f"xt{i}") for i in range(NB)]
        B_ot = [Buf("ot0"), Buf("ot1")]
        B_out = Buf("out")
        fw.dma(fw.sp, lambda: nc.sync.dma_start(out=gfb[:], in_=gf.partition_broadcast(128)), writes=[B_gfb])
        nblk128 = NS * 32

        def load(i):
            s, tt = divmod(i, 32)
            fw.dma(fw.sp, lambda: nc.sync.dma_start(out=xt[i % NB][:], in_=x1d[s][tt * 128:(tt + 1) * 128, :]),
                   writes=[B_xt[i % NB]])

        def F0(i):
            k = i % NB
            if i + 2 < nblk128:
                load(i + 2)
            fw.op(fw.dve, lambda: nc.vector.scalar_tensor_tensor(
                out=junk[:], in0=xt[k][:], scalar=1.0, in1=xt[k][:], op0=ALU.mult, op1=ALU.mult, accum_out=ss[k][:]),
                reads=[B_xt[k]], writes=[B_junk, B_ss[k]])
            fw.op(fw.dve, lambda: nc.vector.tensor_scalar(
                out=rstd[k][:], in0=ss[k][:], scalar1=1.0 / D, scalar2=EPS, op0=ALU.mult, op1=ALU.add),
                reads=[B_ss[k]], writes=[B_rstd[k]])

        def F1(i):
            k = i % NB
            fw.op(fw.act, lambda: nc.scalar.activation(out=rstd[k][:], in_=rstd[k][:], func=ACTF.Sqrt),
                  reads=[B_rstd[k]], writes=[B_rstd[k]])

        def F2(i):
            k = i % NB
            s, tt = divmod(i, 32)
            fw.op(fw.dve, lambda: nc.vector.reciprocal(out=rstd[k][:], in_=rstd[k][:]), reads=[B_rstd[k]], writes=[B_rstd[k]])
            fw.op(fw.dve, lambda: nc.vector.scalar_tensor_tensor(
                out=ot[i % 2][:], in0=xt[k][:], scalar=rstd[k][:], in1=gfb[:], op0=ALU.mult, op1=ALU.mult),
                reads=[B_xt[k], B_rstd[k], B_gfb], writes=[B_ot[i % 2]])
            fw.dma(fw.act, lambda: nc.scalar.dma_start(out=out[s, tt * 128:(tt + 1) * 128, :], in_=ot[i % 2][:]),
                   reads=[B_ot[i % 2]], writes=[B_out], join=True, owner=B_ot[i % 2], is_output=True)

        stages = [F0, F1, F2]
        load(0)
        if nblk128 > 1:
            load(1)
        for n in range(nblk128 + len(stages) - 1):
            for kst in reversed(range(len(stages))):
                i = n - kst
                if 0 <= i < nblk128:
                    stages[kst](i)


def build_full(NS, stop_after="F"):
    nc = bass.Bass("TRN2", target_bir_lowering=False)
    EI = "ExternalInput"
    x = nc.dram_tensor("x", [NS, S, D], F32, kind=EI).ap()
    w_in = nc.dram_tensor("w_in", [D, NIN], F32, kind=EI).ap()
    g1 = nc.dram_tensor("norm1_g", [1, D], F32, kind=EI).ap()
    ident_d = nc.dram_tensor("ident", [128, 128], BF16, kind=EI).ap()
    identf_d = nc.dram_tensor("identf", [128, 128], F32, kind=EI).ap()
    rel_bias = nc.dram_tensor("rel_bias", [32, 12], F32, kind=EI).ap()
    onehot_d = nc.dram_tensor("onehot", [32, LTOT], BF16, kind=EI).ap()
    antiid_d = nc.dram_tensor("antiid", [128, 128], BF16, kind=EI).ap()
    lamv = nc.dram_tensor("lamv", [4, 1, 64], F32, kind=EI).ap()
    subln_g = nc.dram_tensor("subln_g", [1, 128], F32, kind=EI).ap()
    w_out = nc.dram_tensor("w_out", [D, D], F32, kind=EI).ap()
    g2 = nc.dram_tensor("norm2_g", [1, D], F32, kind=EI).ap()
    w_router = nc.dram_tensor("w_router", [D, NE], F32, kind=EI).ap()
    w_gate = nc.dram_tensor("w_gate", [NE, D, DFF], F32, kind=EI).ap()
    w_up = nc.dram_tensor("w_up", [NE, D, DFF], F32, kind=EI).ap()
    w_down = nc.dram_tensor("w_down", [NE, DFF, D], F32, kind=EI).ap()
    gf = nc.dram_tensor("norm_f_g", [1, D], F32, kind=EI).ap()
    kind = "Internal"
    qaT = nc.dram_tensor("qaT", [NS, 4, 128, S], BF16, kind=kind).ap()
    kaT = nc.dram_tensor("kaT", [NS, 4, 128, S], BF16, kind=kind).ap()
    qdT = nc.dram_tensor("qdT", [NS, 4, 128, S], BF16, kind=kind).ap()
    kdT = nc.dram_tensor("kdT", [NS, 4, 128, S], BF16, kind=kind).ap()
    va = nc.dram_tensor("va", [NS, S, 4 * 129], BF16, kind=kind).ap()
    vd = nc.dram_tensor("vd", [NS, S, 8 * 65], BF16, kind=kind).ap()
    a_dram = nc.dram_tensor("a_dram", [12, LTOT], BF16, kind=kind).ap()
    attn = nc.dram_tensor("attn", [NS, S, D], BF16, kind=kind).ap()
    U = nc.dram_tensor("U", [3, NS, S, 520], F32, kind=kind).ap()
    dbg = stop_after != "F"
    x1d = [nc.dram_tensor(f"x1d{i}", [S, D], F32, kind=kind).ap() for i in range(NS)]
    h2d = [nc.dram_tensor(f"h2d{i}", [S, D], BF16, kind=kind).ap() for i in range(NS)]
    out = nc.dram_tensor("out", [NS, S, D], F32, kind="ExternalOutput").ap()
    with ExitStack() as stack:
        fw = FW(nc, stack)
        with ExitStack() as es_tab:
            with nc.named_scope("tables"):
                tabs = setup_tables(nc, fw, es_tab, rel_bias, onehot_d, antiid_d, a_dram, lamv, subln_g)
            fw_barrier(fw)
            with nc.named_scope("phA"):
                phase_a(nc, fw, NS, x, w_in, g1, ident_d, qaT, kaT, va, qdT, kdT, vd)
            fw_barrier(fw)
            with nc.named_scope("phB"):
                phase_b(nc, fw, NS, tabs, qaT, kaT, va, attn)
            fw_barrier(fw)
            with nc.named_scope("phC"):
                phase_c(nc, fw, NS, tabs, qdT, kdT, vd, U)
            fw_barrier(fw)
        fw.out_events = []
        with ExitStack() as es_idx:
            RING_N = 3
            GRP0 = [(0, 4), (4, 4), (8, 4)]
            ring = {"wgu": [es_idx.enter_context(nc.sbuf_tensor(f"R_wgu{i}", [128, 2, 8, 512], BF16)) for i in range(RING_N)],
                    "B_wgu": [Buf(f"R_wgu{i}") for i in range(RING_N)]}
            def prefetch_ring():
                if stop_after == "D":
                    return
                for gi, (f0, nf) in enumerate(GRP0):
                    c0, c1 = f0 * 128, (f0 + nf) * 128
                    fw.dma(fw.pool, lambda: nc.gpsimd.dma_start(
                        out=ring["wgu"][gi][:, 0, :, 0:nf * 128], in_=w_gate[0, :, c0:c1].rearrange("(c p) n -> p c n", p=128)),
                        writes=[ring["B_wgu"][gi]])
                    fw.dma(fw.pool, lambda: nc.gpsimd.dma_start(
                        out=ring["wgu"][gi][:, 1, :, 0:nf * 128], in_=w_up[0, :, c0:c1].rearrange("(c p) n -> p c n", p=128)),
                        writes=[ring["B_wgu"][gi]], join=True)
            NPT = 32 * (NS - 1) + NE
            idxc = es_idx.enter_context(nc.sbuf_tensor("idxc", [128, 4 * NPT], I32))
            gc = es_idx.enter_context(nc.sbuf_tensor("gc", [128, 4 * NPT], F32))
            B_idxc = Buf("idxc")
            B_gc = Buf("gc")
            with ExitStack() as es_aff:
                affT = [es_aff.enter_context(nc.sbuf_tensor(f"affT{s}", [NPT if s == 0 else NE, S], F32)) for s in range(NS)]
                B_affT = [Buf(f"affT{s}") for s in range(NS)]
                fw.op(fw.dve, lambda: nc.vector.memset(affT[0][:], 0.0), writes=[B_affT[0]])
                with nc.named_scope("phD"):
                    phase_d(nc, fw, NS, attn, U, x, w_out, g2, w_router, ident_d, identf_d, x1d, h2d, affT, B_affT, hook=prefetch_ring)
                fw_barrier(fw)
                if stop_after != "D":
                    with nc.named_scope("phEtopk"):
                        phase_e_topk(nc, fw, NS, identf_d, affT, B_affT, idxc, gc, B_idxc, B_gc)
                    fw_barrier(fw)
            if stop_after != "D":
                with nc.named_scope("phEexp"):
                    phase_e_experts(nc, fw, NS, h2d, x1d, w_gate, w_up, w_down, ident_d, idxc, gc, B_idxc, B_gc, ring=ring)
                fw_barrier(fw)
        with nc.named_scope("phF"):
            phase_f(nc, fw, NS, x1d, gf, out)
        fw.finish()
    return nc


def kernel(**inputs):
    import ml_dtypes
    NS = 2
    NCORES = 8
    f32 = lambda a: np.ascontiguousarray(np.asarray(a), dtype=np.float32)
    x = f32(inputs["x"])
    common = {
        "w_in": f32(inputs["w_in"])[0], "norm1_g": f32(inputs["norm1_g"]).reshape(1, D),
        "rel_bias": f32(inputs["rel_bias"]),
        "lamv": np.stack([f32(inputs[k]).reshape(1, 64) for k in ["lam_q1", "lam_k1", "lam_q2", "lam_k2"]]),
        "subln_g": f32(inputs["subln_g"]).reshape(1, 128), "w_out": f32(inputs["w_out"])[0],
        "norm2_g": f32(inputs["norm2_g"]).reshape(1, D), "w_router": f32(inputs["w_router"])[0],
        "w_gate": f32(inputs["w_gate"])[0], "w_up": f32(inputs["w_up"])[0], "w_down": f32(inputs["w_down"])[0],
        "norm_f_g": f32(inputs["norm_f_g"]).reshape(1, D),
        "identf": np.eye(128, dtype=np.float32), **consts_np(), **onehot_np(),
    }
    nc = build_full(NS)
    in_maps = [{"x": np.ascontiguousarray(x[NS * c:NS * (c + 1)]), **common} for c in range(NCORES)]
    res = run_bass_kernel_spmd(nc, in_maps, core_ids=list(range(NCORES)))
    out = np.concatenate([np.asarray(r["out"], dtype=np.float32) for r in res.results], axis=0)
    return out.astype(np.float32)
```
